# Optimizing a Trainium2 kernel written in Bass

```python
import math
import jax, jax.numpy as jnp
from jax import lax
import numpy as np

D_MODEL = 2048
BATCH = 4
SEQ = 4096
DEPTH = 1

CHUNK = 64
S5_WIDTH = 512
S5_GROUP = 16
S5_GROUPS = S5_WIDTH // S5_GROUP
S5_STATE = 64
CONV_CHANNELS = 1024
CONV_SPAN = 31
N_BRANCHES = 2
IN_COLS = S5_WIDTH + 2 * CONV_CHANNELS + N_BRANCHES * D_MODEL
N_EXPERT_GROUPS = 8
EXPERTS_PER_GROUP = 8
N_EXPERTS = N_EXPERT_GROUPS * EXPERTS_PER_GROUP
TOP_K = 2
D_EXPERT = 512
MOE_BLOCK = 128
N_MOD = 6
DEEPNORM_ALPHA = (2.0 * DEPTH) ** 0.25
DEEPNORM_BETA = (8.0 * DEPTH) ** -0.25
LN_EPS = 1e-5

kernel_name = "hybrid_s5_conformer_hmoe_deepnorm"


def _layernorm(x, gain=None, bias=None):
    xf = x.astype(jnp.float32)
    mu = jnp.mean(xf, -1, keepdims=True)
    var = jnp.mean(jnp.square(xf - mu), -1, keepdims=True)
    y = (xf - mu) * lax.rsqrt(var + LN_EPS)
    if gain is not None:
        y = y * gain.astype(jnp.float32) + bias.astype(jnp.float32)
    return y.astype(x.dtype)


def _modulate(x, shift, scale):
    return _layernorm(x) * (1 + scale[:, None, :]) + shift[:, None, :]


def _complex_affine_combine(left, right):
    a1r, a1i, b1r, b1i = left
    a2r, a2i, b2r, b2i = right
    return (a2r * a1r - a2i * a1i,
            a2r * a1i + a2i * a1r,
            a2r * b1r - a2i * b1i + b2r,
            a2r * b1i + a2i * b1r + b2i)


def _s5_branch(u, a_re, a_im, log_dt, b_re, b_im, c_re, c_im, d_skip, w_gate):
    f32 = jnp.float32
    bsz, seq, _ = u.shape
    uf = u.astype(f32).reshape(bsz, seq, S5_GROUPS, S5_GROUP)
    dt = jnp.exp(log_dt.astype(f32))[:, None]
    lr = a_re.astype(f32)
    li = a_im.astype(f32)
    mag = jnp.exp(lr * dt)
    ab_re = mag * jnp.cos(li * dt)
    ab_im = mag * jnp.sin(li * dt)
    den = lr * lr + li * li
    nr = ab_re - 1.0
    ni = ab_im
    q_re = ((nr * lr + ni * li) / den)[:, :, None]
    q_im = ((ni * lr - nr * li) / den)[:, :, None]
    br = b_re.astype(f32)
    bi = b_im.astype(f32)
    bb_re = q_re * br - q_im * bi
    bb_im = q_re * bi + q_im * br
    bu_re = jnp.einsum('gph,bsgh->bsgp', bb_re, uf)
    bu_im = jnp.einsum('gph,bsgh->bsgp', bb_im, uf)
    a_seq_re = jnp.broadcast_to(ab_re[None, None], (1, seq, S5_GROUPS, S5_STATE))
    a_seq_im = jnp.broadcast_to(ab_im[None, None], (1, seq, S5_GROUPS, S5_STATE))
    _, _, s_re, s_im = lax.associative_scan(
        _complex_affine_combine, (a_seq_re, a_seq_im, bu_re, bu_im), axis=1)
    y = (jnp.einsum('ghp,bsgp->bsgh', c_re.astype(f32), s_re)
         - jnp.einsum('ghp,bsgp->bsgh', c_im.astype(f32), s_im)
         + d_skip.astype(f32) * uf)
    y = jax.nn.gelu(y.reshape(bsz, seq, S5_WIDTH)).astype(u.dtype)
    return y * jax.nn.sigmoid(y @ w_gate)


def _conv_branch(z, w_dw, b_dw, ln_g, ln_b):
    a, g = jnp.split(z, 2, axis=-1)
    v = a * jax.nn.sigmoid(g)
    v = lax.conv_general_dilated(
        v, w_dw, window_strides=(1,), padding=[(CONV_SPAN - 1, 0)],
        dimension_numbers=('NWC', 'WIO', 'NWC'),
        feature_group_count=CONV_CHANNELS) + b_dw
    v = _layernorm(v, ln_g, ln_b)
    return jax.nn.silu(v)


def _hier_moe(h, w_rg, b_rg, w_re, b_re, w_g, w_u, w_d):
    f32 = jnp.float32
    bsz, seq, d = h.shape
    t = bsz * seq
    hf = h.reshape(t, d)
    g_logits = (hf @ w_rg).astype(f32) + b_rg.astype(f32)
    g_prob = jax.nn.softmax(g_logits, axis=-1)
    grp = jnp.argmax(g_logits, axis=-1).astype(jnp.int32)
    grp_w = jnp.take_along_axis(g_prob, grp[:, None], axis=-1)
    e_logits = ((hf @ w_re).astype(f32) + b_re.astype(f32)).reshape(
        t, N_EXPERT_GROUPS, EXPERTS_PER_GROUP)
    idx = jnp.broadcast_to(grp[:, None, None], (t, 1, EXPERTS_PER_GROUP))
    e_logits = jnp.take_along_axis(e_logits, idx, axis=1)[:, 0]
    top_val, top_loc = lax.top_k(e_logits, TOP_K)
    slot_w = grp_w * jax.nn.softmax(top_val, axis=-1)
    slot_e = grp[:, None] * EXPERTS_PER_GROUP + top_loc.astype(jnp.int32)

    n_slots = t * TOP_K
    flat_e = slot_e.reshape(-1)
    flat_w = slot_w.reshape(-1)
    order = jnp.argsort(flat_e)
    sorted_e = flat_e[order]
    slot_token = (order // TOP_K).astype(jnp.int32)
    counts = jnp.zeros((N_EXPERTS,), jnp.int32).at[flat_e].add(1)
    padded = (counts + MOE_BLOCK - 1) // MOE_BLOCK * MOE_BLOCK
    starts = jnp.cumsum(counts) - counts
    pends = jnp.cumsum(padded)
    pstarts = pends - padded
    dest = pstarts[sorted_e] + jnp.arange(n_slots, dtype=jnp.int32) - starts[sorted_e]
    n_blocks = -(-n_slots // MOE_BLOCK) + N_EXPERTS
    n_rows = n_blocks * MOE_BLOCK
    row_token = jnp.full((n_rows,), t, jnp.int32).at[dest].set(slot_token)
    h_pad = jnp.concatenate([hf, jnp.zeros((1, d), hf.dtype)], axis=0)
    x_rows = h_pad[row_token].reshape(n_blocks, MOE_BLOCK, d)
    block_start = jnp.arange(n_blocks, dtype=jnp.int32) * MOE_BLOCK
    block_expert = jnp.minimum(jnp.searchsorted(pends, block_start, side='right'),
                               N_EXPERTS - 1).astype(jnp.int32)

    def expert_block(args):
        xb, e = args
        return (jax.nn.silu(xb @ w_g[e]) * (xb @ w_u[e])) @ w_d[e]

    y_rows = lax.map(expert_block, (x_rows, block_expert)).reshape(n_rows, d)
    y_slots = y_rows[dest] * flat_w[order][:, None].astype(y_rows.dtype)
    out = jax.ops.segment_sum(y_slots, slot_token, num_segments=t)
    return out.reshape(bsz, seq, d)


def setup_inputs(seed: int = 0) -> dict:
    key = jax.random.key(seed)
    ks = jax.random.split(key, 40)
    f32 = jnp.float32
    L = DEPTH

    def nrm(k, shape, scale):
        return jax.random.normal(k, shape, f32) * scale

    a_im_base = jnp.pi * jnp.arange(S5_STATE, dtype=f32)
    return {
        "x": nrm(ks[0], (BATCH, SEQ, D_MODEL), 1.0),
        "c": nrm(ks[1], (BATCH, D_MODEL), 1.0),
        "w_ada": nrm(ks[2], (L, D_MODEL, N_MOD * D_MODEL), 0.1 * D_MODEL ** -0.5),
        "b_ada": nrm(ks[3], (L, N_MOD * D_MODEL), 0.01),
        "w_in": nrm(ks[4], (L, D_MODEL, IN_COLS), D_MODEL ** -0.5),
        "b_in": nrm(ks[5], (L, IN_COLS), 0.01),
        "s5_a_re": -0.5 * (1.0 + nrm(ks[6], (L, S5_GROUPS, S5_STATE), 0.01)),
        "s5_a_im": a_im_base + nrm(ks[7], (L, S5_GROUPS, S5_STATE), 0.01),
        "s5_log_dt": jax.random.uniform(ks[8], (L, S5_GROUPS), f32,
                                        minval=math.log(1e-3), maxval=math.log(1e-1)),
        "s5_b_re": nrm(ks[9], (L, S5_GROUPS, S5_STATE, S5_GROUP), (2.0 * S5_GROUP) ** -0.5),
        "s5_b_im": nrm(ks[10], (L, S5_GROUPS, S5_STATE, S5_GROUP), (2.0 * S5_GROUP) ** -0.5),
        "s5_c_re": nrm(ks[11], (L, S5_GROUPS, S5_GROUP, S5_STATE), (2.0 * S5_STATE) ** -0.5),
        "s5_c_im": nrm(ks[12], (L, S5_GROUPS, S5_GROUP, S5_STATE), (2.0 * S5_STATE) ** -0.5),
        "s5_d": nrm(ks[13], (L, S5_GROUPS, S5_GROUP), 1.0),
        "w_s5_gate": nrm(ks[14], (L, S5_WIDTH, S5_WIDTH), S5_WIDTH ** -0.5),
        "w_s5_up": nrm(ks[15], (L, S5_WIDTH, D_MODEL), S5_WIDTH ** -0.5),
        "conv_dw": nrm(ks[16], (L, CONV_SPAN, 1, CONV_CHANNELS), CONV_SPAN ** -0.5),
        "conv_dw_b": nrm(ks[17], (L, CONV_CHANNELS), 0.01),
        "conv_ln_g": 1.0 + nrm(ks[18], (L, CONV_CHANNELS), 0.01),
        "conv_ln_b": nrm(ks[19], (L, CONV_CHANNELS), 0.01),
        "w_conv_out": nrm(ks[20], (L, CONV_CHANNELS, D_MODEL), CONV_CHANNELS ** -0.5),
        "w_out": nrm(ks[21], (L, D_MODEL, D_MODEL), DEEPNORM_BETA * D_MODEL ** -0.5),
        "ln1_g": 1.0 + nrm(ks[22], (L, D_MODEL), 0.01),
        "ln1_b": nrm(ks[23], (L, D_MODEL), 0.01),
        "w_route_group": nrm(ks[24], (L, D_MODEL, N_EXPERT_GROUPS), D_MODEL ** -0.5),
        "b_route_group": nrm(ks[25], (L, N_EXPERT_GROUPS), 0.01),
        "w_route_expert": nrm(ks[26], (L, D_MODEL, N_EXPERTS), D_MODEL ** -0.5),
        "b_route_expert": nrm(ks[27], (L, N_EXPERTS), 0.01),
        "w_exp_gate": nrm(ks[28], (L, N_EXPERTS, D_MODEL, D_EXPERT), D_MODEL ** -0.5),
        "w_exp_up": nrm(ks[29], (L, N_EXPERTS, D_MODEL, D_EXPERT), D_MODEL ** -0.5),
        "w_exp_down": nrm(ks[30], (L, N_EXPERTS, D_EXPERT, D_MODEL), DEEPNORM_BETA * D_EXPERT ** -0.5),
        "ln2_g": 1.0 + nrm(ks[31], (L, D_MODEL), 0.01),
        "ln2_b": nrm(ks[32], (L, D_MODEL), 0.01),
    }


def reference(x, c, w_ada, b_ada, w_in, b_in, s5_a_re, s5_a_im, s5_log_dt, s5_b_re, s5_b_im,
              s5_c_re, s5_c_im, s5_d, w_s5_gate, w_s5_up, conv_dw, conv_dw_b, conv_ln_g,
              conv_ln_b, w_conv_out, w_out, ln1_g, ln1_b, w_route_group, b_route_group,
              w_route_expert, b_route_expert, w_exp_gate, w_exp_up, w_exp_down, ln2_g, ln2_b):
    c_act = jax.nn.silu(c)
    for l in range(DEPTH):
        shift1, scale1, gate1, shift2, scale2, gate2 = jnp.split(
            c_act @ w_ada[l] + b_ada[l], N_MOD, axis=-1)

        h = _modulate(x, shift1, scale1)
        proj = h @ w_in[l] + b_in[l]
        u_s5 = proj[..., :S5_WIDTH]
        z_conv = proj[..., S5_WIDTH:S5_WIDTH + 2 * CONV_CHANNELS]
        gate_logits = proj[..., S5_WIDTH + 2 * CONV_CHANNELS:]
        y_s5 = _s5_branch(u_s5, s5_a_re[l], s5_a_im[l], s5_log_dt[l], s5_b_re[l], s5_b_im[l],
                          s5_c_re[l], s5_c_im[l], s5_d[l], w_s5_gate[l]) @ w_s5_up[l]
        y_conv = _conv_branch(z_conv, conv_dw[l], conv_dw_b[l], conv_ln_g[l],
                              conv_ln_b[l]) @ w_conv_out[l]
        g_s5, g_conv = jnp.split(jax.nn.sigmoid(gate_logits), N_BRANCHES, axis=-1)
        mix = (g_s5 * y_s5 + g_conv * y_conv) @ w_out[l]
        x = _layernorm(DEEPNORM_ALPHA * x + (1 + gate1[:, None, :]) * mix, ln1_g[l], ln1_b[l])

        h = _modulate(x, shift2, scale2)
        ffn = _hier_moe(h, w_route_group[l], b_route_group[l], w_route_expert[l],
                        b_route_expert[l], w_exp_gate[l], w_exp_up[l], w_exp_down[l])
        x = _layernorm(DEEPNORM_ALPHA * x + (1 + gate2[:, None, :]) * ffn, ln2_g[l], ln2_b[l])
    return x
```

```python
import contextlib
import numpy as np
import concourse.bass as bass
import concourse.mybir as mybir
from concourse.bass_utils import run_bass_kernel_spmd

F32 = mybir.dt.float32
BF16 = mybir.dt.bfloat16
I32 = mybir.dt.int32
ALU = mybir.AluOpType
AF = mybir.ActivationFunctionType
AX = mybir.AxisListType

D = 2048
NTOK = 2048
TB = 512
NBLK = NTOK // TB
INC = 6656
NE = 64
CAP = 128
ALPHA = 2.0 ** 0.25
EPS = 1e-5
MAGIC = 12582912.0
TWO_PI = 6.283185307179586
DEBUG = False


class Buf:
    def __init__(self, name):
        self.name = name
        self.w = None
        self.r = {}


class Chan:
    def __init__(self, sem):
        self.sem = sem
        self.cnt = 0


class Eng:
    def __init__(self, h, sem, is_pe=False):
        self.h = h
        self.sem = sem
        self.cnt = 0
        self.waited = {}
        self.is_pe = is_pe


class Sched:
    def __init__(self, nc, es):
        self.nc = nc
        self.es = es
        self.nsem = 0
        self.pe = Eng(nc.tensor, self.mksem(), True)
        self.dve = Eng(nc.vector, self.mksem())
        self.act = Eng(nc.scalar, self.mksem())
        self.pool = Eng(nc.gpsimd, self.mksem())
        self.sp = Eng(nc.sync, self.mksem())
        self.engs = [self.pe, self.dve, self.act, self.pool, self.sp]

    def mksem(self):
        self.nsem += 1
        return self.es.enter_context(self.nc.semaphore("sm%d" % self.nsem))

    def chan(self):
        return Chan(self.mksem())

    def _sync(self, e, reads, writes):
        deps = []
        for b in reads:
            if b.w is not None:
                deps.append(b.w)
        for b in writes:
            if b.w is not None:
                deps.append(b.w)
            deps.extend(b.r.values())
        for (sem, val) in deps:
            if e.is_pe and sem is e.sem:
                continue
            k = id(sem)
            if e.waited.get(k, 0) >= val:
                continue
            e.h.wait_ge(sem, val)
            e.waited[k] = val

    def _mark(self, tok, reads, writes):
        k = id(tok[0])
        for b in reads:
            if b.r.get(k, (None, 0))[1] < tok[1]:
                b.r[k] = tok
        for b in writes:
            b.w = tok
            b.r = {}

    def op(self, e, fn, reads=(), writes=()):
        self._sync(e, reads, writes)
        inst = fn(e.h)
        e.cnt += 1
        inst.then_inc(e.sem, 1)
        tok = (e.sem, e.cnt)
        self._mark(tok, reads, writes)
        return tok

    def dma(self, e, fn, chan, reads=(), writes=()):
        self._sync(e, reads, writes)
        inst = fn(e.h)
        chan.cnt += 16
        inst.then_inc(chan.sem, 16)
        tok = (chan.sem, chan.cnt)
        self._mark(tok, reads, writes)
        return tok

    def barrier(self, chans=()):
        for e in self.engs:
            for o in self.engs:
                if o is e or o.cnt == 0:
                    continue
                if e.waited.get(id(o.sem), 0) < o.cnt:
                    e.h.wait_ge(o.sem, o.cnt)
                    e.waited[id(o.sem)] = o.cnt
            for c in chans:
                if c.cnt and e.waited.get(id(c.sem), 0) < c.cnt:
                    e.h.wait_ge(c.sem, c.cnt)
                    e.waited[id(c.sem)] = c.cnt


def build_nc():
    nc = bass.Bass("TRN2", target_bir_lowering=False)

    def din(name, shape, dt=F32):
        return nc.dram_tensor(name, list(shape), dt, kind="ExternalInput").ap()

    x_own = din("x_own", [NTOK, D])
    x_prev = din("x_prev", [NTOK, D])
    smallp = din("smallp", [128, 512])
    iota_d = din("iota", [128, 512])
    cst_d = din("cst", [128, 4, 128])
    w_ada = din("w_ada", [D, 6 * D])
    b_ada = din("b_ada", [1, 6 * D])
    w_in = din("w_in", [D, INC])
    wb_d = din("wb", [2, 128, 16, 128])
    wc_d = din("wc", [2, 128, 16, 128])
    w_sg = din("w_s5_gate", [512, 512])
    w_su = din("w_s5_up", [512, D])
    w_co = din("w_conv_out", [1024, D])
    w_out = din("w_out", [D, D])
    lnbc_d = din("lnbc", [4, 128, D])
    w_rt = din("w_route", [D, 72])
    brt_d = din("b_route", [128, 72])
    w_eg = din("w_exp_gate", [NE, D, 512])
    w_eu = din("w_exp_up", [NE, D, 512])
    w_ed = din("w_exp_down", [NE, 512, D])
    out = nc.dram_tensor("out", [NTOK, D], F32, kind="ExternalOutput").ap()
    x1_d = nc.dram_tensor("x1s", [NTOK, D], F32, kind="ExternalOutput" if DEBUG else "Internal").ap()
    xdisp = nc.dram_tensor("xdisp", [NE * CAP, D], BF16, kind="Internal").ap()
    ydh = [nc.dram_tensor("ydisp%d" % i, [NE * CAP, 1024], F32, kind="Internal").ap() for i in range(2)]

    es = contextlib.ExitStack()
    S = Sched(nc, es)
    pe, dve, act, pool, sp = S.pe, S.dve, S.act, S.pool, S.sp

    dbg_ch = S.chan()

    def dump(name, ap, shape, dt, reads):
        if not DEBUG:
            return
        dd = nc.dram_tensor("dbg_" + name, list(shape), dt, kind="ExternalOutput").ap()
        S.dma(sp, lambda h: h.dma_start(out=dd, in_=ap), dbg_ch, reads=reads)

    def sb(stack, name, shape, dt):
        return stack.enter_context(nc.sbuf_tensor("s_" + name, list(shape), dt))

    PS = [es.enter_context(nc.psum_tensor("ps%d" % i, [128, 512], F32)) for i in range(6)]
    PSB = [Buf("ps%d" % i) for i in range(6)]
    PT = [es.enter_context(nc.psum_tensor("pt%d" % i, [128, 1024], BF16)) for i in range(2)]
    PTB = [Buf("pt%d" % i) for i in range(2)]
    pctr = [0]

    def pbank():
        i = pctr[0] % 6
        pctr[0] += 1
        return PS[i], PSB[i]

    tctr = [0]

    def tbank():
        i = tctr[0] % 2
        tctr[0] += 1
        return PT[i], PTB[i]

    sp_t = sb(es, "smallp", [128, 512], F32)
    iota = sb(es, "iota", [128, 512], F32)
    cst = sb(es, "cst", [128, 4, 128], F32)
    identb = sb(es, "identb", [128, 128], BF16)
    onesb = sb(es, "onesb", [128, 128], BF16)
    trib = sb(es, "trib", [128, 128], BF16)
    modpp = sb(es, "modpp", [128, 4, 16], F32)
    modbc = sb(es, "modbc", [128, 4, D], F32)
    rinfo = sb(es, "rinfo", [128, 16, 4], F32)
    rdest = sb(es, "rdest", [128, 16, 2], I32)
    basebc = sb(es, "basebc", [128, 64], F32)
    B_const = Buf("const")
    B_modpp = Buf("modpp")
    B_modbc = Buf("modbc")
    B_rinfo = Buf("rinfo")
    B_base = Buf("base")
    identf = cst[:, 0, :]
    onesf = cst[:, 1, :]

    C_C = 0
    C_BIN = 16
    C_ARE = 68
    C_AIM = 84
    C_LDT = 100
    C_SD = 116
    C_DWB = 120
    C_LNG = 128
    C_LNB = 136
    C_FLAG = 144
    C_DW = 160

    ch_par = S.chan()
    S.dma(sp, lambda h: h.dma_start(out=sp_t[:], in_=smallp), ch_par, writes=[B_const])
    S.dma(sp, lambda h: h.dma_start(out=iota[:], in_=iota_d), ch_par, writes=[B_const])
    S.dma(sp, lambda h: h.dma_start(out=cst[:], in_=cst_d), ch_par, writes=[B_const])
    B_const.w = (ch_par.sem, ch_par.cnt)
    S.op(dve, lambda h: h.tensor_copy(out=identb[:], in_=cst[:, 0, :]), reads=[B_const], writes=[B_const])
    S.op(dve, lambda h: h.tensor_copy(out=onesb[:], in_=cst[:, 1, :]), reads=[B_const], writes=[B_const])
    S.op(dve, lambda h: h.tensor_copy(out=trib[:], in_=cst[:, 2, :]), reads=[B_const], writes=[B_const])
    S.op(dve, lambda h: h.memset(basebc[:], 0.0), writes=[B_base])

    B_xd = Buf("xdisp")
    ch_xd = S.chan()
    bc_reg = nc.gpsimd.to_reg(NE * CAP - 1)

    NSLOT = 6
    wbf = [sb(es, "wbf%d" % i, [128, 4096], BF16) for i in range(NSLOT)]
    wbfB = [Buf("wbf%d" % i) for i in range(NSLOT)]
    wbfC = [S.chan() for _ in range(NSLOT)]
    wctr = [0]
    fst = {"t": [], "b": [], "c": [S.chan(), S.chan(), S.chan()], "n": 0, "k": 0}

    def alloc_fst(stack, n, width):
        fst["k"] += 1
        fst["t"] = [sb(stack, "fst%d_%d" % (fst["k"], i), [128, width], F32) for i in range(n)]
        fst["b"] = [Buf("fst%d" % i) for i in range(n)]
        fst["n"] = 0

    def cast(out_ap, in_ap, reads, writes, eng=None):
        if eng is act:
            S.op(act, lambda h: h.activation(out=out_ap, in_=in_ap, func=AF.Copy), reads=reads, writes=writes)
        else:
            S.op(eng, lambda h: h.tensor_copy(out=out_ap, in_=in_ap), reads=reads, writes=writes)

    def fload(pieces):
        i = fst["n"] % len(fst["t"])
        fst["n"] += 1
        off = 0
        for (ap, a, b) in pieces:
            v = fst["t"][i][:, off:off + a * b].rearrange("p (a b) -> p a b", a=a)
            S.dma(sp, lambda h, v=v, ap=ap: h.dma_start(out=v, in_=ap), fst["c"][i], writes=[fst["b"][i]])
            off += a * b
        return fst["t"][i], fst["b"][i]

    def wload(pieces, do_cast=True, dst=None, dstB=None):
        if not do_cast:
            return fload(pieces)
        i = wctr[0] % NSLOT
        wctr[0] += 1
        off = 0
        for (ap, a, b) in pieces:
            v = wbf[i][:, off:off + a * b].rearrange("p (a b) -> p a b", a=a)
            S.dma(pool, lambda h, v=v, ap=ap: h.dma_start(out=v, in_=ap), wbfC[i], writes=[wbfB[i]])
            off += a * b
        return wbf[i], wbfB[i]

    NCV = NE * 6
    wscr_l = [nc.dram_tensor("wscr%d" % j, [NCV // 2, 128, 4096], BF16, kind="Internal").ap() for j in range(2)]
    wscr = lambda u: wscr_l[u // (NCV // 2)][u % (NCV // 2)]
    cvB = [Buf("cv%d" % u) for u in range(NCV)]
    CVG = 32
    CV_MAX = 162
    cvC = [S.chan() for _ in range(NCV // CVG)]
    cv_next = [0]

    def cv_src(u):
        e, r = divmod(u, 6)
        if r < 4:
            wsrc = w_eg if r < 2 else w_eu
            hf = r % 2
            return kview(wsrc[e], 16)[:, :, hf * 256:(hf + 1) * 256], 16
        hf = r - 4
        return kview(w_ed[e], 4)[:, :, hf * 1024:(hf + 1) * 1024], 4

    def cv_issue(n):
        for _ in range(n):
            u = cv_next[0]
            if u >= NCV:
                return
            cv_next[0] += 1
            src, a = cv_src(u)
            dstv = wscr(u).rearrange("p (a b) -> p a b", a=a)
            S.dma(pool, lambda h, dstv=dstv, src=src: h.dma_start(out=dstv, in_=src), cvC[u // CVG], writes=[cvB[u]])

    def cv_finalize():
        for u in range(cv_next[0]):
            c = cvC[u // CVG]
            cvB[u].w = (c.sem, c.cnt)

    def eload(u):
        if u >= cv_next[0]:
            src, a = cv_src(u)
            return wload([(src, a, 4096 // a)])
        i = wctr[0] % NSLOT
        wctr[0] += 1
        S.dma(sp, lambda h: h.dma_start(out=wbf[i][:], in_=wscr(u)), wbfC[i], reads=[cvB[u]], writes=[wbfB[i]])
        return wbf[i], wbfB[i]

    def pipeline(units, depth=4):
        loaded = []
        n = len(units)
        for i in range(n + depth):
            if i < n:
                loaded.append(units[i][0]())
            j = i - depth
            if j >= 0:
                units[j][1](*loaded[j])
                loaded[j] = None

    def kview(ap2d, kt):
        return ap2d.rearrange("(kt p) n -> p kt n", p=128)

    w_in_v = kview(w_in, 16)
    w_ada_v = kview(w_ada, 16)

    mx_pieces = []
    for kp_ in range(8):
        mx_pieces.append([(w_in_v[:, :, 512 + kp_ * 128:512 + (kp_ + 1) * 128], 16, 128),
                          (w_in_v[:, :, 1536 + kp_ * 128:1536 + (kp_ + 1) * 128], 16, 128)])
    for k_ in range(16):
        mx_pieces.append([(w_in_v[:, :, 2560 + k_ * 128:2560 + (k_ + 1) * 128], 16, 128),
                          (w_in_v[:, :, 4608 + k_ * 128:4608 + (k_ + 1) * 128], 16, 128)])
    for k_ in range(16):
        mx_pieces.append([(kview(w_su, 4)[:, :, k_ * 128:(k_ + 1) * 128], 4, 128),
                          (kview(w_co, 8)[:, :, k_ * 128:(k_ + 1) * 128], 8, 128)])
    for fb_ in range(8):
        mx_pieces.append([(kview(w_out, 16)[:, :, fb_ * 256:(fb_ + 1) * 256], 16, 256)])
    MX_CONV, MX_GATE, MX_PROJ, MX_WO = 0, 8, 24, 40
    mscr = nc.dram_tensor("mscr", [48, 128, 4096], BF16, kind="Internal").ap()
    mxB = [Buf("mx%d" % j) for j in range(48)]
    mxC = S.chan()

    def mx_convert_all():
        for j in range(48):
            off = 0
            for (ap, a, b) in mx_pieces[j]:
                dstv = mscr[j][:, off:off + a * b].rearrange("p (a b) -> p a b", a=a)
                S.dma(pool, lambda h, dstv=dstv, ap=ap: h.dma_start(out=dstv, in_=ap), mxC, writes=[mxB[j]])
                off += a * b

    def mx_finalize():
        for j in range(48):
            mxB[j].w = (mxC.sem, mxC.cnt)

    def mload(j):
        i = wctr[0] % NSLOT
        wctr[0] += 1
        S.dma(sp, lambda h: h.dma_start(out=wbf[i][:], in_=mscr[j]), wbfC[i], reads=[mxB[j]], writes=[wbfB[i]])
        return wbf[i], wbfB[i]


    def mm(ps, lhsT, rhs, start, stop, reads, pbuf):
        S.op(pe, lambda h: h.matmul(ps, lhsT=lhsT, rhs=rhs, start=start, stop=stop), reads=reads, writes=[pbuf])

    with contextlib.ExitStack() as pa:
        zt = sb(pa, "zt", [128, D], BF16)
        B_zt = Buf("zt")
        S.op(pool, lambda h: h.memset(zt[:], 0.0), writes=[B_zt])
        for e in range(NE):
            S.dma(pool, lambda h, e=e: h.dma_start(out=xdisp[e * CAP:(e + 1) * CAP, :], in_=zt[:]), ch_xd,
                  reads=[B_zt], writes=[B_xd])
        alloc_fst(pa, 3, D)
        cact = sb(pa, "cact", [128, 16], F32)
        cb = sb(pa, "cb", [128, 16, 128], F32)
        R = sb(pa, "R", [128, D], F32)
        bada = sb(pa, "bada", [1, D], F32)
        tmp3 = sb(pa, "tmp3", [128, 16, 128], F32)
        B_c = Buf("cact")
        B_R = Buf("R")
        B_ba = Buf("bada")
        B_t3 = Buf("tmp3")
        ch_a = S.chan()
        S.op(act, lambda h: h.activation(out=cact[:], in_=sp_t[:, C_C:C_C + 16], func=AF.Silu),
             reads=[B_const], writes=[B_c])
        for kt in range(16):
            S.op(dve, lambda h, kt=kt: h.tensor_copy(out=cb[:, kt, :], in_=cact[:, kt:kt + 1].to_broadcast([128, 128])),
                 reads=[B_c], writes=[B_c])
        pp_dst = {0: (0, False), 1: (1, True), 3: (2, False), 4: (3, True)}
        bc_dst = {2: [(0, 1.0)], 5: [(1, 1.0)], 4: [(2, 1.0)], 3: [(3, 0.0)]}
        for grp in range(6):
            banks = [pbank() for _ in range(4)]
            S.dma(sp, lambda h, grp=grp: h.dma_start(out=bada[:], in_=b_ada[:, grp * D:(grp + 1) * D]), ch_a, writes=[B_ba])

            def ld(grp, kt):
                return wload([(w_ada_v[:, kt:kt + 1, grp * D:(grp + 1) * D], 1, D)], do_cast=False)

            def use(t, tb, kt, banks):
                for nb in range(4):
                    mm(banks[nb][0][:], cb[:, kt, :], t[:, nb * 512:(nb + 1) * 512], kt == 0, False,
                       [tb, B_c], banks[nb][1])

            units = [((lambda kt=kt, grp=grp: ld(grp, kt)), (lambda t, tb, kt=kt, banks=banks: use(t, tb, kt, banks)))
                     for kt in range(16)]
            pipeline(units, depth=2)
            for nb in range(4):
                c0 = nb * 512
                mm(banks[nb][0][:], cst[0:1, 1, :], bada[0:1, c0:c0 + 512], False, True, [B_ba, B_const], banks[nb][1])
                S.op(act, lambda h, nb=nb, c0=c0, banks=banks: h.activation(out=R[:, c0:c0 + 512], in_=banks[nb][0][:], func=AF.Copy),
                     reads=[banks[nb][1]], writes=[B_R])
            if grp in pp_dst:
                vi, plus1 = pp_dst[grp]
                S.op(dve, lambda h: h.tensor_tensor(
                    out=tmp3[:], in0=R[:].rearrange("p (a b) -> p a b", a=16),
                    in1=identf.unsqueeze(1).to_broadcast([128, 16, 128]), op=ALU.mult),
                    reads=[B_R, B_const], writes=[B_t3])
                S.op(dve, lambda h, vi=vi: h.reduce_sum(out=modpp[:, vi, :], in_=tmp3[:], axis=AX.X),
                     reads=[B_t3], writes=[B_modpp])
                if plus1:
                    S.op(dve, lambda h, vi=vi: h.tensor_scalar_add(out=modpp[:, vi, :], in0=modpp[:, vi, :], scalar1=1.0),
                         reads=[B_modpp], writes=[B_modpp])
            for (di, add) in bc_dst.get(grp, []):
                S.op(dve, lambda h, di=di, add=add: h.tensor_scalar_add(out=modbc[:, di, :], in0=R[:], scalar1=add),
                     reads=[B_R], writes=[B_modbc])
        dump("modpp", modpp[:], [128, 4, 16], F32, [B_modpp])
        dump("modbc", modbc[:], [128, 4, D], F32, [B_modbc])
        S.barrier([ch_a, ch_xd, dbg_ch])

    s5p = sb(es, "s5p", [128, 12, 16], F32)
    B_s5p = Buf("s5p")
    hT_halo = sb(es, "hT_halo", [128, 16, 32], BF16)
    B_halo = Buf("halo")
    s5o = sb(es, "s5o", [128, 4, NTOK], BF16)
    B_s5o = Buf("s5o")
    lnst = sb(es, "lnst", [128, 4, 6], F32)
    lnmv = sb(es, "lnmv", [128, 4], F32)
    B_ln = Buf("lnst")
    pus = contextlib.ExitStack()
    wbb = sb(pus, "wbb", [128, 2, 16, 128], BF16)
    wcb = sb(pus, "wcb", [128, 2, 16, 128], BF16)
    B_wb = Buf("wbb")
    uT = sb(pus, "uT", [128, 4, 2 * NTOK], BF16)
    B_uT = [Buf("uT%d" % g) for g in range(2 * NBLK)]

    def sincos(eng, ang_ap, cos_out, sin_out, rb, wbuf, t, a, Bt):
        for (shift, outp) in ((0.0, sin_out), (np.pi / 2, cos_out)):
            S.op(eng, lambda h, shift=shift: h.tensor_scalar(out=a[:], in0=ang_ap, scalar1=float(shift), scalar2=None, op0=ALU.add),
                 reads=rb, writes=[Bt])
            S.op(eng, lambda h: h.tensor_scalar(out=t[:], in0=a[:], scalar1=1.0 / TWO_PI, scalar2=MAGIC, op0=ALU.mult, op1=ALU.add),
                 reads=[Bt], writes=[Bt])
            S.op(eng, lambda h: h.tensor_scalar(out=t[:], in0=t[:], scalar1=-MAGIC, scalar2=None, op0=ALU.add),
                 reads=[Bt], writes=[Bt])
            S.op(eng, lambda h: h.scalar_tensor_tensor(out=a[:], in0=t[:], scalar=-TWO_PI, in1=a[:], op0=ALU.mult, op1=ALU.add),
                 reads=[Bt], writes=[Bt])
            S.op(eng, lambda h: h.tensor_scalar(out=a[:], in0=a[:], scalar1=3.1415925, scalar2=-3.1415925, op0=ALU.min, op1=ALU.max),
                 reads=[Bt], writes=[Bt])
            S.op(act, lambda h, outp=outp: h.activation(out=outp, in_=a[:], func=AF.Sin), reads=[Bt], writes=wbuf)

    with contextlib.ExitStack() as pp:
        alloc_fst(pp, 2, 512)
        are = sp_t[:, C_ARE:C_ARE + 16]
        aim = sp_t[:, C_AIM:C_AIM + 16]
        sc = lambda i: s5p[:, i, :]
        S.op(act, lambda h: h.activation(out=sc(6), in_=sp_t[:, C_LDT:C_LDT + 16], func=AF.Exp), reads=[B_const], writes=[B_s5p])
        S.op(dve, lambda h: h.tensor_mul(out=sc(7), in0=are, in1=sc(6)), reads=[B_s5p, B_const], writes=[B_s5p])
        S.op(dve, lambda h: h.tensor_mul(out=sc(5), in0=aim, in1=sc(6)), reads=[B_s5p, B_const], writes=[B_s5p])
        S.op(act, lambda h: h.activation(out=sc(0), in_=sc(7), func=AF.Exp), reads=[B_s5p], writes=[B_s5p])
        sct = sb(pp, "sct", [128, 16], F32)
        sca = sb(pp, "sca", [128, 16], F32)
        sincos(dve, sc(5), sc(1), sc(2), [B_s5p], [B_s5p], sct, sca, Buf("scp"))
        S.op(dve, lambda h: h.tensor_mul(out=sc(8), in0=sc(0), in1=sc(1)), reads=[B_s5p], writes=[B_s5p])
        S.op(dve, lambda h: h.tensor_scalar_add(out=sc(8), in0=sc(8), scalar1=-1.0), reads=[B_s5p], writes=[B_s5p])
        S.op(dve, lambda h: h.tensor_mul(out=sc(9), in0=sc(0), in1=sc(2)), reads=[B_s5p], writes=[B_s5p])
        S.op(dve, lambda h: h.tensor_mul(out=sc(10), in0=are, in1=are), reads=[B_s5p, B_const], writes=[B_s5p])
        S.op(dve, lambda h: h.tensor_mul(out=sc(11), in0=aim, in1=aim), reads=[B_s5p, B_const], writes=[B_s5p])
        S.op(dve, lambda h: h.tensor_add(out=sc(10), in0=sc(10), in1=sc(11)), reads=[B_s5p], writes=[B_s5p])
        S.op(dve, lambda h: h.reciprocal(out=sc(10), in_=sc(10)), reads=[B_s5p], writes=[B_s5p])
        S.op(dve, lambda h: h.tensor_mul(out=sc(3), in0=sc(8), in1=are), reads=[B_s5p, B_const], writes=[B_s5p])
        S.op(dve, lambda h: h.tensor_mul(out=sc(11), in0=sc(9), in1=aim), reads=[B_s5p, B_const], writes=[B_s5p])
        S.op(dve, lambda h: h.tensor_add(out=sc(3), in0=sc(3), in1=sc(11)), reads=[B_s5p], writes=[B_s5p])
        S.op(dve, lambda h: h.tensor_mul(out=sc(3), in0=sc(3), in1=sc(10)), reads=[B_s5p], writes=[B_s5p])
        S.op(dve, lambda h: h.tensor_mul(out=sc(4), in0=sc(9), in1=are), reads=[B_s5p, B_const], writes=[B_s5p])
        S.op(dve, lambda h: h.tensor_mul(out=sc(11), in0=sc(8), in1=aim), reads=[B_s5p, B_const], writes=[B_s5p])
        S.op(dve, lambda h: h.tensor_sub(out=sc(4), in0=sc(4), in1=sc(11)), reads=[B_s5p], writes=[B_s5p])
        S.op(dve, lambda h: h.tensor_mul(out=sc(4), in0=sc(4), in1=sc(10)), reads=[B_s5p], writes=[B_s5p])
        dump("s5p", s5p[:], [128, 12, 16], F32, [B_s5p])
        for pl in range(2):
            for hf in range(4):
                t, tb = wload([(wb_d[pl, :, hf * 4:(hf + 1) * 4, :], 4, 128)], do_cast=False)
                S.op(dve, lambda h, t=t, pl=pl, hf=hf: h.tensor_copy(
                    out=wbb[:, pl, hf * 4:(hf + 1) * 4, :], in_=t[:, 0:512].rearrange("p (a b) -> p a b", a=4)),
                    reads=[tb], writes=[B_wb])
                t, tb = wload([(wc_d[pl, :, hf * 4:(hf + 1) * 4, :], 4, 128)], do_cast=False)
                S.op(dve, lambda h, t=t, pl=pl, hf=hf: h.tensor_scalar(
                    out=wcb[:, pl, hf * 4:(hf + 1) * 4, :], in0=t[:, 0:512].rearrange("p (a b) -> p a b", a=4),
                    scalar1=(1.0 if pl == 0 else -1.0), scalar2=None, op0=ALU.mult),
                    reads=[tb], writes=[B_wb])
        S.barrier()


    def ln_stats(x_ap, xb):
        for c in range(4):
            S.op(dve, lambda h, c=c: h.bn_stats(out=lnst[:, c, :], in_=x_ap[:, c * 512:(c + 1) * 512]),
                 reads=[xb], writes=[B_ln])
        S.op(dve, lambda h: h.bn_aggr(out=lnmv[:, 0:2], in_=lnst[:].rearrange("p a b -> p (a b)")), reads=[B_ln], writes=[B_ln])
        S.op(dve, lambda h: h.tensor_scalar_add(out=lnmv[:, 3:4], in0=lnmv[:, 1:2], scalar1=EPS), reads=[B_ln], writes=[B_ln])
        S.op(act, lambda h: h.activation(out=lnmv[:, 3:4], in_=lnmv[:, 3:4], func=AF.Sqrt), reads=[B_ln], writes=[B_ln])
        S.op(dve, lambda h: h.reciprocal(out=lnmv[:, 2:3], in_=lnmv[:, 3:4]), reads=[B_ln], writes=[B_ln])

    xts = {"t": [], "b": [], "c": [S.chan(), S.chan()], "n": 0, "k": 0}

    def alloc_xt(stack, n):
        xts["k"] += 1
        xts["t"] = [sb(stack, "xt%d_%d" % (xts["k"], i), [128, D], F32) for i in range(n)]
        xts["b"] = [Buf("xt%d" % i) for i in range(n)]
        xts["n"] = 0

    def load_x(src_ap):
        i = xts["n"] % len(xts["t"])
        xts["n"] += 1
        tt_, bb_ = xts["t"][i], xts["b"][i]
        S.dma(sp, lambda h: h.dma_start(out=tt_[:], in_=src_ap), xts["c"][i], writes=[bb_])
        return tt_, bb_

    with contextlib.ExitStack() as pu:
        alloc_xt(pu, 2)
        xn = sb(pu, "xn", [128, 4, D], BF16)
        B_xn = Buf("xn")
        hTp = [sb(pu, "hTp%d" % i, [128, 16, TB], BF16) for i in range(1)]
        B_hTp = [Buf("hTp%d" % i) for i in range(1)]
        def make_hT(src, blk, xn_t, xn_b, hdst, hbuf):
            for tt in range(4):
                t, tb = load_x(src[blk * TB + tt * 128: blk * TB + (tt + 1) * 128, :])
                ln_stats(t, tb)
                S.op(dve, lambda h, t=t, tt=tt: h.tensor_scalar(out=xn_t[:, tt, :], in0=t[:], scalar1=lnmv[:, 0:1], scalar2=lnmv[:, 2:3],
                                                              op0=ALU.subtract, op1=ALU.mult),
                     reads=[tb, B_ln], writes=[xn_b])
            for kt in range(16):
                ptile, ptb = tbank()
                for tt in range(4):
                    S.op(pe, lambda h, kt=kt, tt=tt, ptile=ptile: h.transpose(
                        out=ptile[:, tt * 128:(tt + 1) * 128], in_=xn_t[:, tt, kt * 128:(kt + 1) * 128], identity=identb[:]),
                        reads=[xn_b, B_const], writes=[ptb])
                S.op(act, lambda h, kt=kt, ptile=ptile: h.activation(
                    out=hdst[:, kt, :], in_=ptile[:, 0:512], func=AF.Identity,
                    scale=modpp[:, 1, kt:kt + 1], bias=modpp[:, 0, kt:kt + 1]),
                    reads=[ptb, B_modpp], writes=[hbuf])

        for g in range(2 * NBLK):
            own = g >= NBLK
            blk = g - NBLK if own else g
            src = x_own if own else x_prev
            make_hT(src, blk, xn, B_xn, hTp[0], B_hTp[0])
            def u_use(t, tb, hf, g=g):
                w = t[:, 0:4096].rearrange("p (a b) -> p a b", a=16)
                for m2 in range(2):
                    m = hf * 2 + m2
                    ps, pb = pbank()
                    for kt in range(16):
                        mm(ps[:], w[:, kt, m2 * 128:(m2 + 1) * 128], hTp[0][:, kt, :], kt == 0, kt == 15, [tb, B_hTp[0]], pb)
                    S.op(act, lambda h, m=m, ps=ps, g=g: h.activation(
                        out=uT[:, m, g * TB:(g + 1) * TB], in_=ps[:], func=AF.Identity, bias=sp_t[:, C_BIN + m:C_BIN + m + 1]),
                        reads=[pb, B_const], writes=[B_uT[g]])

            pipeline([((lambda hf=hf: wload([(w_in_v[:, :, hf * 256:(hf + 1) * 256], 16, 256)])),
                       (lambda t, tb, hf=hf: u_use(t, tb, hf))) for hf in range(2)])
            if g == NBLK - 1:
                S.op(dve, lambda h: h.tensor_copy(out=hT_halo[:], in_=hTp[0][:, :, TB - 32:TB]),
                     reads=[B_hTp[0]], writes=[B_halo])
        S.barrier()

    with contextlib.ExitStack() as ps5:
        cs = sb(ps5, "cs", [128, TB], F32)
        sn = sb(ps5, "sn", [128, TB], F32)
        mqr = sb(ps5, "mqr", [128, TB], F32)
        mqi = sb(ps5, "mqi", [128, TB], F32)
        dec = sb(ps5, "dec", [128, TB], F32)
        B_tab = Buf("tab")
        bre = sb(ps5, "bre", [128, TB], F32)
        bim = sb(ps5, "bim", [128, TB], F32)
        B_b = Buf("b")
        t1 = sb(ps5, "t1", [128, TB], F32)
        t2 = sb(ps5, "t2", [128, TB], F32)
        t3 = sb(ps5, "t3", [128, TB], F32)
        t4 = sb(ps5, "t4", [128, TB], F32)
        B_t12 = Buf("t12")
        B_t34 = Buf("t34")
        ang = t1
        sct2, sca2, B_sc2 = t3, t4, B_t34
        mre = sb(ps5, "mre", [128, TB], F32)
        mim = sb(ps5, "mim", [128, TB], F32)
        B_mre = Buf("mre")
        B_mim = Buf("mim")
        sre = sb(ps5, "sre", [128, TB], F32)
        sim = sb(ps5, "sim", [128, TB], F32)
        B_sre = Buf("sre")
        B_sim = Buf("sim")
        srb = sb(ps5, "srb", [128, TB], BF16)
        sib = sb(ps5, "sib", [128, TB], BF16)
        B_srb = Buf("srb")
        B_sib = Buf("sib")
        st = sb(ps5, "st", [128, 8], F32)
        B_st = Buf("st")
        gT = sb(ps5, "gT", [128, 4, NTOK], BF16)
        B_gT = Buf("gT")
        yp, y2, B_yp = bre, bim, B_b
        sreX = sb(ps5, "sreX", [128, TB], F32)
        simX = sb(ps5, "simX", [128, TB], F32)
        bre2, bim2, B_b2 = [bre, bre], [bim, bim], [B_b, B_b]
        sre2, sim2 = [sre, sreX], [sim, simX]
        B_sre2, B_sim2 = [B_sre, Buf("sreX")], [B_sim, Buf("simX")]
        mx_convert_all()
        ybanks = None
        for i in range(16):
            q = i % 4
            ut = i // 4
            if q == 0:
                ybanks = [(PS[b_], PSB[b_]) for b_ in range(NBLK)]
            th = s5p[:, 5, i:i + 1]
            S.op(dve, lambda h, th=th: h.tensor_scalar(out=ang[:], in0=iota[:], scalar1=th, scalar2=None, op0=ALU.mult),
                 reads=[B_const, B_s5p], writes=[B_t12])
            sincos(dve, ang[:], cs[:], sn[:], [B_t12], [B_tab], sct2, sca2, B_sc2)
            qre = s5p[:, 3, i:i + 1]
            qim = s5p[:, 4, i:i + 1]
            S.op(dve, lambda h, qre=qre: h.tensor_scalar(out=mqr[:], in0=cs[:], scalar1=qre, scalar2=None, op0=ALU.mult),
                 reads=[B_tab, B_s5p], writes=[B_tab])
            S.op(dve, lambda h, qim=qim: h.scalar_tensor_tensor(out=mqr[:], in0=sn[:], scalar=qim, in1=mqr[:], op0=ALU.mult, op1=ALU.add),
                 reads=[B_tab, B_s5p], writes=[B_tab])
            S.op(dve, lambda h, qim=qim: h.tensor_scalar(out=mqi[:], in0=cs[:], scalar1=qim, scalar2=None, op0=ALU.mult),
                 reads=[B_tab, B_s5p], writes=[B_tab])
            S.op(dve, lambda h, qre=qre: h.tensor_scalar(out=t1[:], in0=sn[:], scalar1=qre, scalar2=None, op0=ALU.mult),
                 reads=[B_tab, B_s5p], writes=[B_t12])
            S.op(dve, lambda h: h.tensor_sub(out=mqi[:], in0=mqi[:], in1=t1[:]), reads=[B_tab, B_t12], writes=[B_tab])
            S.op(dve, lambda h, i=i: h.tensor_copy(out=dec[:], in_=s5p[:, 0, i:i + 1].to_broadcast([128, TB])),
                 reads=[B_s5p], writes=[B_tab])
            S.op(dve, lambda h: h.memset(st[:], 0.0), writes=[B_st])
            cth = s5p[:, 1, i:i + 1]
            sth = s5p[:, 2, i:i + 1]
            LL = TB - 1
            S.op(dve, lambda h, sth=sth: h.tensor_scalar(out=st[:, 4:5], in0=sn[:, LL:LL + 1], scalar1=sth, scalar2=None, op0=ALU.mult),
                 reads=[B_tab, B_s5p, B_st], writes=[B_st])
            S.op(dve, lambda h, cth=cth: h.scalar_tensor_tensor(out=st[:, 6:7], in0=cs[:, LL:LL + 1], scalar=cth, in1=st[:, 4:5],
                                                                op0=ALU.mult, op1=ALU.subtract), reads=[B_tab, B_s5p, B_st], writes=[B_st])
            S.op(dve, lambda h, cth=cth: h.tensor_scalar(out=st[:, 5:6], in0=sn[:, LL:LL + 1], scalar1=cth, scalar2=None, op0=ALU.mult),
                 reads=[B_tab, B_s5p, B_st], writes=[B_st])
            S.op(dve, lambda h, sth=sth: h.scalar_tensor_tensor(out=st[:, 7:8], in0=cs[:, LL:LL + 1], scalar=sth, in1=st[:, 5:6],
                                                                op0=ALU.mult, op1=ALU.add), reads=[B_tab, B_s5p, B_st], writes=[B_st])
            for g in range(2 * NBLK):
                own = g >= NBLK
                blk = g - NBLK
                if cv_next[0] < CV_MAX:
                    cv_issue(2)
                pr, prb = PS[4], PSB[4]
                pi, pib = PS[5], PSB[5]
                par = g % 2
                bre_, bim_, B_b_ = bre2[par], bim2[par], B_b2[par]
                sre_, sim_, B_sre_, B_sim_ = sre2[par], sim2[par], B_sre2[par], B_sim2[par]
                mm(pr[:], wbb[:, 0, i, :], uT[:, ut, g * TB:(g + 1) * TB], True, True, [B_wb, B_uT[g]], prb)
                mm(pi[:], wbb[:, 1, i, :], uT[:, ut, g * TB:(g + 1) * TB], True, True, [B_wb, B_uT[g]], pib)
                S.op(dve, lambda h, pr=pr: h.tensor_mul(out=t1[:], in0=pr[:], in1=mqr[:]), reads=[prb, B_tab], writes=[B_t12])
                S.op(dve, lambda h, pi=pi: h.tensor_mul(out=t2[:], in0=pi[:], in1=mqi[:]), reads=[pib, B_tab], writes=[B_t12])
                S.op(dve, lambda h: h.tensor_sub(out=mre[:], in0=t1[:], in1=t2[:]), reads=[B_t12], writes=[B_mre])
                S.op(dve, lambda h, pr=pr: h.tensor_mul(out=t1[:], in0=pr[:], in1=mqi[:]), reads=[prb, B_tab], writes=[B_t12])
                S.op(dve, lambda h, pi=pi: h.tensor_mul(out=t2[:], in0=pi[:], in1=mqr[:]), reads=[pib, B_tab], writes=[B_t12])
                S.op(dve, lambda h: h.tensor_add(out=mim[:], in0=t1[:], in1=t2[:]), reads=[B_t12], writes=[B_mim])
                S.op(dve, lambda h, sre_=sre_: h.tensor_tensor_scan(out=sre_[:], data0=dec[:], data1=mre[:], initial=st[:, 2:3],
                                                                    op0=ALU.mult, op1=ALU.add), reads=[B_tab, B_mre, B_st], writes=[B_sre_])
                S.op(dve, lambda h, sim_=sim_: h.tensor_tensor_scan(out=sim_[:], data0=dec[:], data1=mim[:], initial=st[:, 3:4],
                                                                    op0=ALU.mult, op1=ALU.add), reads=[B_tab, B_mim, B_st], writes=[B_sim_])
                L = TB - 1
                S.op(dve, lambda h, sim_=sim_: h.tensor_scalar(out=st[:, 4:5], in0=sim_[:, L:L + 1], scalar1=st[:, 7:8], scalar2=None, op0=ALU.mult),
                     reads=[B_sim_, B_st], writes=[B_st])
                S.op(dve, lambda h, sre_=sre_: h.scalar_tensor_tensor(out=st[:, 2:3], in0=sre_[:, L:L + 1], scalar=st[:, 6:7], in1=st[:, 4:5],
                                                                      op0=ALU.mult, op1=ALU.subtract), reads=[B_sre_, B_st], writes=[B_st])
                S.op(dve, lambda h, sim_=sim_: h.tensor_scalar(out=st[:, 5:6], in0=sim_[:, L:L + 1], scalar1=st[:, 6:7], scalar2=None, op0=ALU.mult),
                     reads=[B_sim_, B_st], writes=[B_st])
                S.op(dve, lambda h, sre_=sre_: h.scalar_tensor_tensor(out=st[:, 3:4], in0=sre_[:, L:L + 1], scalar=st[:, 7:8], in1=st[:, 5:6],
                                                                      op0=ALU.mult, op1=ALU.add), reads=[B_sre_, B_st], writes=[B_st])
                if g == NBLK - 1:
                    S.op(dve, lambda h: h.tensor_scalar(out=st[:, 2:4], in0=st[:, 2:4], scalar1=sp_t[:, C_FLAG:C_FLAG + 1],
                                                        scalar2=None, op0=ALU.mult), reads=[B_st, B_const], writes=[B_st])
                if not own:
                    continue
                S.op(pool, lambda h, sre_=sre_: h.tensor_mul(out=t3[:], in0=sre_[:], in1=cs[:]), reads=[B_sre_, B_tab], writes=[B_t34])
                S.op(pool, lambda h, sim_=sim_: h.tensor_mul(out=t4[:], in0=sim_[:], in1=sn[:]), reads=[B_sim_, B_tab], writes=[B_t34])
                S.op(pool, lambda h: h.tensor_sub(out=srb[:], in0=t3[:], in1=t4[:]), reads=[B_t34], writes=[B_srb])
                S.op(pool, lambda h, sre_=sre_: h.tensor_mul(out=t3[:], in0=sre_[:], in1=sn[:]), reads=[B_sre_, B_tab], writes=[B_t34])
                S.op(pool, lambda h, sim_=sim_: h.tensor_mul(out=t4[:], in0=sim_[:], in1=cs[:]), reads=[B_sim_, B_tab], writes=[B_t34])
                S.op(pool, lambda h: h.tensor_add(out=sib[:], in0=t3[:], in1=t4[:]), reads=[B_t34], writes=[B_sib])
                yb, ybb = ybanks[blk]
                mm(yb[:], wcb[:, 0, i, :], srb[:], q == 0, False, [B_wb, B_srb], ybb)
                mm(yb[:], wcb[:, 1, i, :], sib[:], False, q == 3, [B_wb, B_sib], ybb)
            if q == 3:
                for blk in range(NBLK):
                    yb, ybb = ybanks[blk]
                    g = NBLK + blk
                    S.op(dve, lambda h, yb=yb, g=g: h.scalar_tensor_tensor(
                        out=yp[:], in0=uT[:, ut, g * TB:(g + 1) * TB], scalar=sp_t[:, C_SD + ut:C_SD + ut + 1], in1=yb[:],
                        op0=ALU.mult, op1=ALU.add), reads=[B_uT[g], ybb, B_const], writes=[B_yp])
                    S.op(dve, lambda h: h.tensor_mul(out=y2[:], in0=yp[:], in1=yp[:]), reads=[B_yp], writes=[B_yp])
                    S.op(dve, lambda h: h.tensor_scalar(out=y2[:], in0=y2[:], scalar1=0.044715, scalar2=1.0, op0=ALU.mult, op1=ALU.add),
                         reads=[B_yp], writes=[B_yp])
                    S.op(dve, lambda h: h.tensor_mul(out=y2[:], in0=y2[:], in1=yp[:]), reads=[B_yp], writes=[B_yp])
                    S.op(act, lambda h: h.activation(out=y2[:], in_=y2[:], func=AF.Sigmoid, scale=1.5957691216057308),
                         reads=[B_yp], writes=[B_yp])
                    S.op(dve, lambda h, blk=blk: h.tensor_mul(out=gT[:, ut, blk * TB:(blk + 1) * TB], in0=y2[:], in1=yp[:]),
                         reads=[B_yp], writes=[B_gT])
        wsg_t, B_wsg = wload([(kview(w_sg, 4), 4, 512)])
        wsg = wsg_t[:, 0:2048].rearrange("p (a b) -> p a b", a=4)
        for m in range(4):
            for blk in range(NBLK):
                ps_, pb = pbank()
                for kt in range(4):
                    mm(ps_[:], wsg[:, kt, m * 128:(m + 1) * 128], gT[:, kt, blk * TB:(blk + 1) * TB], kt == 0, kt == 3,
                       [B_wsg, B_gT], pb)
                S.op(act, lambda h, ps_=ps_: h.activation(out=yp[:], in_=ps_[:], func=AF.Sigmoid), reads=[pb], writes=[B_yp])
                S.op(dve, lambda h, m=m, blk=blk: h.tensor_mul(out=s5o[:, m, blk * TB:(blk + 1) * TB],
                                                              in0=gT[:, m, blk * TB:(blk + 1) * TB], in1=yp[:]),
                     reads=[B_yp, B_gT], writes=[B_s5o])
        dump("uT", uT[:], [128, 4, 2 * NTOK], BF16, B_uT)
        dump("gT", gT[:], [128, 4, NTOK], BF16, [B_gT])
        dump("s5o", s5o[:], [128, 4, NTOK], BF16, [B_s5o])
        S.barrier([dbg_ch])
    pus.close()

    mx_finalize()
    ch_x1 = S.chan()
    ch_sc = S.chan()
    B_x1d = Buf("x1d")
    with contextlib.ExitStack() as pm:
        alloc_xt(pm, 1)
        big = sb(pm, "big", [128, 4 * D], F32)
        cv = big[:, 0:8 * TB].rearrange("p (a b) -> p a b", a=8)
        sq = big[:, 8 * TB:16 * TB].rearrange("p (a b) -> p a b", a=8)
        res = big[:].rearrange("p (a b) -> p a b", a=4)
        B_cv = Buf("cv")
        B_sq = Buf("sq")
        B_res = [Buf("res%d" % i) for i in range(4)]
        b16a = sb(pm, "b16a", [128, 4 * D], BF16)
        xn2 = b16a[:].rearrange("p (a b) -> p a b", a=4)
        mgT = b16a[:].rearrange("p (a b) -> p a b", a=16)
        B_xn2 = Buf("xn2")
        B_mg = Buf("mgT")
        vtail = sb(pm, "vtail", [128, 8, 32], BF16)
        B_vt = Buf("vtail")
        flag = sp_t[:, C_FLAG:C_FLAG + 1]

        for blk in range(NBLK):
          with contextlib.ExitStack() as sa:
            hTb = sb(sa, "hTb%d" % blk, [128, 16, TB], BF16)
            hb = Buf("hTb")
            coT = sb(sa, "coT%d" % blk, [128, 8, TB], BF16)
            B_co = Buf("coT")
            hsl = lambda kt, hTb=hTb: hTb[:, kt, :]
            make_hT(x_own, blk, xn2, B_xn2, hTb, hb)
            S.barrier()
            sa12 = contextlib.ExitStack()
            vT = sb(sa12, "vT%d" % blk, [128, 8, 32 + TB], BF16)
            B_vT = Buf("vT")
            asb = sb(sa12, "asb%d" % blk, [128, TB], F32)
            gsb = sb(sa12, "gsb%d" % blk, [128, TB], F32)
            B_ag = Buf("ag")
            diag = sb(sa12, "diag%d" % blk, [128, 31, 128], BF16)
            B_dg = Buf("diag")
            mean, rstd, B_mr = asb, gsb, B_ag
            ctmp = xts["t"][0][:, 0:TB]
            B_ct = xts["b"][0]
            if blk > 0:
                S.op(dve, lambda h: h.tensor_copy(out=vT[:, :, 0:32], in_=vtail[:]), reads=[B_vt], writes=[B_vT])
            def conv_ld(kp):
                return mload(MX_CONV + kp)

            def conv_use(t, tb, kp, blk=blk, hb=hb, hsl=hsl):
                wa = t[:, 0:2048].rearrange("p (a b) -> p a b", a=16)
                wg = t[:, 2048:4096].rearrange("p (a b) -> p a b", a=16)
                pa_, pab = pbank()
                pg_, pgb = pbank()
                for kt in range(16):
                    mm(pa_[:], wa[:, kt, :], hsl(kt), kt == 0, kt == 15, [tb, hb], pab)
                for kt in range(16):
                    mm(pg_[:], wg[:, kt, :], hsl(kt), kt == 0, kt == 15, [tb, hb], pgb)
                ba = sp_t[:, C_BIN + 4 + kp:C_BIN + 5 + kp]
                bg = sp_t[:, C_BIN + 12 + kp:C_BIN + 13 + kp]
                S.op(act, lambda h: h.activation(out=asb[:], in_=pa_[:], func=AF.Identity, bias=ba), reads=[pab, B_const], writes=[B_ag])
                S.op(act, lambda h: h.activation(out=gsb[:], in_=pg_[:], func=AF.Sigmoid, bias=bg), reads=[pgb, B_const], writes=[B_ag])
                S.op(dve, lambda h: h.tensor_mul(out=vT[:, kp, 32:32 + TB], in0=asb[:], in1=gsb[:]), reads=[B_ag], writes=[B_vT])
                if blk == 0:
                    ph_, phb = pbank()
                    for kt in range(16):
                        mm(ph_[:, 0:32], wa[:, kt, :], hT_halo[:, kt, :], kt == 0, kt == 15, [tb, B_halo], phb)
                    for kt in range(16):
                        mm(ph_[:, 32:64], wg[:, kt, :], hT_halo[:, kt, :], kt == 0, kt == 15, [tb, B_halo], phb)
                    S.op(act, lambda h: h.activation(out=asb[:, 0:32], in_=ph_[:, 0:32], func=AF.Identity, bias=ba),
                         reads=[phb, B_const], writes=[B_ag])
                    S.op(act, lambda h: h.activation(out=gsb[:, 0:32], in_=ph_[:, 32:64], func=AF.Sigmoid, bias=bg),
                         reads=[phb, B_const], writes=[B_ag])
                    S.op(dve, lambda h: h.scalar_tensor_tensor(out=vT[:, kp, 0:32], in0=asb[:, 0:32], scalar=flag, in1=gsb[:, 0:32],
                                                               op0=ALU.mult, op1=ALU.mult), reads=[B_ag, B_const], writes=[B_vT])

            pipeline([((lambda kp=kp: conv_ld(kp)), (lambda t, tb, kp=kp: conv_use(t, tb, kp))) for kp in range(8)])

            for kp in range(8):
                S.op(dve, lambda h, kp=kp: h.tensor_tensor(
                    out=diag[:], in0=identb[:].unsqueeze(1).to_broadcast([128, 31, 128]),
                    in1=sp_t[:, C_DW + kp * 31:C_DW + (kp + 1) * 31].unsqueeze(2).to_broadcast([128, 31, 128]), op=ALU.mult),
                    reads=[B_const], writes=[B_dg])
                pc_, pcb = pbank()
                for tap in range(31):
                    mm(pc_[:], diag[:, tap, :], vT[:, kp, 2 + tap:2 + tap + TB], tap == 0, tap == 30, [B_dg, B_vT], pcb)
                bb = sp_t[:, C_DWB + kp:C_DWB + kp + 1]
                S.op(act, lambda h, kp=kp, pc_=pc_, bb=bb: h.activation(out=cv[:, kp, :], in_=pc_[:], func=AF.Identity, bias=bb),
                     reads=[pcb, B_const], writes=[B_cv])
                S.op(act, lambda h, kp=kp, pc_=pc_, bb=bb: h.activation(out=sq[:, kp, :], in_=pc_[:], func=AF.Square, bias=bb),
                     reads=[pcb, B_const], writes=[B_sq])
            S.op(dve, lambda h: h.tensor_copy(out=vtail[:], in_=vT[:, :, TB:TB + 32]), reads=[B_vT], writes=[B_vt])
            pm_, pmb = pbank()
            pq_, pqb = pbank()
            for kp in range(8):
                mm(pm_[:], onesf, cv[:, kp, :], kp == 0, kp == 7, [B_const, B_cv], pmb)
            for kp in range(8):
                mm(pq_[:], onesf, sq[:, kp, :], kp == 0, kp == 7, [B_const, B_sq], pqb)
            S.op(dve, lambda h: h.tensor_scalar(out=mean[:], in0=pm_[:], scalar1=1.0 / 1024, scalar2=None, op0=ALU.mult),
                 reads=[pmb], writes=[B_mr])
            S.op(dve, lambda h: h.tensor_mul(out=ctmp[:], in0=mean[:], in1=mean[:]), reads=[B_mr], writes=[B_ct])
            S.op(dve, lambda h: h.scalar_tensor_tensor(out=rstd[:], in0=pq_[:], scalar=1.0 / 1024, in1=ctmp[:], op0=ALU.mult, op1=ALU.subtract),
                 reads=[pqb, B_ct], writes=[B_mr])
            S.op(dve, lambda h: h.tensor_scalar_add(out=rstd[:], in0=rstd[:], scalar1=EPS), reads=[B_mr], writes=[B_mr])
            S.op(act, lambda h: h.activation(out=rstd[:], in_=rstd[:], func=AF.Sqrt), reads=[B_mr], writes=[B_mr])
            S.op(dve, lambda h: h.reciprocal(out=rstd[:], in_=rstd[:]), reads=[B_mr], writes=[B_mr])
            for kp in range(8):
                S.op(dve, lambda h, kp=kp: h.tensor_sub(out=ctmp[:], in0=cv[:, kp, :], in1=mean[:]), reads=[B_cv, B_mr], writes=[B_ct])
                S.op(dve, lambda h: h.tensor_mul(out=ctmp[:], in0=ctmp[:], in1=rstd[:]), reads=[B_ct, B_mr], writes=[B_ct])
                S.op(act, lambda h, kp=kp: h.activation(out=coT[:, kp, :], in_=ctmp[:], func=AF.Silu,
                                                        scale=sp_t[:, C_LNG + kp:C_LNG + kp + 1], bias=sp_t[:, C_LNB + kp:C_LNB + kp + 1]),
                     reads=[B_ct, B_const], writes=[B_co])

            S.barrier()
            sa12.close()
            sa3 = contextlib.ExitStack()
            sg1 = sb(sa3, "sg1%d" % blk, [128, TB], F32)
            sg2 = sb(sa3, "sg2%d" % blk, [128, TB], F32)
            B_sg = Buf("sg")
            def gate_ld(k):
                return mload(MX_GATE + k)

            def proj_ld(k):
                return mload(MX_PROJ + k)

            def gate_use(t, tb, k, blk=blk, hb=hb, hsl=hsl):
                w1 = t[:, 0:2048].rearrange("p (a b) -> p a b", a=16)
                w2 = t[:, 2048:4096].rearrange("p (a b) -> p a b", a=16)
                p1, p1b = pbank()
                p2, p2b = pbank()
                for kt in range(16):
                    mm(p1[:], w1[:, kt, :], hsl(kt), kt == 0, kt == 15, [tb, hb], p1b)
                for kt in range(16):
                    mm(p2[:], w2[:, kt, :], hsl(kt), kt == 0, kt == 15, [tb, hb], p2b)
                b1 = sp_t[:, C_BIN + 20 + k:C_BIN + 21 + k]
                b2 = sp_t[:, C_BIN + 36 + k:C_BIN + 37 + k]
                S.op(act, lambda h: h.activation(out=sg1[:], in_=p1[:], func=AF.Sigmoid, bias=b1), reads=[p1b, B_const], writes=[B_sg])
                S.op(act, lambda h: h.activation(out=sg2[:], in_=p2[:], func=AF.Sigmoid, bias=b2), reads=[p2b, B_const], writes=[B_sg])

            def proj_use(t, tb, k, blk=blk):
                wu_ = t[:, 0:512].rearrange("p (a b) -> p a b", a=4)
                wc_ = t[:, 512:1536].rearrange("p (a b) -> p a b", a=8)
                p3, p3b = pbank()
                p4, p4b = pbank()
                for kt in range(4):
                    mm(p3[:], wu_[:, kt, :], s5o[:, kt, blk * TB:(blk + 1) * TB], kt == 0, kt == 3, [tb, B_s5o], p3b)
                for kt in range(8):
                    mm(p4[:], wc_[:, kt, :], coT[:, kt, :], kt == 0, kt == 7, [tb, B_co], p4b)
                S.op(dve, lambda h: h.tensor_mul(out=sg1[:], in0=sg1[:], in1=p3[:]), reads=[B_sg, p3b], writes=[B_sg])
                S.op(dve, lambda h: h.tensor_mul(out=sg2[:], in0=sg2[:], in1=p4[:]), reads=[B_sg, p4b], writes=[B_sg])
                S.op(dve, lambda h: h.tensor_add(out=mgT[:, k, :], in0=sg1[:], in1=sg2[:]), reads=[B_sg], writes=[B_mg])

            units = []
            for k in range(16):
                units.append(((lambda k=k: gate_ld(k)), (lambda t, tb, k=k: gate_use(t, tb, k))))
                units.append(((lambda k=k: proj_ld(k)), (lambda t, tb, k=k: proj_use(t, tb, k))))
            pipeline(units)

            if blk == 0:
                dump("coT", coT[:], [128, 8, TB], BF16, [B_co])
                dump("mgT", mgT, [128, 16, TB], BF16, [B_mg])
            S.barrier([dbg_ch])
            sa3.close()
          with contextlib.ExitStack() as sb_:
            lnb1 = sb(sb_, "lnb1%d" % blk, [128, 2, D], F32)
            B_lnb = Buf("lnb1")
            ch_l = S.chan()
            S.dma(sp, lambda h: h.dma_start(out=lnb1[:, 0, :], in_=lnbc_d[0]), ch_l, writes=[B_lnb])
            S.dma(sp, lambda h: h.dma_start(out=lnb1[:, 1, :], in_=lnbc_d[1]), ch_l, writes=[B_lnb])
            B_lnb.w = (ch_l.sem, ch_l.cnt)
            x1t = sb(sb_, "x1t%d" % blk, [128, D], F32)
            B_x1t = Buf("x1t")
            h2, B_h2 = x1t, B_x1t
            h2b = sb(sb_, "h2b%d" % blk, [128, D], BF16)
            B_h2b = Buf("h2b")
            h2T = sb(sb_, "h2T%d" % blk, [128, 16, 128], F32)
            B_h2T = Buf("h2T")
            rt = sb(sb_, "rt%d" % blk, [128, 16, 64], F32)
            B_rt = Buf("rt")
            ohb = sb(sb_, "ohb%d" % blk, [128, 64], BF16)
            B_oh = Buf("ohb")
            wr = sb(sb_, "wr%d" % blk, [128, 16, 72], F32)
            brt = sb(sb_, "brt%d" % blk, [128, 72], F32)
            B_wr = Buf("wr")
            S.dma(sp, lambda h: h.dma_start(out=wr[:], in_=kview(w_rt, 16)), ch_l, writes=[B_wr])
            S.dma(sp, lambda h: h.dma_start(out=brt[:], in_=brt_d), ch_l, writes=[B_wr])
            B_wr.w = (ch_l.sem, ch_l.cnt)
            B_lnb.w = (ch_l.sem, ch_l.cnt)
            def wo_ld(fb):
                return mload(MX_WO + fb)

            def wo_use(t, tb, fb):
                wo = t[:, 0:4096].rearrange("p (a b) -> p a b", a=16)
                for tt in range(4):
                    po, pob = pbank()
                    for kt in range(16):
                        mm(po[:, 0:256], mgT[:, kt, tt * 128:(tt + 1) * 128], wo[:, kt, :], kt == 0, kt == 15, [tb, B_mg], pob)
                    S.op(dve, lambda h, tt=tt, po=po: h.tensor_mul(out=res[:, tt, fb * 256:(fb + 1) * 256], in0=po[:, 0:256],
                                                                  in1=modbc[:, 0, fb * 256:(fb + 1) * 256]),
                         reads=[pob, B_modbc], writes=[B_res[tt]])

            pipeline([((lambda fb=fb: wo_ld(fb)), (lambda t, tb, fb=fb: wo_use(t, tb, fb))) for fb in range(8)])

            for tt in range(4):
                T = blk * 4 + tt
                rows = slice(blk * TB + tt * 128, blk * TB + (tt + 1) * 128)
                t, tb = load_x(x_own[rows, :])
                r_ = res[:, tt, :]
                rb_ = B_res[tt]
                S.op(dve, lambda h, t=t, r_=r_: h.scalar_tensor_tensor(out=r_, in0=t[:], scalar=ALPHA, in1=r_, op0=ALU.mult, op1=ALU.add),
                     reads=[tb, rb_], writes=[rb_])
                ln_stats(r_, rb_)
                S.op(dve, lambda h, r_=r_: h.tensor_scalar(out=r_, in0=r_, scalar1=lnmv[:, 0:1], scalar2=lnmv[:, 2:3],
                                                         op0=ALU.subtract, op1=ALU.mult), reads=[rb_, B_ln], writes=[rb_])
                S.op(dve, lambda h, r_=r_: h.tensor_mul(out=r_, in0=r_, in1=lnb1[:, 0, :]), reads=[rb_, B_lnb], writes=[rb_])
                S.op(dve, lambda h, r_=r_: h.tensor_add(out=x1t[:], in0=r_, in1=lnb1[:, 1, :]), reads=[rb_, B_lnb], writes=[B_x1t])
                S.dma(sp, lambda h, rows=rows: h.dma_start(out=x1_d[rows, :], in_=x1t[:]), ch_x1, reads=[B_x1t], writes=[B_x1d])
                ln_stats(x1t[:], B_x1t)
                S.op(dve, lambda h: h.tensor_scalar(out=h2[:], in0=x1t[:], scalar1=lnmv[:, 0:1], scalar2=lnmv[:, 2:3],
                                                    op0=ALU.subtract, op1=ALU.mult), reads=[B_x1t, B_ln], writes=[B_h2])
                S.op(dve, lambda h: h.tensor_mul(out=h2[:], in0=h2[:], in1=modbc[:, 2, :]), reads=[B_h2, B_modbc], writes=[B_h2])
                S.op(dve, lambda h: h.tensor_add(out=h2[:], in0=h2[:], in1=modbc[:, 3, :]), reads=[B_h2, B_modbc], writes=[B_h2])
                S.op(act, lambda h: h.activation(out=h2b[:], in_=h2[:], func=AF.Copy), reads=[B_h2], writes=[B_h2b])
                for k4 in range(4):
                    pt_, ptb_ = pbank()
                    for j in range(4):
                        kt = k4 * 4 + j
                        S.op(pe, lambda h, kt=kt, j=j, pt_=pt_: h.transpose(out=pt_[:, j * 128:(j + 1) * 128],
                                                                        in_=h2[:, kt * 128:(kt + 1) * 128], identity=identf),
                             reads=[B_h2, B_const], writes=[ptb_])
                    S.op(act, lambda h, k4=k4, pt_=pt_: h.activation(
                        out=h2T[:, k4 * 4:(k4 + 1) * 4, :].rearrange("p a b -> p (a b)"), in_=pt_[:], func=AF.Copy),
                        reads=[ptb_], writes=[B_h2T])
                pl_, plb = pbank()
                for kt in range(16):
                    mm(pl_[:, 0:72], h2T[:, kt, :], wr[:, kt, :], kt == 0, kt == 15, [B_h2T, B_wr], plb)
                lg = rt[:, 0:2, :].rearrange("p a b -> p (a b)")[:, 0:72]
                V = lambda r, n=8: rt[:, r, 0:n]
                S.op(dve, lambda h: h.tensor_add(out=lg, in0=pl_[:, 0:72], in1=brt[:]), reads=[plb, B_wr], writes=[B_rt])
                o = lambda fn, **kw: S.op(dve, fn, reads=[B_rt] + kw.get("r", []), writes=[B_rt] + kw.get("w", []))
                gl = lg[:, 0:8]
                el3 = lg[:, 8:72].rearrange("p (g e) -> p g e", g=8)
                o(lambda h: h.reduce_max(out=V(2, 1), in_=gl, axis=AX.X))
                o(lambda h: h.tensor_scalar(out=V(3), in0=gl, scalar1=V(2, 1), scalar2=None, op0=ALU.is_equal))
                o(lambda h: h.tensor_scalar(out=V(4), in0=gl, scalar1=V(2, 1), scalar2=None, op0=ALU.subtract))
                S.op(act, lambda h: h.activation(out=V(4), in_=V(4), func=AF.Exp), reads=[B_rt], writes=[B_rt])
                o(lambda h: h.reduce_sum(out=V(5, 1), in_=V(4), axis=AX.X))
                o(lambda h: h.reciprocal(out=V(5, 1), in_=V(5, 1)))
                prod = rt[:, 6, :].rearrange("p (g e) -> p g e", g=8)
                o(lambda h: h.tensor_tensor(out=prod, in0=el3, in1=V(3).unsqueeze(2).to_broadcast([128, 8, 8]), op=ALU.mult))
                o(lambda h: h.reduce_sum(out=V(7), in_=rt[:, 6, :].rearrange("p (g e) -> p e g", g=8), axis=AX.X))
                o(lambda h: h.reduce_max(out=V(8, 1), in_=V(7), axis=AX.X))
                o(lambda h: h.tensor_scalar(out=V(9), in0=V(7), scalar1=V(8, 1), scalar2=None, op0=ALU.is_equal))
                o(lambda h: h.scalar_tensor_tensor(out=V(10), in0=V(9), scalar=-1e30, in1=V(7), op0=ALU.mult, op1=ALU.add))
                o(lambda h: h.reduce_max(out=V(11, 1), in_=V(10), axis=AX.X))
                o(lambda h: h.tensor_scalar(out=V(12), in0=V(10), scalar1=V(11, 1), scalar2=None, op0=ALU.is_equal))
                o(lambda h: h.tensor_sub(out=V(13, 1), in0=V(11, 1), in1=V(8, 1)))
                S.op(act, lambda h: h.activation(out=V(13, 1), in_=V(13, 1), func=AF.Exp), reads=[B_rt], writes=[B_rt])
                o(lambda h: h.tensor_scalar_add(out=V(14, 1), in0=V(13, 1), scalar1=1.0))
                o(lambda h: h.reciprocal(out=V(14, 1), in_=V(14, 1)))
                o(lambda h: h.tensor_mul(out=V(15, 1), in0=V(13, 1), in1=V(14, 1)))
                o(lambda h: h.tensor_mul(out=rinfo[:, T, 2:3], in0=V(14, 1), in1=V(5, 1)), w=[B_rinfo])
                o(lambda h: h.tensor_mul(out=rinfo[:, T, 3:4], in0=V(15, 1), in1=V(5, 1)), w=[B_rinfo])
                oh1 = rt[:, 0, :].rearrange("p (g e) -> p g e", g=8)
                oh2 = rt[:, 1, :].rearrange("p (g e) -> p g e", g=8)
                o(lambda h: h.tensor_tensor(out=oh1, in0=V(3).unsqueeze(2).to_broadcast([128, 8, 8]),
                                            in1=V(9).unsqueeze(1).to_broadcast([128, 8, 8]), op=ALU.mult))
                o(lambda h: h.tensor_tensor(out=oh2, in0=V(3).unsqueeze(2).to_broadcast([128, 8, 8]),
                                            in1=V(12).unsqueeze(1).to_broadcast([128, 8, 8]), op=ALU.mult))
                S.op(dve, lambda h: h.tensor_add(out=ohb[:], in0=rt[:, 0, :], in1=rt[:, 1, :]), reads=[B_rt], writes=[B_oh])
                pc2, pc2b = pbank()
                mm(pc2[:, 0:64], trib[:], ohb[:], True, True, [B_const, B_oh], pc2b)
                pt2, pt2b = pbank()
                mm(pt2[:, 0:64], onesb[:], ohb[:], True, True, [B_const, B_oh], pt2b)
                S.op(dve, lambda h: h.tensor_add(out=rt[:, 6, :], in0=pc2[:, 0:64], in1=basebc[:]), reads=[pc2b, B_base, B_rt], writes=[B_rt])
                S.op(dve, lambda h: h.tensor_add(out=basebc[:], in0=basebc[:], in1=pt2[:, 0:64]), reads=[pt2b, B_rt], writes=[B_base])
                for s_, ohr in ((0, 0), (1, 1)):
                    o(lambda h, ohr=ohr: h.tensor_mul(out=rt[:, 7, :], in0=rt[:, 6, :], in1=rt[:, ohr, :]))
                    o(lambda h: h.reduce_sum(out=V(8, 1), in_=rt[:, 7, :], axis=AX.X))
                    o(lambda h, ohr=ohr: h.tensor_mul(out=rt[:, 7, :], in0=iota[:, 0:64], in1=rt[:, ohr, :]), r=[B_const])
                    o(lambda h: h.reduce_sum(out=V(9, 1), in_=rt[:, 7, :], axis=AX.X))
                    o(lambda h: h.tensor_scalar(out=V(10, 1), in0=V(8, 1), scalar1=float(CAP), scalar2=1.0e6, op0=ALU.is_ge, op1=ALU.mult))
                    o(lambda h: h.scalar_tensor_tensor(out=V(9, 1), in0=V(9, 1), scalar=float(CAP), in1=V(8, 1), op0=ALU.mult, op1=ALU.add))
                    o(lambda h, s_=s_: h.tensor_add(out=rinfo[:, T, s_:s_ + 1], in0=V(9, 1), in1=V(10, 1)), w=[B_rinfo])
                S.op(dve, lambda h, T=T: h.tensor_copy(out=rdest[:, T, :], in_=rinfo[:, T, 0:2]), reads=[B_rinfo], writes=[B_rinfo])
                for s_ in range(2):
                    S.dma(pool, lambda h, T=T, s_=s_: h.indirect_dma_start(
                        out=xdisp[:, :], out_offset=bass.IndirectOffsetOnAxis(ap=rdest[:, T, s_:s_ + 1], axis=0),
                        in_=h2b[:, :], in_offset=None, bounds_check=bc_reg, oob_is_err=False),
                        ch_xd, reads=[B_h2b, B_rinfo], writes=[B_xd])
            S.barrier([ch_l])
        S.barrier([ch_x1, ch_xd])

    cv_finalize()
    ch_y = S.chan()
    B_yd = Buf("ydisp")
    with contextlib.ExitStack() as pe_:
        xg = [sb(pe_, "xg%d" % i, [128, D], BF16) for i in range(2)]
        B_xg = [Buf("xg%d" % i) for i in range(2)]
        C_xg = [S.chan() for _ in range(2)]
        xgT = sb(pe_, "xgT", [128, 16, 128], BF16)
        B_xgT = Buf("xgT")
        sil = sb(pe_, "sil", [128, 512], F32)
        B_sil = Buf("sil")
        actb = sb(pe_, "actb", [128, 512], BF16)
        B_actb = Buf("actb")
        actT = sb(pe_, "actT", [128, 4, 128], BF16)
        B_actT = Buf("actT")
        yo = [sb(pe_, "yo%d" % i, [128, D], F32) for i in range(2)]
        B_yo = [Buf("yo%d" % i) for i in range(2)]

        def ex_load(e):
            i = e % 2
            S.dma(sp, lambda h: h.dma_start(out=xg[i][:], in_=xdisp[e * CAP:(e + 1) * CAP, :]), C_xg[i], reads=[B_xd], writes=[B_xg[i]])

        ex_load(0)
        state = {}
        units = []
        for e in range(NE):
            def tr_in(e):
                i = e % 2
                if e + 1 < NE:
                    ex_load(e + 1)
                for k4 in range(4):
                    ptile, ptb = tbank()
                    for j in range(4):
                        kt = k4 * 4 + j
                        S.op(pe, lambda h, kt=kt, j=j, ptile=ptile: h.transpose(out=ptile[:, j * 128:(j + 1) * 128],
                                                                          in_=xg[i][:, kt * 128:(kt + 1) * 128], identity=identb[:]),
                             reads=[B_xg[i], B_const], writes=[ptb])
                    cast(xgT[:, k4 * 4:(k4 + 1) * 4, :].rearrange("p a b -> p (a b)"), ptile[:, 0:512], [ptb], [B_xgT], eng=[act, dve][k4 % 2])
                if e == 0:
                    dump("xg0", xg[i][:], [128, D], BF16, [B_xg[i]])
                    dump("xgT0", xgT[:], [128, 16, 128], BF16, [B_xgT])
                state["pg"] = pbank()
                state["pu"] = pbank()

            def gu_use(t, tb, which, hf, e=e):
                if which == 0 and hf == 0:
                    tr_in(e)
                w = t[:, 0:4096].rearrange("p (a b) -> p a b", a=16)
                ps_, pb = state["pg"] if which == 0 else state["pu"]
                for kt in range(16):
                    mm(ps_[:, hf * 256:(hf + 1) * 256], xgT[:, kt, :], w[:, kt, :], kt == 0, kt == 15, [tb, B_xgT], pb)
                if which == 1 and hf == 1:
                    pg_, pgb = state["pg"]
                    S.op(act, lambda h: h.activation(out=sil[:], in_=pg_[:], func=AF.Silu), reads=[pgb], writes=[B_sil])
                    S.op(dve, lambda h: h.tensor_mul(out=actb[:], in0=sil[:], in1=ps_[:]), reads=[B_sil, pb], writes=[B_actb])
                    ptile, ptb = tbank()
                    for j in range(4):
                        S.op(pe, lambda h, j=j, ptile=ptile: h.transpose(out=ptile[:, j * 128:(j + 1) * 128],
                                                                     in_=actb[:, j * 128:(j + 1) * 128], identity=identb[:]),
                             reads=[B_actb, B_const], writes=[ptb])
                    cast(actT[:].rearrange("p a b -> p (a b)"), ptile[:, 0:512], [ptb], [B_actT], eng=act)

            def dn_use(t, tb, hf, e=e):
                w = t[:, 0:4096].rearrange("p (a b) -> p a b", a=4)
                i = e % 2
                for nb in range(2):
                    ps_, pb = pbank()
                    for kt in range(4):
                        mm(ps_[:], actT[:, kt, :], w[:, kt, nb * 512:(nb + 1) * 512], kt == 0, kt == 3, [tb, B_actT], pb)
                    c0 = hf * 1024 + nb * 512
                    cast(yo[i][:, c0:c0 + 512], ps_[:], [pb], [B_yo[i]], eng=[act, dve][nb])
                if hf == 1:
                    if e == 0:
                        dump("yo0", yo[i][:], [128, D], F32, [B_yo[i]])
                        dump("actT0", actT[:], [128, 4, 128], BF16, [B_actT])
                    for hc in range(2):
                        S.dma(sp, lambda h, hc=hc: h.dma_start(out=ydh[hc][e * CAP:(e + 1) * CAP, :], in_=yo[i][:, hc * 1024:(hc + 1) * 1024]),
                              ch_y, reads=[B_yo[i]], writes=[B_yd])

            for which in (0, 1):
                for hf in range(2):
                    units.append(((lambda e=e, which=which, hf=hf: eload(e * 6 + which * 2 + hf)),
                                  (lambda t, tb, which=which, hf=hf, f=gu_use: f(t, tb, which, hf))))
            for hf in range(2):
                units.append(((lambda e=e, hf=hf: eload(e * 6 + 4 + hf)),
                              (lambda t, tb, hf=hf, f=dn_use: f(t, tb, hf))))
        pipeline(units)
        S.barrier([ch_y])

    ch_o = [S.chan() for _ in range(2)]
    with contextlib.ExitStack() as pf:
        alloc_xt(pf, 2)
        ya_ = [sb(pf, "ya%d" % i, [128, D], F32) for i in range(2)]
        yb_ = [sb(pf, "yb%d" % i, [128, D], F32) for i in range(2)]
        B_ya_ = [Buf("ya%d" % i) for i in range(2)]
        B_yb_ = [Buf("yb%d" % i) for i in range(2)]
        ch_g = [S.chan() for _ in range(4)]
        ot = [sb(pf, "ot%d" % i, [128, D], F32) for i in range(2)]
        B_ot = [Buf("ot%d" % i) for i in range(2)]
        lnb2 = sb(pf, "lnb2", [128, 2, D], F32)
        B_lnb2 = Buf("lnb2")
        ch_f = S.chan()
        S.dma(sp, lambda h: h.dma_start(out=lnb2[:, 0, :], in_=lnbc_d[2]), ch_f, writes=[B_lnb2])
        S.dma(sp, lambda h: h.dma_start(out=lnb2[:, 1, :], in_=lnbc_d[3]), ch_f, writes=[B_lnb2])
        B_lnb2.w = (ch_f.sem, ch_f.cnt)

        def fetch(T):
            k = T % 2
            rows = slice(T * 128, (T + 1) * 128)
            S.op(pool, lambda h: h.memset(ya_[k][:], 0.0), writes=[B_ya_[k]])
            S.op(pool, lambda h: h.memset(yb_[k][:], 0.0), writes=[B_yb_[k]])
            for s_, (dst, db, chn) in enumerate(((ya_[k], B_ya_[k], ch_g[2 * k]), (yb_[k], B_yb_[k], ch_g[2 * k + 1]))):
                for hc in range(2):
                    S.dma(pool, lambda h, dst=dst, s_=s_, hc=hc: h.indirect_dma_start(
                        out=dst[:, hc * 1024:(hc + 1) * 1024], out_offset=None, in_=ydh[hc][:, :],
                        in_offset=bass.IndirectOffsetOnAxis(ap=rdest[:, T, s_:s_ + 1], axis=0),
                        bounds_check=bc_reg, oob_is_err=False), chn, reads=[B_yd, B_rinfo], writes=[db])
            return load_x(x1_d[rows, :])

        pend = fetch(0)
        for T in range(16):
            rows = slice(T * 128, (T + 1) * 128)
            nxt = fetch(T + 1) if T + 1 < 16 else None
            t, tb = pend
            pend = nxt
            ya, yb2, B_ya, B_yb = ya_[T % 2], yb_[T % 2], B_ya_[T % 2], B_yb_[T % 2]
            o_ = ot[T % 2]
            ob = B_ot[T % 2]
            S.op(dve, lambda h, ya=ya: h.tensor_scalar(out=ya[:], in0=ya[:], scalar1=rinfo[:, T, 2:3], scalar2=None, op0=ALU.mult),
                 reads=[B_ya, B_rinfo], writes=[B_ya])
            S.op(dve, lambda h, ya=ya, yb2=yb2: h.scalar_tensor_tensor(out=ya[:], in0=yb2[:], scalar=rinfo[:, T, 3:4], in1=ya[:], op0=ALU.mult, op1=ALU.add),
                 reads=[B_ya, B_yb, B_rinfo], writes=[B_ya])
            S.op(dve, lambda h, ya=ya: h.tensor_mul(out=ya[:], in0=ya[:], in1=modbc[:, 1, :]), reads=[B_ya, B_modbc], writes=[B_ya])
            S.op(dve, lambda h, t=t, ya=ya: h.scalar_tensor_tensor(out=ya[:], in0=t[:], scalar=ALPHA, in1=ya[:], op0=ALU.mult, op1=ALU.add),
                 reads=[tb, B_ya], writes=[B_ya])
            ln_stats(ya[:], B_ya)
            S.op(dve, lambda h, ya=ya: h.tensor_scalar(out=ya[:], in0=ya[:], scalar1=lnmv[:, 0:1], scalar2=lnmv[:, 2:3],
                                                       op0=ALU.subtract, op1=ALU.mult), reads=[B_ya, B_ln], writes=[B_ya])
            S.op(dve, lambda h, ya=ya: h.tensor_mul(out=ya[:], in0=ya[:], in1=lnb2[:, 0, :]), reads=[B_ya, B_lnb2], writes=[B_ya])
            S.op(dve, lambda h, o_=o_, ya=ya: h.tensor_add(out=o_[:], in0=ya[:], in1=lnb2[:, 1, :]), reads=[B_ya, B_lnb2], writes=[ob])
            S.dma(sp, lambda h, o_=o_, rows=rows: h.dma_start(out=out[rows, :], in_=o_[:]), ch_o[T % 2], reads=[ob])
        dump("rinfo", rinfo[:], [128, 16, 4], F32, [B_rinfo])
        S.barrier(ch_o + [dbg_ch])
    es.close()
    return nc


def _prep_inputs(inp):
    f = lambda k: np.ascontiguousarray(np.asarray(inp[k], dtype=np.float32))
    x = f("x")
    c = f("c")
    iota = np.tile(np.arange(512, dtype=np.float32)[None, :], (128, 1))
    cst = np.zeros((128, 4, 128), np.float32)
    cst[:, 0, :] = np.eye(128, dtype=np.float32)
    cst[:, 1, :] = 1.0
    cst[:, 2, :] = np.triu(np.ones((128, 128), np.float32), 1)
    b_in = f("b_in")[0]
    a_re = f("s5_a_re")[0]
    a_im = f("s5_a_im")[0]
    ldt = f("s5_log_dt")[0]
    b_re = f("s5_b_re")[0]
    b_im = f("s5_b_im")[0]
    c_re = f("s5_c_re")[0]
    c_im = f("s5_c_im")[0]
    sd = f("s5_d")[0].reshape(512)
    dw = f("conv_dw")[0][:, 0, :]
    sm = np.zeros((128, 512), np.float32)
    sm[:, 16:68] = b_in.reshape(52, 128).T
    sm[:, 68:84] = a_re.reshape(16, 128).T
    sm[:, 84:100] = a_im.reshape(16, 128).T
    sm[:, 100:116] = np.repeat(ldt, 64).reshape(16, 128).T
    sm[:, 116:120] = sd.reshape(4, 128).T
    sm[:, 120:128] = f("conv_dw_b")[0].reshape(8, 128).T
    sm[:, 128:136] = f("conv_ln_g")[0].reshape(8, 128).T
    sm[:, 136:144] = f("conv_ln_b")[0].reshape(8, 128).T
    sm[:, 160:408] = dw.T.reshape(8, 128, 31).transpose(1, 0, 2).reshape(128, 248)
    wb = np.zeros((2, 128, 16, 128), np.float32)
    wc = np.zeros((2, 128, 16, 128), np.float32)
    for g in range(32):
        i = g // 2
        gl = g % 2
        ch0 = (g % 8) * 16
        st0 = gl * 64
        wb[0, ch0:ch0 + 16, i, st0:st0 + 64] = b_re[g].T
        wb[1, ch0:ch0 + 16, i, st0:st0 + 64] = b_im[g].T
        wc[0, st0:st0 + 64, i, ch0:ch0 + 16] = c_re[g].T
        wc[1, st0:st0 + 64, i, ch0:ch0 + 16] = c_im[g].T
    lnbc = np.stack([np.tile(f(k)[0][None, :], (128, 1)) for k in ("ln1_g", "ln1_b", "ln2_g", "ln2_b")])
    w_route = np.ascontiguousarray(np.concatenate([f("w_route_group")[0], f("w_route_expert")[0]], axis=1))
    b_route = np.tile(np.concatenate([f("b_route_group")[0], f("b_route_expert")[0]])[None, :], (128, 1)).astype(np.float32)
    shared = {
        "iota": iota, "cst": cst, "w_ada": f("w_ada")[0], "b_ada": f("b_ada"), "w_in": f("w_in")[0],
        "wb": wb, "wc": wc, "w_s5_gate": f("w_s5_gate")[0], "w_s5_up": f("w_s5_up")[0],
        "w_conv_out": f("w_conv_out")[0], "w_out": f("w_out")[0], "lnbc": lnbc, "w_route": w_route,
        "b_route": b_route, "w_exp_gate": f("w_exp_gate")[0], "w_exp_up": f("w_exp_up")[0], "w_exp_down": f("w_exp_down")[0],
    }
    maps = []
    for core in range(8):
        b, half = core // 2, core % 2
        smc = sm.copy()
        smc[:, 0:16] = c[b].reshape(16, 128).T
        smc[:, 144] = float(half)
        m = dict(shared)
        m["smallp"] = smc
        m["x_own"] = np.ascontiguousarray(x[b, half * NTOK:(half + 1) * NTOK])
        m["x_prev"] = np.ascontiguousarray(x[b, 0:NTOK]) if half == 1 else np.zeros((NTOK, D), np.float32)
        maps.append(m)
    return maps


_NC = [None]


def kernel(**inputs):
    maps = _prep_inputs(inputs)
    if _NC[0] is None:
        _NC[0] = build_nc()
    res = run_bass_kernel_spmd(_NC[0], maps, core_ids=list(range(8)))
    outp = np.zeros((4, 2 * NTOK, D), np.float32)
    for core in range(8):
        b, half = core // 2, core % 2
        outp[b, half * NTOK:(half + 1) * NTOK] = res.results[core]["out"]
    if DEBUG:
        kernel.dbg = [res.results[c] for c in range(8)]
    return outp
```

```python
import contextlib
import numpy as np
import concourse.bass as bass
import concourse.mybir as mybir
from concourse.bass_utils import run_bass_kernel_spmd

F32 = mybir.dt.float32
BF16 = mybir.dt.bfloat16
I32 = mybir.dt.int32
ALU = mybir.AluOpType
AF = mybir.ActivationFunctionType
AX = mybir.AxisListType

D = 2048
NTOK = 2048
TB = 512
NBLK = NTOK // TB
INC = 6656
NE = 64
CAP = 128
ALPHA = 2.0 ** 0.25
EPS = 1e-5
MAGIC = 12582912.0
TWO_PI = 6.283185307179586
DEBUG = False


class Buf:
    def __init__(self, name):
        self.name = name
        self.w = None
        self.r = {}


class Chan:
    def __init__(self, sem):
        self.sem = sem
        self.cnt = 0


class Eng:
    def __init__(self, h, sem, is_pe=False):
        self.h = h
        self.sem = sem
        self.cnt = 0
        self.waited = {}
        self.is_pe = is_pe


class Sched:
    def __init__(self, nc, es):
        self.nc = nc
        self.es = es
        self.nsem = 0
        self.pe = Eng(nc.tensor, self.mksem(), True)
        self.dve = Eng(nc.vector, self.mksem())
        self.act = Eng(nc.scalar, self.mksem())
        self.pool = Eng(nc.gpsimd, self.mksem())
        self.sp = Eng(nc.sync, self.mksem())
        self.engs = [self.pe, self.dve, self.act, self.pool, self.sp]

    def mksem(self):
        self.nsem += 1
        return self.es.enter_context(self.nc.semaphore("sm%d" % self.nsem))

    def chan(self):
        return Chan(self.mksem())

    def _sync(self, e, reads, writes):
        deps = []
        for b in reads:
            if b.w is not None:
                deps.append(b.w)
        for b in writes:
            if b.w is not None:
                deps.append(b.w)
            deps.extend(b.r.values())
        for (sem, val) in deps:
            if e.is_pe and sem is e.sem:
                continue
            k = id(sem)
            if e.waited.get(k, 0) >= val:
                continue
            e.h.wait_ge(sem, val)
            e.waited[k] = val

    def _mark(self, tok, reads, writes):
        k = id(tok[0])
        for b in reads:
            if b.r.get(k, (None, 0))[1] < tok[1]:
                b.r[k] = tok
        for b in writes:
            b.w = tok
            b.r = {}

    def op(self, e, fn, reads=(), writes=()):
        self._sync(e, reads, writes)
        inst = fn(e.h)
        e.cnt += 1
        inst.then_inc(e.sem, 1)
        tok = (e.sem, e.cnt)
        self._mark(tok, reads, writes)
        return tok

    def dma(self, e, fn, chan, reads=(), writes=()):
        self._sync(e, reads, writes)
        inst = fn(e.h)
        chan.cnt += 16
        inst.then_inc(chan.sem, 16)
        tok = (chan.sem, chan.cnt)
        self._mark(tok, reads, writes)
        return tok

    def barrier(self, chans=()):
        for e in self.engs:
            for o in self.engs:
                if o is e or o.cnt == 0:
                    continue
                if e.waited.get(id(o.sem), 0) < o.cnt:
                    e.h.wait_ge(o.sem, o.cnt)
                    e.waited[id(o.sem)] = o.cnt
            for c in chans:
                if c.cnt and e.waited.get(id(c.sem), 0) < c.cnt:
                    e.h.wait_ge(c.sem, c.cnt)
                    e.waited[id(c.sem)] = c.cnt


def build_nc():
    nc = bass.Bass("TRN2", target_bir_lowering=False)

    def din(name, shape, dt=F32):
        return nc.dram_tensor(name, list(shape), dt, kind="ExternalInput").ap()

    x_own = din("x_own", [NTOK, D])
    x_prev = din("x_prev", [NTOK, D])
    smallp = din("smallp", [128, 512])
    iota_d = din("iota", [128, 512])
    cst_d = din("cst", [128, 4, 128])
    w_ada = din("w_ada", [D, 6 * D])
    b_ada = din("b_ada", [1, 6 * D])
    w_in = din("w_in", [D, INC])
    wb_d = din("wb", [2, 128, 16, 128])
    wc_d = din("wc", [2, 128, 16, 128])
    w_sg = din("w_s5_gate", [512, 512])
    w_su = din("w_s5_up", [512, D])
    w_co = din("w_conv_out", [1024, D])
    w_out = din("w_out", [D, D])
    lnbc_d = din("lnbc", [4, 128, D])
    w_rt = din("w_route", [D, 72])
    brt_d = din("b_route", [128, 72])
    w_eg = din("w_exp_gate", [NE, D, 512])
    w_eu = din("w_exp_up", [NE, D, 512])
    w_ed = din("w_exp_down", [NE, 512, D])
    out = nc.dram_tensor("out", [NTOK, D], F32, kind="ExternalOutput").ap()
    x1_d = nc.dram_tensor("x1s", [NTOK, D], F32, kind="ExternalOutput" if DEBUG else "Internal").ap()
    xdisp = nc.dram_tensor("xdisp", [NE * CAP, D], BF16, kind="Internal").ap()
    ydh = [nc.dram_tensor("ydisp%d" % i, [NE * CAP, 1024], F32, kind="Internal").ap() for i in range(2)]

    es = contextlib.ExitStack()
    S = Sched(nc, es)
    pe, dve, act, pool, sp = S.pe, S.dve, S.act, S.pool, S.sp

    dbg_ch = S.chan()

    def dump(name, ap, shape, dt, reads):
        if not DEBUG:
            return
        dd = nc.dram_tensor("dbg_" + name, list(shape), dt, kind="ExternalOutput").ap()
        S.dma(sp, lambda h: h.dma_start(out=dd, in_=ap), dbg_ch, reads=reads)

    def sb(stack, name, shape, dt):
        return stack.enter_context(nc.sbuf_tensor("s_" + name, list(shape), dt))

    PS = [es.enter_context(nc.psum_tensor("ps%d" % i, [128, 512], F32)) for i in range(6)]
    PSB = [Buf("ps%d" % i) for i in range(6)]
    PT = [es.enter_context(nc.psum_tensor("pt%d" % i, [128, 1024], BF16)) for i in range(2)]
    PTB = [Buf("pt%d" % i) for i in range(2)]
    pctr = [0]

    def pbank():
        i = pctr[0] % 6
        pctr[0] += 1
        return PS[i], PSB[i]

    tctr = [0]

    def tbank():
        i = tctr[0] % 2
        tctr[0] += 1
        return PT[i], PTB[i]

    sp_t = sb(es, "smallp", [128, 512], F32)
    iota = sb(es, "iota", [128, 512], F32)
    cst = sb(es, "cst", [128, 4, 128], F32)
    identb = sb(es, "identb", [128, 128], BF16)
    onesb = sb(es, "onesb", [128, 128], BF16)
    trib = sb(es, "trib", [128, 128], BF16)
    modpp = sb(es, "modpp", [128, 4, 16], F32)
    modbc = sb(es, "modbc", [128, 4, D], F32)
    rinfo = sb(es, "rinfo", [128, 16, 4], F32)
    rdest = sb(es, "rdest", [128, 16, 2], I32)
    basebc = sb(es, "basebc", [128, 64], F32)
    B_const = Buf("const")
    B_modpp = Buf("modpp")
    B_modbc = Buf("modbc")
    B_rinfo = Buf("rinfo")
    B_base = Buf("base")
    identf = cst[:, 0, :]
    onesf = cst[:, 1, :]

    C_C = 0
    C_BIN = 16
    C_ARE = 68
    C_AIM = 84
    C_LDT = 100
    C_SD = 116
    C_DWB = 120
    C_LNG = 128
    C_LNB = 136
    C_FLAG = 144
    C_DW = 160

    ch_par = S.chan()
    S.dma(sp, lambda h: h.dma_start(out=sp_t[:], in_=smallp), ch_par, writes=[B_const])
    S.dma(sp, lambda h: h.dma_start(out=iota[:], in_=iota_d), ch_par, writes=[B_const])
    S.dma(sp, lambda h: h.dma_start(out=cst[:], in_=cst_d), ch_par, writes=[B_const])
    B_const.w = (ch_par.sem, ch_par.cnt)
    S.op(dve, lambda h: h.tensor_copy(out=identb[:], in_=cst[:, 0, :]), reads=[B_const], writes=[B_const])
    S.op(dve, lambda h: h.tensor_copy(out=onesb[:], in_=cst[:, 1, :]), reads=[B_const], writes=[B_const])
    S.op(dve, lambda h: h.tensor_copy(out=trib[:], in_=cst[:, 2, :]), reads=[B_const], writes=[B_const])
    S.op(dve, lambda h: h.memset(basebc[:], 0.0), writes=[B_base])

    B_xd = Buf("xdisp")
    ch_xd = S.chan()
    bc_reg = nc.gpsimd.to_reg(NE * CAP - 1)

    NSLOT = 6
    wbf = [sb(es, "wbf%d" % i, [128, 4096], BF16) for i in range(NSLOT)]
    wbfB = [Buf("wbf%d" % i) for i in range(NSLOT)]
    wbfC = [S.chan() for _ in range(NSLOT)]
    wctr = [0]
    fst = {"t": [], "b": [], "c": [S.chan(), S.chan(), S.chan()], "n": 0, "k": 0}

    def alloc_fst(stack, n, width):
        fst["k"] += 1
        fst["t"] = [sb(stack, "fst%d_%d" % (fst["k"], i), [128, width], F32) for i in range(n)]
        fst["b"] = [Buf("fst%d" % i) for i in range(n)]
        fst["n"] = 0

    def cast(out_ap, in_ap, reads, writes, eng=None):
        if eng is act:
            S.op(act, lambda h: h.activation(out=out_ap, in_=in_ap, func=AF.Copy), reads=reads, writes=writes)
        else:
            S.op(eng, lambda h: h.tensor_copy(out=out_ap, in_=in_ap), reads=reads, writes=writes)

    def fload(pieces):
        i = fst["n"] % len(fst["t"])
        fst["n"] += 1
        off = 0
        for (ap, a, b) in pieces:
            v = fst["t"][i][:, off:off + a * b].rearrange("p (a b) -> p a b", a=a)
            S.dma(sp, lambda h, v=v, ap=ap: h.dma_start(out=v, in_=ap), fst["c"][i], writes=[fst["b"][i]])
            off += a * b
        return fst["t"][i], fst["b"][i]

    def wload(pieces, do_cast=True, dst=None, dstB=None):
        if not do_cast:
            return fload(pieces)
        i = wctr[0] % NSLOT
        wctr[0] += 1
        off = 0
        for (ap, a, b) in pieces:
            v = wbf[i][:, off:off + a * b].rearrange("p (a b) -> p a b", a=a)
            S.dma(pool, lambda h, v=v, ap=ap: h.dma_start(out=v, in_=ap), wbfC[i], writes=[wbfB[i]])
            off += a * b
        return wbf[i], wbfB[i]

    NCV = NE * 6
    wscr_l = [nc.dram_tensor("wscr%d" % j, [NCV // 2, 128, 4096], BF16, kind="Internal").ap() for j in range(2)]
    wscr = lambda u: wscr_l[u // (NCV // 2)][u % (NCV // 2)]
    cvB = [Buf("cv%d" % u) for u in range(NCV)]
    CVG = 32
    CV_MAX = 160
    cvC = [S.chan() for _ in range(NCV // CVG)]
    cv_next = [0]

    def cv_src(u):
        e, r = divmod(u, 6)
        if r < 4:
            wsrc = w_eg if r < 2 else w_eu
            hf = r % 2
            return kview(wsrc[e], 16)[:, :, hf * 256:(hf + 1) * 256], 16
        hf = r - 4
        return kview(w_ed[e], 4)[:, :, hf * 1024:(hf + 1) * 1024], 4

    def cv_issue(n):
        for _ in range(n):
            u = cv_next[0]
            if u >= NCV:
                return
            cv_next[0] += 1
            src, a = cv_src(u)
            dstv = wscr(u).rearrange("p (a b) -> p a b", a=a)
            S.dma(pool, lambda h, dstv=dstv, src=src: h.dma_start(out=dstv, in_=src), cvC[u // CVG], writes=[cvB[u]])

    def cv_finalize():
        for u in range(cv_next[0]):
            c = cvC[u // CVG]
            cvB[u].w = (c.sem, c.cnt)

    def eload(u):
        if u >= cv_next[0]:
            src, a = cv_src(u)
            return wload([(src, a, 4096 // a)])
        i = wctr[0] % NSLOT
        wctr[0] += 1
        S.dma(sp, lambda h: h.dma_start(out=wbf[i][:], in_=wscr(u)), wbfC[i], reads=[cvB[u]], writes=[wbfB[i]])
        return wbf[i], wbfB[i]

    def pipeline(units, depth=4):
        loaded = []
        n = len(units)
        for i in range(n + depth):
            if i < n:
                loaded.append(units[i][0]())
            j = i - depth
            if j >= 0:
                units[j][1](*loaded[j])
                loaded[j] = None

    def kview(ap2d, kt):
        return ap2d.rearrange("(kt p) n -> p kt n", p=128)

    w_in_v = kview(w_in, 16)
    w_ada_v = kview(w_ada, 16)

    mx_pieces = []
    for kp_ in range(8):
        mx_pieces.append([(w_in_v[:, :, 512 + kp_ * 128:512 + (kp_ + 1) * 128], 16, 128),
                          (w_in_v[:, :, 1536 + kp_ * 128:1536 + (kp_ + 1) * 128], 16, 128)])
    for k_ in range(16):
        mx_pieces.append([(w_in_v[:, :, 2560 + k_ * 128:2560 + (k_ + 1) * 128], 16, 128),
                          (w_in_v[:, :, 4608 + k_ * 128:4608 + (k_ + 1) * 128], 16, 128)])
    for k_ in range(16):
        mx_pieces.append([(kview(w_su, 4)[:, :, k_ * 128:(k_ + 1) * 128], 4, 128),
                          (kview(w_co, 8)[:, :, k_ * 128:(k_ + 1) * 128], 8, 128)])
    for fb_ in range(8):
        mx_pieces.append([(kview(w_out, 16)[:, :, fb_ * 256:(fb_ + 1) * 256], 16, 256)])
    MX_CONV, MX_GATE, MX_PROJ, MX_WO = 0, 8, 24, 40
    mscr = nc.dram_tensor("mscr", [48, 128, 4096], BF16, kind="Internal").ap()
    mxB = [Buf("mx%d" % j) for j in range(48)]
    mxC = S.chan()

    mxq = []
    for j_ in range(48):
        off_ = 0
        for (ap_, a_, b_) in mx_pieces[j_]:
            mxq.append((j_, off_, ap_, a_, b_))
            off_ += a_ * b_

    def mx_issue(n):
        k = 0
        while k < n and mxq:
            j, off, ap, a, b = mxq.pop(0)
            dstv = mscr[j][:, off:off + a * b].rearrange("p (a b) -> p a b", a=a)
            S.dma(pool, lambda h, dstv=dstv, ap=ap: h.dma_start(out=dstv, in_=ap), mxC, writes=[mxB[j]])
            k += 1
        return k

    def mx_convert_all():
        mx_issue(10 ** 6)

    def mx_finalize():
        for j in range(48):
            mxB[j].w = (mxC.sem, mxC.cnt)

    def mload(j):
        i = wctr[0] % NSLOT
        wctr[0] += 1
        S.dma(sp, lambda h: h.dma_start(out=wbf[i][:], in_=mscr[j]), wbfC[i], reads=[mxB[j]], writes=[wbfB[i]])
        return wbf[i], wbfB[i]


    def mm(ps, lhsT, rhs, start, stop, reads, pbuf):
        S.op(pe, lambda h: h.matmul(ps, lhsT=lhsT, rhs=rhs, start=start, stop=stop), reads=reads, writes=[pbuf])

    with contextlib.ExitStack() as pa:
        zt = sb(pa, "zt", [128, D], BF16)
        B_zt = Buf("zt")
        S.op(pool, lambda h: h.memset(zt[:], 0.0), writes=[B_zt])
        for e in range(NE):
            S.dma(pool, lambda h, e=e: h.dma_start(out=xdisp[e * CAP:(e + 1) * CAP, :], in_=zt[:]), ch_xd,
                  reads=[B_zt], writes=[B_xd])
        alloc_fst(pa, 3, D)
        cact = sb(pa, "cact", [128, 16], F32)
        cb = sb(pa, "cb", [128, 16, 128], F32)
        R = sb(pa, "R", [128, D], F32)
        bada = sb(pa, "bada", [1, D], F32)
        tmp3 = sb(pa, "tmp3", [128, 16, 128], F32)
        B_c = Buf("cact")
        B_R = Buf("R")
        B_ba = Buf("bada")
        B_t3 = Buf("tmp3")
        ch_a = S.chan()
        S.op(act, lambda h: h.activation(out=cact[:], in_=sp_t[:, C_C:C_C + 16], func=AF.Silu),
             reads=[B_const], writes=[B_c])
        for kt in range(16):
            S.op(dve, lambda h, kt=kt: h.tensor_copy(out=cb[:, kt, :], in_=cact[:, kt:kt + 1].to_broadcast([128, 128])),
                 reads=[B_c], writes=[B_c])
        pp_dst = {0: (0, False), 1: (1, True), 3: (2, False), 4: (3, True)}
        bc_dst = {2: [(0, 1.0)], 5: [(1, 1.0)], 4: [(2, 1.0)], 3: [(3, 0.0)]}
        for grp in range(6):
            banks = [pbank() for _ in range(4)]
            S.dma(sp, lambda h, grp=grp: h.dma_start(out=bada[:], in_=b_ada[:, grp * D:(grp + 1) * D]), ch_a, writes=[B_ba])

            def ld(grp, kt):
                return wload([(w_ada_v[:, kt:kt + 1, grp * D:(grp + 1) * D], 1, D)], do_cast=False)

            def use(t, tb, kt, banks):
                for nb in range(4):
                    mm(banks[nb][0][:], cb[:, kt, :], t[:, nb * 512:(nb + 1) * 512], kt == 0, False,
                       [tb, B_c], banks[nb][1])

            units = [((lambda kt=kt, grp=grp: ld(grp, kt)), (lambda t, tb, kt=kt, banks=banks: use(t, tb, kt, banks)))
                     for kt in range(16)]
            pipeline(units, depth=2)
            for nb in range(4):
                c0 = nb * 512
                mm(banks[nb][0][:], cst[0:1, 1, :], bada[0:1, c0:c0 + 512], False, True, [B_ba, B_const], banks[nb][1])
                S.op(act, lambda h, nb=nb, c0=c0, banks=banks: h.activation(out=R[:, c0:c0 + 512], in_=banks[nb][0][:], func=AF.Copy),
                     reads=[banks[nb][1]], writes=[B_R])
            if grp in pp_dst:
                vi, plus1 = pp_dst[grp]
                S.op(dve, lambda h: h.tensor_tensor(
                    out=tmp3[:], in0=R[:].rearrange("p (a b) -> p a b", a=16),
                    in1=identf.unsqueeze(1).to_broadcast([128, 16, 128]), op=ALU.mult),
                    reads=[B_R, B_const], writes=[B_t3])
                S.op(dve, lambda h, vi=vi: h.reduce_sum(out=modpp[:, vi, :], in_=tmp3[:], axis=AX.X),
                     reads=[B_t3], writes=[B_modpp])
                if plus1:
                    S.op(dve, lambda h, vi=vi: h.tensor_scalar_add(out=modpp[:, vi, :], in0=modpp[:, vi, :], scalar1=1.0),
                         reads=[B_modpp], writes=[B_modpp])
            for (di, add) in bc_dst.get(grp, []):
                S.op(dve, lambda h, di=di, add=add: h.tensor_scalar_add(out=modbc[:, di, :], in0=R[:], scalar1=add),
                     reads=[B_R], writes=[B_modbc])
        dump("modpp", modpp[:], [128, 4, 16], F32, [B_modpp])
        dump("modbc", modbc[:], [128, 4, D], F32, [B_modbc])
        S.barrier([ch_a, ch_xd, dbg_ch])

    s5p = sb(es, "s5p", [128, 12, 16], F32)
    B_s5p = Buf("s5p")
    hT_halo = sb(es, "hT_halo", [128, 16, 32], BF16)
    B_halo = Buf("halo")
    s5o = sb(es, "s5o", [128, 4, NTOK], BF16)
    B_s5o = Buf("s5o")
    lnst = sb(es, "lnst", [128, 4, 6], F32)
    lnmv = sb(es, "lnmv", [128, 4], F32)
    B_ln = Buf("lnst")
    pus = contextlib.ExitStack()
    wbb = sb(pus, "wbb", [128, 2, 16, 128], BF16)
    wcb = sb(pus, "wcb", [128, 2, 16, 128], BF16)
    B_wb = Buf("wbb")
    uT = sb(pus, "uT", [128, 4, 2 * NTOK], BF16)
    B_uT = [Buf("uT%d" % g) for g in range(2 * NBLK)]

    def sincos(eng, ang_ap, cos_out, sin_out, rb, wbuf, t, a, Bt):
        for (shift, outp) in ((0.0, sin_out), (np.pi / 2, cos_out)):
            S.op(eng, lambda h, shift=shift: h.tensor_scalar(out=a[:], in0=ang_ap, scalar1=float(shift), scalar2=None, op0=ALU.add),
                 reads=rb, writes=[Bt])
            S.op(eng, lambda h: h.tensor_scalar(out=t[:], in0=a[:], scalar1=1.0 / TWO_PI, scalar2=MAGIC, op0=ALU.mult, op1=ALU.add),
                 reads=[Bt], writes=[Bt])
            S.op(eng, lambda h: h.tensor_scalar(out=t[:], in0=t[:], scalar1=-MAGIC, scalar2=None, op0=ALU.add),
                 reads=[Bt], writes=[Bt])
            S.op(eng, lambda h: h.scalar_tensor_tensor(out=a[:], in0=t[:], scalar=-TWO_PI, in1=a[:], op0=ALU.mult, op1=ALU.add),
                 reads=[Bt], writes=[Bt])
            S.op(eng, lambda h: h.tensor_scalar(out=a[:], in0=a[:], scalar1=3.1415925, scalar2=-3.1415925, op0=ALU.min, op1=ALU.max),
                 reads=[Bt], writes=[Bt])
            S.op(act, lambda h, outp=outp: h.activation(out=outp, in_=a[:], func=AF.Sin), reads=[Bt], writes=wbuf)

    with contextlib.ExitStack() as pp:
        alloc_fst(pp, 2, 512)
        are = sp_t[:, C_ARE:C_ARE + 16]
        aim = sp_t[:, C_AIM:C_AIM + 16]
        sc = lambda i: s5p[:, i, :]
        S.op(act, lambda h: h.activation(out=sc(6), in_=sp_t[:, C_LDT:C_LDT + 16], func=AF.Exp), reads=[B_const], writes=[B_s5p])
        S.op(dve, lambda h: h.tensor_mul(out=sc(7), in0=are, in1=sc(6)), reads=[B_s5p, B_const], writes=[B_s5p])
        S.op(dve, lambda h: h.tensor_mul(out=sc(5), in0=aim, in1=sc(6)), reads=[B_s5p, B_const], writes=[B_s5p])
        S.op(act, lambda h: h.activation(out=sc(0), in_=sc(7), func=AF.Exp), reads=[B_s5p], writes=[B_s5p])
        sct = sb(pp, "sct", [128, 16], F32)
        sca = sb(pp, "sca", [128, 16], F32)
        sincos(dve, sc(5), sc(1), sc(2), [B_s5p], [B_s5p], sct, sca, Buf("scp"))
        S.op(dve, lambda h: h.tensor_mul(out=sc(8), in0=sc(0), in1=sc(1)), reads=[B_s5p], writes=[B_s5p])
        S.op(dve, lambda h: h.tensor_scalar_add(out=sc(8), in0=sc(8), scalar1=-1.0), reads=[B_s5p], writes=[B_s5p])
        S.op(dve, lambda h: h.tensor_mul(out=sc(9), in0=sc(0), in1=sc(2)), reads=[B_s5p], writes=[B_s5p])
        S.op(dve, lambda h: h.tensor_mul(out=sc(10), in0=are, in1=are), reads=[B_s5p, B_const], writes=[B_s5p])
        S.op(dve, lambda h: h.tensor_mul(out=sc(11), in0=aim, in1=aim), reads=[B_s5p, B_const], writes=[B_s5p])
        S.op(dve, lambda h: h.tensor_add(out=sc(10), in0=sc(10), in1=sc(11)), reads=[B_s5p], writes=[B_s5p])
        S.op(dve, lambda h: h.reciprocal(out=sc(10), in_=sc(10)), reads=[B_s5p], writes=[B_s5p])
        S.op(dve, lambda h: h.tensor_mul(out=sc(3), in0=sc(8), in1=are), reads=[B_s5p, B_const], writes=[B_s5p])
        S.op(dve, lambda h: h.tensor_mul(out=sc(11), in0=sc(9), in1=aim), reads=[B_s5p, B_const], writes=[B_s5p])
        S.op(dve, lambda h: h.tensor_add(out=sc(3), in0=sc(3), in1=sc(11)), reads=[B_s5p], writes=[B_s5p])
        S.op(dve, lambda h: h.tensor_mul(out=sc(3), in0=sc(3), in1=sc(10)), reads=[B_s5p], writes=[B_s5p])
        S.op(dve, lambda h: h.tensor_mul(out=sc(4), in0=sc(9), in1=are), reads=[B_s5p, B_const], writes=[B_s5p])
        S.op(dve, lambda h: h.tensor_mul(out=sc(11), in0=sc(8), in1=aim), reads=[B_s5p, B_const], writes=[B_s5p])
        S.op(dve, lambda h: h.tensor_sub(out=sc(4), in0=sc(4), in1=sc(11)), reads=[B_s5p], writes=[B_s5p])
        S.op(dve, lambda h: h.tensor_mul(out=sc(4), in0=sc(4), in1=sc(10)), reads=[B_s5p], writes=[B_s5p])
        dump("s5p", s5p[:], [128, 12, 16], F32, [B_s5p])
        for pl in range(2):
            for hf in range(4):
                t, tb = wload([(wb_d[pl, :, hf * 4:(hf + 1) * 4, :], 4, 128)], do_cast=False)
                S.op(dve, lambda h, t=t, pl=pl, hf=hf: h.tensor_copy(
                    out=wbb[:, pl, hf * 4:(hf + 1) * 4, :], in_=t[:, 0:512].rearrange("p (a b) -> p a b", a=4)),
                    reads=[tb], writes=[B_wb])
                t, tb = wload([(wc_d[pl, :, hf * 4:(hf + 1) * 4, :], 4, 128)], do_cast=False)
                S.op(dve, lambda h, t=t, pl=pl, hf=hf: h.tensor_scalar(
                    out=wcb[:, pl, hf * 4:(hf + 1) * 4, :], in0=t[:, 0:512].rearrange("p (a b) -> p a b", a=4),
                    scalar1=(1.0 if pl == 0 else -1.0), scalar2=None, op0=ALU.mult),
                    reads=[tb], writes=[B_wb])
        S.barrier()


    def ln_stats(x_ap, xb):
        for c in range(4):
            S.op(dve, lambda h, c=c: h.bn_stats(out=lnst[:, c, :], in_=x_ap[:, c * 512:(c + 1) * 512]),
                 reads=[xb], writes=[B_ln])
        S.op(dve, lambda h: h.bn_aggr(out=lnmv[:, 0:2], in_=lnst[:].rearrange("p a b -> p (a b)")), reads=[B_ln], writes=[B_ln])
        S.op(dve, lambda h: h.tensor_scalar_add(out=lnmv[:, 3:4], in0=lnmv[:, 1:2], scalar1=EPS), reads=[B_ln], writes=[B_ln])
        S.op(act, lambda h: h.activation(out=lnmv[:, 3:4], in_=lnmv[:, 3:4], func=AF.Sqrt), reads=[B_ln], writes=[B_ln])
        S.op(dve, lambda h: h.reciprocal(out=lnmv[:, 2:3], in_=lnmv[:, 3:4]), reads=[B_ln], writes=[B_ln])

    xts = {"t": [], "b": [], "c": [S.chan(), S.chan()], "n": 0, "k": 0}

    def alloc_xt(stack, n):
        xts["k"] += 1
        xts["t"] = [sb(stack, "xt%d_%d" % (xts["k"], i), [128, D], F32) for i in range(n)]
        xts["b"] = [Buf("xt%d" % i) for i in range(n)]
        xts["n"] = 0

    def load_x(src_ap):
        i = xts["n"] % len(xts["t"])
        xts["n"] += 1
        tt_, bb_ = xts["t"][i], xts["b"][i]
        S.dma(sp, lambda h: h.dma_start(out=tt_[:], in_=src_ap), xts["c"][i], writes=[bb_])
        return tt_, bb_

    with contextlib.ExitStack() as pu:
        alloc_xt(pu, 2)
        xn = sb(pu, "xn", [128, 4, D], BF16)
        B_xn = Buf("xn")
        hTp = [sb(pu, "hTp%d" % i, [128, 16, TB], BF16) for i in range(1)]
        B_hTp = [Buf("hTp%d" % i) for i in range(1)]
        def make_hT(src, blk, xn_t, xn_b, hdst, hbuf):
            for tt in range(4):
                t, tb = load_x(src[blk * TB + tt * 128: blk * TB + (tt + 1) * 128, :])
                ln_stats(t, tb)
                S.op(dve, lambda h, t=t, tt=tt: h.tensor_scalar(out=xn_t[:, tt, :], in0=t[:], scalar1=lnmv[:, 0:1], scalar2=lnmv[:, 2:3],
                                                              op0=ALU.subtract, op1=ALU.mult),
                     reads=[tb, B_ln], writes=[xn_b])
            for kt in range(16):
                ptile, ptb = tbank()
                for tt in range(4):
                    S.op(pe, lambda h, kt=kt, tt=tt, ptile=ptile: h.transpose(
                        out=ptile[:, tt * 128:(tt + 1) * 128], in_=xn_t[:, tt, kt * 128:(kt + 1) * 128], identity=identb[:]),
                        reads=[xn_b, B_const], writes=[ptb])
                S.op(act, lambda h, kt=kt, ptile=ptile: h.activation(
                    out=hdst[:, kt, :], in_=ptile[:, 0:512], func=AF.Identity,
                    scale=modpp[:, 1, kt:kt + 1], bias=modpp[:, 0, kt:kt + 1]),
                    reads=[ptb, B_modpp], writes=[hbuf])

        for g in range(2 * NBLK):
            own = g >= NBLK
            blk = g - NBLK if own else g
            src = x_own if own else x_prev
            make_hT(src, blk, xn, B_xn, hTp[0], B_hTp[0])
            def u_use(t, tb, hf, g=g):
                w = t[:, 0:4096].rearrange("p (a b) -> p a b", a=16)
                for m2 in range(2):
                    m = hf * 2 + m2
                    ps, pb = pbank()
                    for kt in range(16):
                        mm(ps[:], w[:, kt, m2 * 128:(m2 + 1) * 128], hTp[0][:, kt, :], kt == 0, kt == 15, [tb, B_hTp[0]], pb)
                    S.op(act, lambda h, m=m, ps=ps, g=g: h.activation(
                        out=uT[:, m, g * TB:(g + 1) * TB], in_=ps[:], func=AF.Identity, bias=sp_t[:, C_BIN + m:C_BIN + m + 1]),
                        reads=[pb, B_const], writes=[B_uT[g]])

            pipeline([((lambda hf=hf: wload([(w_in_v[:, :, hf * 256:(hf + 1) * 256], 16, 256)])),
                       (lambda t, tb, hf=hf: u_use(t, tb, hf))) for hf in range(2)])
            if g == NBLK - 1:
                S.op(dve, lambda h: h.tensor_copy(out=hT_halo[:], in_=hTp[0][:, :, TB - 32:TB]),
                     reads=[B_hTp[0]], writes=[B_halo])
        S.barrier()

    with contextlib.ExitStack() as ps5:
        cs = sb(ps5, "cs", [128, TB], F32)
        sn = sb(ps5, "sn", [128, TB], F32)
        mqr = sb(ps5, "mqr", [128, TB], F32)
        mqi = sb(ps5, "mqi", [128, TB], F32)
        dec = sb(ps5, "dec", [128, TB], F32)
        B_tab = Buf("tab")
        bre = sb(ps5, "bre", [128, TB], F32)
        bim = sb(ps5, "bim", [128, TB], F32)
        B_b = Buf("b")
        t1 = sb(ps5, "t1", [128, TB], F32)
        t2 = sb(ps5, "t2", [128, TB], F32)
        t3 = sb(ps5, "t3", [128, TB], F32)
        t4 = sb(ps5, "t4", [128, TB], F32)
        B_t12 = Buf("t12")
        B_t34 = Buf("t34")
        ang = t1
        sct2, sca2, B_sc2 = t3, t4, B_t34
        mre = sb(ps5, "mre", [128, TB], F32)
        mim = sb(ps5, "mim", [128, TB], F32)
        B_mre = Buf("mre")
        B_mim = Buf("mim")
        sre = sb(ps5, "sre", [128, TB], F32)
        sim = sb(ps5, "sim", [128, TB], F32)
        B_sre = Buf("sre")
        B_sim = Buf("sim")
        srb = sb(ps5, "srb", [128, TB], BF16)
        sib = sb(ps5, "sib", [128, TB], BF16)
        B_srb = Buf("srb")
        B_sib = Buf("sib")
        st = sb(ps5, "st", [128, 8], F32)
        B_st = Buf("st")
        gT = sb(ps5, "gT", [128, 4, NTOK], BF16)
        B_gT = Buf("gT")
        yp, y2, B_yp = bre, bim, B_b
        sreX = sb(ps5, "sreX", [128, TB], F32)
        simX = sb(ps5, "simX", [128, TB], F32)
        bre2, bim2, B_b2 = [bre, bre], [bim, bim], [B_b, B_b]
        sre2, sim2 = [sre, sreX], [sim, simX]
        B_sre2, B_sim2 = [B_sre, Buf("sreX")], [B_sim, Buf("simX")]
        ybanks = None
        for i in range(16):
            q = i % 4
            ut = i // 4
            if q == 0:
                ybanks = [(PS[b_], PSB[b_]) for b_ in range(NBLK)]
            th = s5p[:, 5, i:i + 1]
            S.op(dve, lambda h, th=th: h.tensor_scalar(out=ang[:], in0=iota[:], scalar1=th, scalar2=None, op0=ALU.mult),
                 reads=[B_const, B_s5p], writes=[B_t12])
            sincos(dve, ang[:], cs[:], sn[:], [B_t12], [B_tab], sct2, sca2, B_sc2)
            qre = s5p[:, 3, i:i + 1]
            qim = s5p[:, 4, i:i + 1]
            S.op(dve, lambda h, qre=qre: h.tensor_scalar(out=mqr[:], in0=cs[:], scalar1=qre, scalar2=None, op0=ALU.mult),
                 reads=[B_tab, B_s5p], writes=[B_tab])
            S.op(dve, lambda h, qim=qim: h.scalar_tensor_tensor(out=mqr[:], in0=sn[:], scalar=qim, in1=mqr[:], op0=ALU.mult, op1=ALU.add),
                 reads=[B_tab, B_s5p], writes=[B_tab])
            S.op(dve, lambda h, qim=qim: h.tensor_scalar(out=mqi[:], in0=cs[:], scalar1=qim, scalar2=None, op0=ALU.mult),
                 reads=[B_tab, B_s5p], writes=[B_tab])
            S.op(dve, lambda h, qre=qre: h.tensor_scalar(out=t1[:], in0=sn[:], scalar1=qre, scalar2=None, op0=ALU.mult),
                 reads=[B_tab, B_s5p], writes=[B_t12])
            S.op(dve, lambda h: h.tensor_sub(out=mqi[:], in0=mqi[:], in1=t1[:]), reads=[B_tab, B_t12], writes=[B_tab])
            S.op(dve, lambda h, i=i: h.tensor_copy(out=dec[:], in_=s5p[:, 0, i:i + 1].to_broadcast([128, TB])),
                 reads=[B_s5p], writes=[B_tab])
            S.op(dve, lambda h: h.memset(st[:], 0.0), writes=[B_st])
            cth = s5p[:, 1, i:i + 1]
            sth = s5p[:, 2, i:i + 1]
            LL = TB - 1
            S.op(dve, lambda h, sth=sth: h.tensor_scalar(out=st[:, 4:5], in0=sn[:, LL:LL + 1], scalar1=sth, scalar2=None, op0=ALU.mult),
                 reads=[B_tab, B_s5p, B_st], writes=[B_st])
            S.op(dve, lambda h, cth=cth: h.scalar_tensor_tensor(out=st[:, 6:7], in0=cs[:, LL:LL + 1], scalar=cth, in1=st[:, 4:5],
                                                                op0=ALU.mult, op1=ALU.subtract), reads=[B_tab, B_s5p, B_st], writes=[B_st])
            S.op(dve, lambda h, cth=cth: h.tensor_scalar(out=st[:, 5:6], in0=sn[:, LL:LL + 1], scalar1=cth, scalar2=None, op0=ALU.mult),
                 reads=[B_tab, B_s5p, B_st], writes=[B_st])
            S.op(dve, lambda h, sth=sth: h.scalar_tensor_tensor(out=st[:, 7:8], in0=cs[:, LL:LL + 1], scalar=sth, in1=st[:, 5:6],
                                                                op0=ALU.mult, op1=ALU.add), reads=[B_tab, B_s5p, B_st], writes=[B_st])
            for g in range(2 * NBLK):
                own = g >= NBLK
                blk = g - NBLK
                n_mx = mx_issue(2)
                if n_mx < 2 and cv_next[0] < CV_MAX:
                    cv_issue(2 - n_mx)
                pr, prb = PS[4], PSB[4]
                pi, pib = PS[5], PSB[5]
                par = g % 2
                bre_, bim_, B_b_ = bre2[par], bim2[par], B_b2[par]
                sre_, sim_, B_sre_, B_sim_ = sre2[par], sim2[par], B_sre2[par], B_sim2[par]
                mm(pr[:], wbb[:, 0, i, :], uT[:, ut, g * TB:(g + 1) * TB], True, True, [B_wb, B_uT[g]], prb)
                mm(pi[:], wbb[:, 1, i, :], uT[:, ut, g * TB:(g + 1) * TB], True, True, [B_wb, B_uT[g]], pib)
                S.op(dve, lambda h, pr=pr: h.tensor_mul(out=t1[:], in0=pr[:], in1=mqr[:]), reads=[prb, B_tab], writes=[B_t12])
                S.op(dve, lambda h, pi=pi: h.tensor_mul(out=t2[:], in0=pi[:], in1=mqi[:]), reads=[pib, B_tab], writes=[B_t12])
                S.op(dve, lambda h: h.tensor_sub(out=mre[:], in0=t1[:], in1=t2[:]), reads=[B_t12], writes=[B_mre])
                S.op(dve, lambda h, pr=pr: h.tensor_mul(out=t1[:], in0=pr[:], in1=mqi[:]), reads=[prb, B_tab], writes=[B_t12])
                S.op(dve, lambda h, pi=pi: h.tensor_mul(out=t2[:], in0=pi[:], in1=mqr[:]), reads=[pib, B_tab], writes=[B_t12])
                S.op(dve, lambda h: h.tensor_add(out=mim[:], in0=t1[:], in1=t2[:]), reads=[B_t12], writes=[B_mim])
                S.op(dve, lambda h, sre_=sre_: h.tensor_tensor_scan(out=sre_[:], data0=dec[:], data1=mre[:], initial=st[:, 2:3],
                                                                    op0=ALU.mult, op1=ALU.add), reads=[B_tab, B_mre, B_st], writes=[B_sre_])
                S.op(dve, lambda h, sim_=sim_: h.tensor_tensor_scan(out=sim_[:], data0=dec[:], data1=mim[:], initial=st[:, 3:4],
                                                                    op0=ALU.mult, op1=ALU.add), reads=[B_tab, B_mim, B_st], writes=[B_sim_])
                L = TB - 1
                S.op(dve, lambda h, sim_=sim_: h.tensor_scalar(out=st[:, 4:5], in0=sim_[:, L:L + 1], scalar1=st[:, 7:8], scalar2=None, op0=ALU.mult),
                     reads=[B_sim_, B_st], writes=[B_st])
                S.op(dve, lambda h, sre_=sre_: h.scalar_tensor_tensor(out=st[:, 2:3], in0=sre_[:, L:L + 1], scalar=st[:, 6:7], in1=st[:, 4:5],
                                                                      op0=ALU.mult, op1=ALU.subtract), reads=[B_sre_, B_st], writes=[B_st])
                S.op(dve, lambda h, sim_=sim_: h.tensor_scalar(out=st[:, 5:6], in0=sim_[:, L:L + 1], scalar1=st[:, 6:7], scalar2=None, op0=ALU.mult),
                     reads=[B_sim_, B_st], writes=[B_st])
                S.op(dve, lambda h, sre_=sre_: h.scalar_tensor_tensor(out=st[:, 3:4], in0=sre_[:, L:L + 1], scalar=st[:, 7:8], in1=st[:, 5:6],
                                                                      op0=ALU.mult, op1=ALU.add), reads=[B_sre_, B_st], writes=[B_st])
                if g == NBLK - 1:
                    S.op(dve, lambda h: h.tensor_scalar(out=st[:, 2:4], in0=st[:, 2:4], scalar1=sp_t[:, C_FLAG:C_FLAG + 1],
                                                        scalar2=None, op0=ALU.mult), reads=[B_st, B_const], writes=[B_st])
                if not own:
                    continue
                S.op(pool, lambda h, sre_=sre_: h.tensor_mul(out=t3[:], in0=sre_[:], in1=cs[:]), reads=[B_sre_, B_tab], writes=[B_t34])
                S.op(pool, lambda h, sim_=sim_: h.tensor_mul(out=t4[:], in0=sim_[:], in1=sn[:]), reads=[B_sim_, B_tab], writes=[B_t34])
                S.op(pool, lambda h: h.tensor_sub(out=srb[:], in0=t3[:], in1=t4[:]), reads=[B_t34], writes=[B_srb])
                S.op(pool, lambda h, sre_=sre_: h.tensor_mul(out=t3[:], in0=sre_[:], in1=sn[:]), reads=[B_sre_, B_tab], writes=[B_t34])
                S.op(pool, lambda h, sim_=sim_: h.tensor_mul(out=t4[:], in0=sim_[:], in1=cs[:]), reads=[B_sim_, B_tab], writes=[B_t34])
                S.op(pool, lambda h: h.tensor_add(out=sib[:], in0=t3[:], in1=t4[:]), reads=[B_t34], writes=[B_sib])
                yb, ybb = ybanks[blk]
                mm(yb[:], wcb[:, 0, i, :], srb[:], q == 0, False, [B_wb, B_srb], ybb)
                mm(yb[:], wcb[:, 1, i, :], sib[:], False, q == 3, [B_wb, B_sib], ybb)
            if q == 3:
                for blk in range(NBLK):
                    yb, ybb = ybanks[blk]
                    g = NBLK + blk
                    S.op(dve, lambda h, yb=yb, g=g: h.scalar_tensor_tensor(
                        out=yp[:], in0=uT[:, ut, g * TB:(g + 1) * TB], scalar=sp_t[:, C_SD + ut:C_SD + ut + 1], in1=yb[:],
                        op0=ALU.mult, op1=ALU.add), reads=[B_uT[g], ybb, B_const], writes=[B_yp])
                    S.op(dve, lambda h: h.tensor_mul(out=y2[:], in0=yp[:], in1=yp[:]), reads=[B_yp], writes=[B_yp])
                    S.op(dve, lambda h: h.tensor_scalar(out=y2[:], in0=y2[:], scalar1=0.044715, scalar2=1.0, op0=ALU.mult, op1=ALU.add),
                         reads=[B_yp], writes=[B_yp])
                    S.op(dve, lambda h: h.tensor_mul(out=y2[:], in0=y2[:], in1=yp[:]), reads=[B_yp], writes=[B_yp])
                    S.op(act, lambda h: h.activation(out=y2[:], in_=y2[:], func=AF.Sigmoid, scale=1.5957691216057308),
                         reads=[B_yp], writes=[B_yp])
                    S.op(dve, lambda h, blk=blk: h.tensor_mul(out=gT[:, ut, blk * TB:(blk + 1) * TB], in0=y2[:], in1=yp[:]),
                         reads=[B_yp], writes=[B_gT])
        wsg_t, B_wsg = wload([(kview(w_sg, 4), 4, 512)])
        wsg = wsg_t[:, 0:2048].rearrange("p (a b) -> p a b", a=4)
        for m in range(4):
            for blk in range(NBLK):
                ps_, pb = pbank()
                for kt in range(4):
                    mm(ps_[:], wsg[:, kt, m * 128:(m + 1) * 128], gT[:, kt, blk * TB:(blk + 1) * TB], kt == 0, kt == 3,
                       [B_wsg, B_gT], pb)
                S.op(act, lambda h, ps_=ps_: h.activation(out=yp[:], in_=ps_[:], func=AF.Sigmoid), reads=[pb], writes=[B_yp])
                S.op(dve, lambda h, m=m, blk=blk: h.tensor_mul(out=s5o[:, m, blk * TB:(blk + 1) * TB],
                                                              in0=gT[:, m, blk * TB:(blk + 1) * TB], in1=yp[:]),
                     reads=[B_yp, B_gT], writes=[B_s5o])
        dump("uT", uT[:], [128, 4, 2 * NTOK], BF16, B_uT)
        dump("gT", gT[:], [128, 4, NTOK], BF16, [B_gT])
        dump("s5o", s5o[:], [128, 4, NTOK], BF16, [B_s5o])
        S.barrier([dbg_ch])
    pus.close()

    mx_convert_all()
    mx_finalize()
    ch_x1 = S.chan()
    ch_sc = S.chan()
    B_x1d = Buf("x1d")
    with contextlib.ExitStack() as pm:
        alloc_xt(pm, 1)
        big = sb(pm, "big", [128, 4 * D], F32)
        cv = big[:, 0:8 * TB].rearrange("p (a b) -> p a b", a=8)
        sq = big[:, 8 * TB:16 * TB].rearrange("p (a b) -> p a b", a=8)
        res = big[:].rearrange("p (a b) -> p a b", a=4)
        B_cv = Buf("cv")
        B_sq = Buf("sq")
        B_res = [Buf("res%d" % i) for i in range(4)]
        b16a = sb(pm, "b16a", [128, 4 * D], BF16)
        xn2 = b16a[:].rearrange("p (a b) -> p a b", a=4)
        mgT = b16a[:].rearrange("p (a b) -> p a b", a=16)
        B_xn2 = Buf("xn2")
        B_mg = Buf("mgT")
        vtail = sb(pm, "vtail", [128, 8, 32], BF16)
        B_vt = Buf("vtail")
        flag = sp_t[:, C_FLAG:C_FLAG + 1]

        for blk in range(NBLK):
          with contextlib.ExitStack() as sa:
            hTb = sb(sa, "hTb%d" % blk, [128, 16, TB], BF16)
            hb = Buf("hTb")
            coT = sb(sa, "coT%d" % blk, [128, 8, TB], BF16)
            B_co = Buf("coT")
            hsl = lambda kt, hTb=hTb: hTb[:, kt, :]
            make_hT(x_own, blk, xn2, B_xn2, hTb, hb)
            S.barrier()
            sa12 = contextlib.ExitStack()
            vT = sb(sa12, "vT%d" % blk, [128, 8, 32 + TB], BF16)
            B_vT = Buf("vT")
            asb = sb(sa12, "asb%d" % blk, [128, TB], F32)
            gsb = sb(sa12, "gsb%d" % blk, [128, TB], F32)
            B_ag = Buf("ag")
            diag = sb(sa12, "diag%d" % blk, [128, 31, 128], BF16)
            B_dg = Buf("diag")
            mean, rstd, B_mr = asb, gsb, B_ag
            ctmp = xts["t"][0][:, 0:TB]
            B_ct = xts["b"][0]
            if blk > 0:
                S.op(dve, lambda h: h.tensor_copy(out=vT[:, :, 0:32], in_=vtail[:]), reads=[B_vt], writes=[B_vT])
            def conv_ld(kp):
                return mload(MX_CONV + kp)

            def conv_use(t, tb, kp, blk=blk, hb=hb, hsl=hsl):
                wa = t[:, 0:2048].rearrange("p (a b) -> p a b", a=16)
                wg = t[:, 2048:4096].rearrange("p (a b) -> p a b", a=16)
                pa_, pab = pbank()
                pg_, pgb = pbank()
                for kt in range(16):
                    mm(pa_[:], wa[:, kt, :], hsl(kt), kt == 0, kt == 15, [tb, hb], pab)
                for kt in range(16):
                    mm(pg_[:], wg[:, kt, :], hsl(kt), kt == 0, kt == 15, [tb, hb], pgb)
                ba = sp_t[:, C_BIN + 4 + kp:C_BIN + 5 + kp]
                bg = sp_t[:, C_BIN + 12 + kp:C_BIN + 13 + kp]
                S.op(act, lambda h: h.activation(out=asb[:], in_=pa_[:], func=AF.Identity, bias=ba), reads=[pab, B_const], writes=[B_ag])
                S.op(act, lambda h: h.activation(out=gsb[:], in_=pg_[:], func=AF.Sigmoid, bias=bg), reads=[pgb, B_const], writes=[B_ag])
                S.op(dve, lambda h: h.tensor_mul(out=vT[:, kp, 32:32 + TB], in0=asb[:], in1=gsb[:]), reads=[B_ag], writes=[B_vT])
                if blk == 0:
                    ph_, phb = pbank()
                    for kt in range(16):
                        mm(ph_[:, 0:32], wa[:, kt, :], hT_halo[:, kt, :], kt == 0, kt == 15, [tb, B_halo], phb)
                    for kt in range(16):
                        mm(ph_[:, 32:64], wg[:, kt, :], hT_halo[:, kt, :], kt == 0, kt == 15, [tb, B_halo], phb)
                    S.op(act, lambda h: h.activation(out=asb[:, 0:32], in_=ph_[:, 0:32], func=AF.Identity, bias=ba),
                         reads=[phb, B_const], writes=[B_ag])
                    S.op(act, lambda h: h.activation(out=gsb[:, 0:32], in_=ph_[:, 32:64], func=AF.Sigmoid, bias=bg),
                         reads=[phb, B_const], writes=[B_ag])
                    S.op(dve, lambda h: h.scalar_tensor_tensor(out=vT[:, kp, 0:32], in0=asb[:, 0:32], scalar=flag, in1=gsb[:, 0:32],
                                                               op0=ALU.mult, op1=ALU.mult), reads=[B_ag, B_const], writes=[B_vT])

            pipeline([((lambda kp=kp: conv_ld(kp)), (lambda t, tb, kp=kp: conv_use(t, tb, kp))) for kp in range(8)])

            for kp in range(8):
                S.op(dve, lambda h, kp=kp: h.tensor_tensor(
                    out=diag[:], in0=identb[:].unsqueeze(1).to_broadcast([128, 31, 128]),
                    in1=sp_t[:, C_DW + kp * 31:C_DW + (kp + 1) * 31].unsqueeze(2).to_broadcast([128, 31, 128]), op=ALU.mult),
                    reads=[B_const], writes=[B_dg])
                pc_, pcb = pbank()
                for tap in range(31):
                    mm(pc_[:], diag[:, tap, :], vT[:, kp, 2 + tap:2 + tap + TB], tap == 0, tap == 30, [B_dg, B_vT], pcb)
                bb = sp_t[:, C_DWB + kp:C_DWB + kp + 1]
                S.op(act, lambda h, kp=kp, pc_=pc_, bb=bb: h.activation(out=cv[:, kp, :], in_=pc_[:], func=AF.Identity, bias=bb),
                     reads=[pcb, B_const], writes=[B_cv])
                S.op(act, lambda h, kp=kp, pc_=pc_, bb=bb: h.activation(out=sq[:, kp, :], in_=pc_[:], func=AF.Square, bias=bb),
                     reads=[pcb, B_const], writes=[B_sq])
            S.op(dve, lambda h: h.tensor_copy(out=vtail[:], in_=vT[:, :, TB:TB + 32]), reads=[B_vT], writes=[B_vt])
            pm_, pmb = pbank()
            pq_, pqb = pbank()
            for kp in range(8):
                mm(pm_[:], onesf, cv[:, kp, :], kp == 0, kp == 7, [B_const, B_cv], pmb)
            for kp in range(8):
                mm(pq_[:], onesf, sq[:, kp, :], kp == 0, kp == 7, [B_const, B_sq], pqb)
            S.op(dve, lambda h: h.tensor_scalar(out=mean[:], in0=pm_[:], scalar1=1.0 / 1024, scalar2=None, op0=ALU.mult),
                 reads=[pmb], writes=[B_mr])
            S.op(dve, lambda h: h.tensor_mul(out=ctmp[:], in0=mean[:], in1=mean[:]), reads=[B_mr], writes=[B_ct])
            S.op(dve, lambda h: h.scalar_tensor_tensor(out=rstd[:], in0=pq_[:], scalar=1.0 / 1024, in1=ctmp[:], op0=ALU.mult, op1=ALU.subtract),
                 reads=[pqb, B_ct], writes=[B_mr])
            S.op(dve, lambda h: h.tensor_scalar_add(out=rstd[:], in0=rstd[:], scalar1=EPS), reads=[B_mr], writes=[B_mr])
            S.op(act, lambda h: h.activation(out=rstd[:], in_=rstd[:], func=AF.Sqrt), reads=[B_mr], writes=[B_mr])
            S.op(dve, lambda h: h.reciprocal(out=rstd[:], in_=rstd[:]), reads=[B_mr], writes=[B_mr])
            for kp in range(8):
                S.op(dve, lambda h, kp=kp: h.tensor_sub(out=ctmp[:], in0=cv[:, kp, :], in1=mean[:]), reads=[B_cv, B_mr], writes=[B_ct])
                S.op(dve, lambda h: h.tensor_mul(out=ctmp[:], in0=ctmp[:], in1=rstd[:]), reads=[B_ct, B_mr], writes=[B_ct])
                S.op(act, lambda h, kp=kp: h.activation(out=coT[:, kp, :], in_=ctmp[:], func=AF.Silu,
                                                        scale=sp_t[:, C_LNG + kp:C_LNG + kp + 1], bias=sp_t[:, C_LNB + kp:C_LNB + kp + 1]),
                     reads=[B_ct, B_const], writes=[B_co])

            S.barrier()
            sa12.close()
            sa3 = contextlib.ExitStack()
            sg1 = sb(sa3, "sg1%d" % blk, [128, TB], F32)
            sg2 = sb(sa3, "sg2%d" % blk, [128, TB], F32)
            B_sg = Buf("sg")
            def gate_ld(k):
                return mload(MX_GATE + k)

            def proj_ld(k):
                return mload(MX_PROJ + k)

            def gate_use(t, tb, k, blk=blk, hb=hb, hsl=hsl):
                w1 = t[:, 0:2048].rearrange("p (a b) -> p a b", a=16)
                w2 = t[:, 2048:4096].rearrange("p (a b) -> p a b", a=16)
                p1, p1b = pbank()
                p2, p2b = pbank()
                for kt in range(16):
                    mm(p1[:], w1[:, kt, :], hsl(kt), kt == 0, kt == 15, [tb, hb], p1b)
                for kt in range(16):
                    mm(p2[:], w2[:, kt, :], hsl(kt), kt == 0, kt == 15, [tb, hb], p2b)
                b1 = sp_t[:, C_BIN + 20 + k:C_BIN + 21 + k]
                b2 = sp_t[:, C_BIN + 36 + k:C_BIN + 37 + k]
                S.op(act, lambda h: h.activation(out=sg1[:], in_=p1[:], func=AF.Sigmoid, bias=b1), reads=[p1b, B_const], writes=[B_sg])
                S.op(act, lambda h: h.activation(out=sg2[:], in_=p2[:], func=AF.Sigmoid, bias=b2), reads=[p2b, B_const], writes=[B_sg])

            def proj_use(t, tb, k, blk=blk):
                wu_ = t[:, 0:512].rearrange("p (a b) -> p a b", a=4)
                wc_ = t[:, 512:1536].rearrange("p (a b) -> p a b", a=8)
                p3, p3b = pbank()
                p4, p4b = pbank()
                for kt in range(4):
                    mm(p3[:], wu_[:, kt, :], s5o[:, kt, blk * TB:(blk + 1) * TB], kt == 0, kt == 3, [tb, B_s5o], p3b)
                for kt in range(8):
                    mm(p4[:], wc_[:, kt, :], coT[:, kt, :], kt == 0, kt == 7, [tb, B_co], p4b)
                S.op(dve, lambda h: h.tensor_mul(out=sg1[:], in0=sg1[:], in1=p3[:]), reads=[B_sg, p3b], writes=[B_sg])
                S.op(dve, lambda h: h.tensor_mul(out=sg2[:], in0=sg2[:], in1=p4[:]), reads=[B_sg, p4b], writes=[B_sg])
                S.op(dve, lambda h: h.tensor_add(out=mgT[:, k, :], in0=sg1[:], in1=sg2[:]), reads=[B_sg], writes=[B_mg])

            units = []
            for k in range(16):
                units.append(((lambda k=k: gate_ld(k)), (lambda t, tb, k=k: gate_use(t, tb, k))))
                units.append(((lambda k=k: proj_ld(k)), (lambda t, tb, k=k: proj_use(t, tb, k))))
            pipeline(units)

            if blk == 0:
                dump("coT", coT[:], [128, 8, TB], BF16, [B_co])
                dump("mgT", mgT, [128, 16, TB], BF16, [B_mg])
            S.barrier([dbg_ch])
            sa3.close()
          with contextlib.ExitStack() as sb_:
            lnb1 = sb(sb_, "lnb1%d" % blk, [128, 2, D], F32)
            B_lnb = Buf("lnb1")
            ch_l = S.chan()
            S.dma(sp, lambda h: h.dma_start(out=lnb1[:, 0, :], in_=lnbc_d[0]), ch_l, writes=[B_lnb])
            S.dma(sp, lambda h: h.dma_start(out=lnb1[:, 1, :], in_=lnbc_d[1]), ch_l, writes=[B_lnb])
            B_lnb.w = (ch_l.sem, ch_l.cnt)
            x1t = sb(sb_, "x1t%d" % blk, [128, D], F32)
            B_x1t = Buf("x1t")
            h2, B_h2 = x1t, B_x1t
            h2b = sb(sb_, "h2b%d" % blk, [128, D], BF16)
            B_h2b = Buf("h2b")
            h2T = sb(sb_, "h2T%d" % blk, [128, 16, 128], F32)
            B_h2T = Buf("h2T")
            rt = sb(sb_, "rt%d" % blk, [128, 16, 64], F32)
            B_rt = Buf("rt")
            ohb = sb(sb_, "ohb%d" % blk, [128, 64], BF16)
            B_oh = Buf("ohb")
            wr = sb(sb_, "wr%d" % blk, [128, 16, 72], F32)
            brt = sb(sb_, "brt%d" % blk, [128, 72], F32)
            B_wr = Buf("wr")
            S.dma(sp, lambda h: h.dma_start(out=wr[:], in_=kview(w_rt, 16)), ch_l, writes=[B_wr])
            S.dma(sp, lambda h: h.dma_start(out=brt[:], in_=brt_d), ch_l, writes=[B_wr])
            B_wr.w = (ch_l.sem, ch_l.cnt)
            B_lnb.w = (ch_l.sem, ch_l.cnt)
            def wo_ld(fb):
                return mload(MX_WO + fb)

            def wo_use(t, tb, fb):
                wo = t[:, 0:4096].rearrange("p (a b) -> p a b", a=16)
                for tt in range(4):
                    po, pob = pbank()
                    for kt in range(16):
                        mm(po[:, 0:256], mgT[:, kt, tt * 128:(tt + 1) * 128], wo[:, kt, :], kt == 0, kt == 15, [tb, B_mg], pob)
                    S.op(dve, lambda h, tt=tt, po=po: h.tensor_mul(out=res[:, tt, fb * 256:(fb + 1) * 256], in0=po[:, 0:256],
                                                                  in1=modbc[:, 0, fb * 256:(fb + 1) * 256]),
                         reads=[pob, B_modbc], writes=[B_res[tt]])

            pipeline([((lambda fb=fb: wo_ld(fb)), (lambda t, tb, fb=fb: wo_use(t, tb, fb))) for fb in range(8)])

            for tt in range(4):
                T = blk * 4 + tt
                rows = slice(blk * TB + tt * 128, blk * TB + (tt + 1) * 128)
                t, tb = load_x(x_own[rows, :])
                r_ = res[:, tt, :]
                rb_ = B_res[tt]
                S.op(dve, lambda h, t=t, r_=r_: h.scalar_tensor_tensor(out=r_, in0=t[:], scalar=ALPHA, in1=r_, op0=ALU.mult, op1=ALU.add),
                     reads=[tb, rb_], writes=[rb_])
                ln_stats(r_, rb_)
                S.op(dve, lambda h, r_=r_: h.tensor_scalar(out=r_, in0=r_, scalar1=lnmv[:, 0:1], scalar2=lnmv[:, 2:3],
                                                         op0=ALU.subtract, op1=ALU.mult), reads=[rb_, B_ln], writes=[rb_])
                S.op(dve, lambda h, r_=r_: h.tensor_mul(out=r_, in0=r_, in1=lnb1[:, 0, :]), reads=[rb_, B_lnb], writes=[rb_])
                S.op(dve, lambda h, r_=r_: h.tensor_add(out=x1t[:], in0=r_, in1=lnb1[:, 1, :]), reads=[rb_, B_lnb], writes=[B_x1t])
                S.dma(sp, lambda h, rows=rows: h.dma_start(out=x1_d[rows, :], in_=x1t[:]), ch_x1, reads=[B_x1t], writes=[B_x1d])
                ln_stats(x1t[:], B_x1t)
                S.op(dve, lambda h: h.tensor_scalar(out=h2[:], in0=x1t[:], scalar1=lnmv[:, 0:1], scalar2=lnmv[:, 2:3],
                                                    op0=ALU.subtract, op1=ALU.mult), reads=[B_x1t, B_ln], writes=[B_h2])
                S.op(dve, lambda h: h.tensor_mul(out=h2[:], in0=h2[:], in1=modbc[:, 2, :]), reads=[B_h2, B_modbc], writes=[B_h2])
                S.op(dve, lambda h: h.tensor_add(out=h2[:], in0=h2[:], in1=modbc[:, 3, :]), reads=[B_h2, B_modbc], writes=[B_h2])
                S.op(act, lambda h: h.activation(out=h2b[:], in_=h2[:], func=AF.Copy), reads=[B_h2], writes=[B_h2b])
                for k4 in range(4):
                    pt_, ptb_ = pbank()
                    for j in range(4):
                        kt = k4 * 4 + j
                        S.op(pe, lambda h, kt=kt, j=j, pt_=pt_: h.transpose(out=pt_[:, j * 128:(j + 1) * 128],
                                                                        in_=h2[:, kt * 128:(kt + 1) * 128], identity=identf),
                             reads=[B_h2, B_const], writes=[ptb_])
                    S.op(act, lambda h, k4=k4, pt_=pt_: h.activation(
                        out=h2T[:, k4 * 4:(k4 + 1) * 4, :].rearrange("p a b -> p (a b)"), in_=pt_[:], func=AF.Copy),
                        reads=[ptb_], writes=[B_h2T])
                pl_, plb = pbank()
                for kt in range(16):
                    mm(pl_[:, 0:72], h2T[:, kt, :], wr[:, kt, :], kt == 0, kt == 15, [B_h2T, B_wr], plb)
                lg = rt[:, 0:2, :].rearrange("p a b -> p (a b)")[:, 0:72]
                V = lambda r, n=8: rt[:, r, 0:n]
                S.op(dve, lambda h: h.tensor_add(out=lg, in0=pl_[:, 0:72], in1=brt[:]), reads=[plb, B_wr], writes=[B_rt])
                o = lambda fn, **kw: S.op(dve, fn, reads=[B_rt] + kw.get("r", []), writes=[B_rt] + kw.get("w", []))
                gl = lg[:, 0:8]
                el3 = lg[:, 8:72].rearrange("p (g e) -> p g e", g=8)
                o(lambda h: h.reduce_max(out=V(2, 1), in_=gl, axis=AX.X))
                o(lambda h: h.tensor_scalar(out=V(3), in0=gl, scalar1=V(2, 1), scalar2=None, op0=ALU.is_equal))
                o(lambda h: h.tensor_scalar(out=V(4), in0=gl, scalar1=V(2, 1), scalar2=None, op0=ALU.subtract))
                S.op(act, lambda h: h.activation(out=V(4), in_=V(4), func=AF.Exp), reads=[B_rt], writes=[B_rt])
                o(lambda h: h.reduce_sum(out=V(5, 1), in_=V(4), axis=AX.X))
                o(lambda h: h.reciprocal(out=V(5, 1), in_=V(5, 1)))
                prod = rt[:, 6, :].rearrange("p (g e) -> p g e", g=8)
                o(lambda h: h.tensor_tensor(out=prod, in0=el3, in1=V(3).unsqueeze(2).to_broadcast([128, 8, 8]), op=ALU.mult))
                o(lambda h: h.reduce_sum(out=V(7), in_=rt[:, 6, :].rearrange("p (g e) -> p e g", g=8), axis=AX.X))
                o(lambda h: h.reduce_max(out=V(8, 1), in_=V(7), axis=AX.X))
                o(lambda h: h.tensor_scalar(out=V(9), in0=V(7), scalar1=V(8, 1), scalar2=None, op0=ALU.is_equal))
                o(lambda h: h.scalar_tensor_tensor(out=V(10), in0=V(9), scalar=-1e30, in1=V(7), op0=ALU.mult, op1=ALU.add))
                o(lambda h: h.reduce_max(out=V(11, 1), in_=V(10), axis=AX.X))
                o(lambda h: h.tensor_scalar(out=V(12), in0=V(10), scalar1=V(11, 1), scalar2=None, op0=ALU.is_equal))
                o(lambda h: h.tensor_sub(out=V(13, 1), in0=V(11, 1), in1=V(8, 1)))
                S.op(act, lambda h: h.activation(out=V(13, 1), in_=V(13, 1), func=AF.Exp), reads=[B_rt], writes=[B_rt])
                o(lambda h: h.tensor_scalar_add(out=V(14, 1), in0=V(13, 1), scalar1=1.0))
                o(lambda h: h.reciprocal(out=V(14, 1), in_=V(14, 1)))
                o(lambda h: h.tensor_mul(out=V(15, 1), in0=V(13, 1), in1=V(14, 1)))
                o(lambda h: h.tensor_mul(out=rinfo[:, T, 2:3], in0=V(14, 1), in1=V(5, 1)), w=[B_rinfo])
                o(lambda h: h.tensor_mul(out=rinfo[:, T, 3:4], in0=V(15, 1), in1=V(5, 1)), w=[B_rinfo])
                oh1 = rt[:, 0, :].rearrange("p (g e) -> p g e", g=8)
                oh2 = rt[:, 1, :].rearrange("p (g e) -> p g e", g=8)
                o(lambda h: h.tensor_tensor(out=oh1, in0=V(3).unsqueeze(2).to_broadcast([128, 8, 8]),
                                            in1=V(9).unsqueeze(1).to_broadcast([128, 8, 8]), op=ALU.mult))
                o(lambda h: h.tensor_tensor(out=oh2, in0=V(3).unsqueeze(2).to_broadcast([128, 8, 8]),
                                            in1=V(12).unsqueeze(1).to_broadcast([128, 8, 8]), op=ALU.mult))
                S.op(dve, lambda h: h.tensor_add(out=ohb[:], in0=rt[:, 0, :], in1=rt[:, 1, :]), reads=[B_rt], writes=[B_oh])
                pc2, pc2b = pbank()
                mm(pc2[:, 0:64], trib[:], ohb[:], True, True, [B_const, B_oh], pc2b)
                pt2, pt2b = pbank()
                mm(pt2[:, 0:64], onesb[:], ohb[:], True, True, [B_const, B_oh], pt2b)
                S.op(dve, lambda h: h.tensor_add(out=rt[:, 6, :], in0=pc2[:, 0:64], in1=basebc[:]), reads=[pc2b, B_base, B_rt], writes=[B_rt])
                S.op(dve, lambda h: h.tensor_add(out=basebc[:], in0=basebc[:], in1=pt2[:, 0:64]), reads=[pt2b, B_rt], writes=[B_base])
                for s_, ohr in ((0, 0), (1, 1)):
                    o(lambda h, ohr=ohr: h.tensor_mul(out=rt[:, 7, :], in0=rt[:, 6, :], in1=rt[:, ohr, :]))
                    o(lambda h: h.reduce_sum(out=V(8, 1), in_=rt[:, 7, :], axis=AX.X))
                    o(lambda h, ohr=ohr: h.tensor_mul(out=rt[:, 7, :], in0=iota[:, 0:64], in1=rt[:, ohr, :]), r=[B_const])
                    o(lambda h: h.reduce_sum(out=V(9, 1), in_=rt[:, 7, :], axis=AX.X))
                    o(lambda h: h.tensor_scalar(out=V(10, 1), in0=V(8, 1), scalar1=float(CAP), scalar2=1.0e6, op0=ALU.is_ge, op1=ALU.mult))
                    o(lambda h: h.scalar_tensor_tensor(out=V(9, 1), in0=V(9, 1), scalar=float(CAP), in1=V(8, 1), op0=ALU.mult, op1=ALU.add))
                    o(lambda h, s_=s_: h.tensor_add(out=rinfo[:, T, s_:s_ + 1], in0=V(9, 1), in1=V(10, 1)), w=[B_rinfo])
                S.op(dve, lambda h, T=T: h.tensor_copy(out=rdest[:, T, :], in_=rinfo[:, T, 0:2]), reads=[B_rinfo], writes=[B_rinfo])
                for s_ in range(2):
                    S.dma(pool, lambda h, T=T, s_=s_: h.indirect_dma_start(
                        out=xdisp[:, :], out_offset=bass.IndirectOffsetOnAxis(ap=rdest[:, T, s_:s_ + 1], axis=0),
                        in_=h2b[:, :], in_offset=None, bounds_check=bc_reg, oob_is_err=False),
                        ch_xd, reads=[B_h2b, B_rinfo], writes=[B_xd])
            S.barrier([ch_l])
        S.barrier([ch_x1, ch_xd])

    cv_finalize()
    ch_y = S.chan()
    B_yd = Buf("ydisp")
    with contextlib.ExitStack() as pe_:
        xg = [sb(pe_, "xg%d" % i, [128, D], BF16) for i in range(2)]
        B_xg = [Buf("xg%d" % i) for i in range(2)]
        C_xg = [S.chan() for _ in range(2)]
        xgT = sb(pe_, "xgT", [128, 16, 128], BF16)
        B_xgT = Buf("xgT")
        sil = sb(pe_, "sil", [128, 512], F32)
        B_sil = Buf("sil")
        actb = sb(pe_, "actb", [128, 512], BF16)
        B_actb = Buf("actb")
        actT = sb(pe_, "actT", [128, 4, 128], BF16)
        B_actT = Buf("actT")
        yo = [sb(pe_, "yo%d" % i, [128, D], F32) for i in range(2)]
        B_yo = [Buf("yo%d" % i) for i in range(2)]

        def ex_load(e):
            i = e % 2
            S.dma(sp, lambda h: h.dma_start(out=xg[i][:], in_=xdisp[e * CAP:(e + 1) * CAP, :]), C_xg[i], reads=[B_xd], writes=[B_xg[i]])

        ex_load(0)
        state = {}
        units = []
        for e in range(NE):
            def tr_in(e):
                i = e % 2
                if e + 1 < NE:
                    ex_load(e + 1)
                for k4 in range(4):
                    ptile, ptb = tbank()
                    for j in range(4):
                        kt = k4 * 4 + j
                        S.op(pe, lambda h, kt=kt, j=j, ptile=ptile: h.transpose(out=ptile[:, j * 128:(j + 1) * 128],
                                                                          in_=xg[i][:, kt * 128:(kt + 1) * 128], identity=identb[:]),
                             reads=[B_xg[i], B_const], writes=[ptb])
                    cast(xgT[:, k4 * 4:(k4 + 1) * 4, :].rearrange("p a b -> p (a b)"), ptile[:, 0:512], [ptb], [B_xgT], eng=[act, dve][k4 % 2])
                if e == 0:
                    dump("xg0", xg[i][:], [128, D], BF16, [B_xg[i]])
                    dump("xgT0", xgT[:], [128, 16, 128], BF16, [B_xgT])
                state["pg"] = pbank()
                state["pu"] = pbank()

            def gu_use(t, tb, which, hf, e=e):
                if which == 0 and hf == 0:
                    tr_in(e)
                w = t[:, 0:4096].rearrange("p (a b) -> p a b", a=16)
                ps_, pb = state["pg"] if which == 0 else state["pu"]
                for kt in range(16):
                    mm(ps_[:, hf * 256:(hf + 1) * 256], xgT[:, kt, :], w[:, kt, :], kt == 0, kt == 15, [tb, B_xgT], pb)
                if which == 1 and hf == 1:
                    pg_, pgb = state["pg"]
                    S.op(act, lambda h: h.activation(out=sil[:], in_=pg_[:], func=AF.Silu), reads=[pgb], writes=[B_sil])
                    S.op(dve, lambda h: h.tensor_mul(out=actb[:], in0=sil[:], in1=ps_[:]), reads=[B_sil, pb], writes=[B_actb])
                    ptile, ptb = tbank()
                    for j in range(4):
                        S.op(pe, lambda h, j=j, ptile=ptile: h.transpose(out=ptile[:, j * 128:(j + 1) * 128],
                                                                     in_=actb[:, j * 128:(j + 1) * 128], identity=identb[:]),
                             reads=[B_actb, B_const], writes=[ptb])
                    cast(actT[:].rearrange("p a b -> p (a b)"), ptile[:, 0:512], [ptb], [B_actT], eng=act)

            def dn_use(t, tb, hf, e=e):
                w = t[:, 0:4096].rearrange("p (a b) -> p a b", a=4)
                i = e % 2
                for nb in range(2):
                    ps_, pb = pbank()
                    for kt in range(4):
                        mm(ps_[:], actT[:, kt, :], w[:, kt, nb * 512:(nb + 1) * 512], kt == 0, kt == 3, [tb, B_actT], pb)
                    c0 = hf * 1024 + nb * 512
                    cast(yo[i][:, c0:c0 + 512], ps_[:], [pb], [B_yo[i]], eng=[act, dve][nb])
                if hf == 1:
                    if e == 0:
                        dump("yo0", yo[i][:], [128, D], F32, [B_yo[i]])
                        dump("actT0", actT[:], [128, 4, 128], BF16, [B_actT])
                    for hc in range(2):
                        S.dma(sp, lambda h, hc=hc: h.dma_start(out=ydh[hc][e * CAP:(e + 1) * CAP, :], in_=yo[i][:, hc * 1024:(hc + 1) * 1024]),
                              ch_y, reads=[B_yo[i]], writes=[B_yd])

            for which in (0, 1):
                for hf in range(2):
                    units.append(((lambda e=e, which=which, hf=hf: eload(e * 6 + which * 2 + hf)),
                                  (lambda t, tb, which=which, hf=hf, f=gu_use: f(t, tb, which, hf))))
            for hf in range(2):
                units.append(((lambda e=e, hf=hf: eload(e * 6 + 4 + hf)),
                              (lambda t, tb, hf=hf, f=dn_use: f(t, tb, hf))))
        pipeline(units)
        S.barrier([ch_y])

    ch_o = [S.chan() for _ in range(2)]
    with contextlib.ExitStack() as pf:
        alloc_xt(pf, 2)
        ya_ = [sb(pf, "ya%d" % i, [128, D], F32) for i in range(2)]
        yb_ = [sb(pf, "yb%d" % i, [128, D], F32) for i in range(2)]
        B_ya_ = [Buf("ya%d" % i) for i in range(2)]
        B_yb_ = [Buf("yb%d" % i) for i in range(2)]
        ch_g = [S.chan() for _ in range(4)]
        ot = [sb(pf, "ot%d" % i, [128, D], F32) for i in range(2)]
        B_ot = [Buf("ot%d" % i) for i in range(2)]
        lnb2 = sb(pf, "lnb2", [128, 2, D], F32)
        B_lnb2 = Buf("lnb2")
        ch_f = S.chan()
        S.dma(sp, lambda h: h.dma_start(out=lnb2[:, 0, :], in_=lnbc_d[2]), ch_f, writes=[B_lnb2])
        S.dma(sp, lambda h: h.dma_start(out=lnb2[:, 1, :], in_=lnbc_d[3]), ch_f, writes=[B_lnb2])
        B_lnb2.w = (ch_f.sem, ch_f.cnt)

        def fetch(T):
            k = T % 2
            rows = slice(T * 128, (T + 1) * 128)
            S.op(pool, lambda h: h.memset(ya_[k][:], 0.0), writes=[B_ya_[k]])
            S.op(pool, lambda h: h.memset(yb_[k][:], 0.0), writes=[B_yb_[k]])
            for s_, (dst, db, chn) in enumerate(((ya_[k], B_ya_[k], ch_g[2 * k]), (yb_[k], B_yb_[k], ch_g[2 * k + 1]))):
                for hc in range(2):
                    S.dma(pool, lambda h, dst=dst, s_=s_, hc=hc: h.indirect_dma_start(
                        out=dst[:, hc * 1024:(hc + 1) * 1024], out_offset=None, in_=ydh[hc][:, :],
                        in_offset=bass.IndirectOffsetOnAxis(ap=rdest[:, T, s_:s_ + 1], axis=0),
                        bounds_check=bc_reg, oob_is_err=False), chn, reads=[B_yd, B_rinfo], writes=[db])
            return load_x(x1_d[rows, :])

        pend = fetch(0)
        for T in range(16):
            rows = slice(T * 128, (T + 1) * 128)
            nxt = fetch(T + 1) if T + 1 < 16 else None
            t, tb = pend
            pend = nxt
            ya, yb2, B_ya, B_yb = ya_[T % 2], yb_[T % 2], B_ya_[T % 2], B_yb_[T % 2]
            o_ = ot[T % 2]
            ob = B_ot[T % 2]
            S.op(dve, lambda h, ya=ya: h.tensor_scalar(out=ya[:], in0=ya[:], scalar1=rinfo[:, T, 2:3], scalar2=None, op0=ALU.mult),
                 reads=[B_ya, B_rinfo], writes=[B_ya])
            S.op(dve, lambda h, ya=ya, yb2=yb2: h.scalar_tensor_tensor(out=ya[:], in0=yb2[:], scalar=rinfo[:, T, 3:4], in1=ya[:], op0=ALU.mult, op1=ALU.add),
                 reads=[B_ya, B_yb, B_rinfo], writes=[B_ya])
            S.op(dve, lambda h, ya=ya: h.tensor_mul(out=ya[:], in0=ya[:], in1=modbc[:, 1, :]), reads=[B_ya, B_modbc], writes=[B_ya])
            S.op(dve, lambda h, t=t, ya=ya: h.scalar_tensor_tensor(out=ya[:], in0=t[:], scalar=ALPHA, in1=ya[:], op0=ALU.mult, op1=ALU.add),
                 reads=[tb, B_ya], writes=[B_ya])
            ln_stats(ya[:], B_ya)
            S.op(dve, lambda h, ya=ya: h.tensor_scalar(out=ya[:], in0=ya[:], scalar1=lnmv[:, 0:1], scalar2=lnmv[:, 2:3],
                                                       op0=ALU.subtract, op1=ALU.mult), reads=[B_ya, B_ln], writes=[B_ya])
            S.op(dve, lambda h, ya=ya: h.tensor_mul(out=ya[:], in0=ya[:], in1=lnb2[:, 0, :]), reads=[B_ya, B_lnb2], writes=[B_ya])
            S.op(dve, lambda h, o_=o_, ya=ya: h.tensor_add(out=o_[:], in0=ya[:], in1=lnb2[:, 1, :]), reads=[B_ya, B_lnb2], writes=[ob])
            S.dma(sp, lambda h, o_=o_, rows=rows: h.dma_start(out=out[rows, :], in_=o_[:]), ch_o[T % 2], reads=[ob])
        dump("rinfo", rinfo[:], [128, 16, 4], F32, [B_rinfo])
        S.barrier(ch_o + [dbg_ch])
    es.close()
    return nc


def _prep_inputs(inp):
    f = lambda k: np.ascontiguousarray(np.asarray(inp[k], dtype=np.float32))
    x = f("x")
    c = f("c")
    iota = np.tile(np.arange(512, dtype=np.float32)[None, :], (128, 1))
    cst = np.zeros((128, 4, 128), np.float32)
    cst[:, 0, :] = np.eye(128, dtype=np.float32)
    cst[:, 1, :] = 1.0
    cst[:, 2, :] = np.triu(np.ones((128, 128), np.float32), 1)
    b_in = f("b_in")[0]
    a_re = f("s5_a_re")[0]
    a_im = f("s5_a_im")[0]
    ldt = f("s5_log_dt")[0]
    b_re = f("s5_b_re")[0]
    b_im = f("s5_b_im")[0]
    c_re = f("s5_c_re")[0]
    c_im = f("s5_c_im")[0]
    sd = f("s5_d")[0].reshape(512)
    dw = f("conv_dw")[0][:, 0, :]
    sm = np.zeros((128, 512), np.float32)
    sm[:, 16:68] = b_in.reshape(52, 128).T
    sm[:, 68:84] = a_re.reshape(16, 128).T
    sm[:, 84:100] = a_im.reshape(16, 128).T
    sm[:, 100:116] = np.repeat(ldt, 64).reshape(16, 128).T
    sm[:, 116:120] = sd.reshape(4, 128).T
    sm[:, 120:128] = f("conv_dw_b")[0].reshape(8, 128).T
    sm[:, 128:136] = f("conv_ln_g")[0].reshape(8, 128).T
    sm[:, 136:144] = f("conv_ln_b")[0].reshape(8, 128).T
    sm[:, 160:408] = dw.T.reshape(8, 128, 31).transpose(1, 0, 2).reshape(128, 248)
    wb = np.zeros((2, 128, 16, 128), np.float32)
    wc = np.zeros((2, 128, 16, 128), np.float32)
    for g in range(32):
        i = g // 2
        gl = g % 2
        ch0 = (g % 8) * 16
        st0 = gl * 64
        wb[0, ch0:ch0 + 16, i, st0:st0 + 64] = b_re[g].T
        wb[1, ch0:ch0 + 16, i, st0:st0 + 64] = b_im[g].T
        wc[0, st0:st0 + 64, i, ch0:ch0 + 16] = c_re[g].T
        wc[1, st0:st0 + 64, i, ch0:ch0 + 16] = c_im[g].T
    lnbc = np.stack([np.tile(f(k)[0][None, :], (128, 1)) for k in ("ln1_g", "ln1_b", "ln2_g", "ln2_b")])
    w_route = np.ascontiguousarray(np.concatenate([f("w_route_group")[0], f("w_route_expert")[0]], axis=1))
    b_route = np.tile(np.concatenate([f("b_route_group")[0], f("b_route_expert")[0]])[None, :], (128, 1)).astype(np.float32)
    shared = {
        "iota": iota, "cst": cst, "w_ada": f("w_ada")[0], "b_ada": f("b_ada"), "w_in": f("w_in")[0],
        "wb": wb, "wc": wc, "w_s5_gate": f("w_s5_gate")[0], "w_s5_up": f("w_s5_up")[0],
        "w_conv_out": f("w_conv_out")[0], "w_out": f("w_out")[0], "lnbc": lnbc, "w_route": w_route,
        "b_route": b_route, "w_exp_gate": f("w_exp_gate")[0], "w_exp_up": f("w_exp_up")[0], "w_exp_down": f("w_exp_down")[0],
    }
    maps = []
    for core in range(8):
        b, half = core // 2, core % 2
        smc = sm.copy()
        smc[:, 0:16] = c[b].reshape(16, 128).T
        smc[:, 144] = float(half)
        m = dict(shared)
        m["smallp"] = smc
        m["x_own"] = np.ascontiguousarray(x[b, half * NTOK:(half + 1) * NTOK])
        m["x_prev"] = np.ascontiguousarray(x[b, 0:NTOK]) if half == 1 else np.zeros((NTOK, D), np.float32)
        maps.append(m)
    return maps


_NC = [None]


def kernel(**inputs):
    maps = _prep_inputs(inputs)
    if _NC[0] is None:
        _NC[0] = build_nc()
    res = run_bass_kernel_spmd(_NC[0], maps, core_ids=list(range(8)))
    outp = np.zeros((4, 2 * NTOK, D), np.float32)
    for core in range(8):
        b, half = core // 2, core % 2
        outp[b, half * NTOK:(half + 1) * NTOK] = res.results[core]["out"]
    if DEBUG:
        kernel.dbg = [res.results[c] for c in range(8)]
    return outp
```

```python
import contextlib
import numpy as np
import concourse.bass as bass
import concourse.mybir as mybir
from concourse.bass_utils import run_bass_kernel_spmd

F32 = mybir.dt.float32
BF16 = mybir.dt.bfloat16
I32 = mybir.dt.int32
ALU = mybir.AluOpType
AF = mybir.ActivationFunctionType
AX = mybir.AxisListType

D = 2048
NTOK = 2048
TB = 512
NBLK = NTOK // TB
INC = 6656
NE = 64
CAP = 128
ALPHA = 2.0 ** 0.25
EPS = 1e-5
MAGIC = 12582912.0
TWO_PI = 6.283185307179586
DEBUG = False


class Buf:
    def __init__(self, name):
        self.name = name
        self.w = None
        self.r = {}


class Chan:
    def __init__(self, sem):
        self.sem = sem
        self.cnt = 0


class Eng:
    def __init__(self, h, sem, is_pe=False):
        self.h = h
        self.sem = sem
        self.cnt = 0
        self.waited = {}
        self.is_pe = is_pe


class Sched:
    def __init__(self, nc, es):
        self.nc = nc
        self.es = es
        self.nsem = 0
        self.pe = Eng(nc.tensor, self.mksem(), True)
        self.dve = Eng(nc.vector, self.mksem())
        self.act = Eng(nc.scalar, self.mksem())
        self.pool = Eng(nc.gpsimd, self.mksem())
        self.sp = Eng(nc.sync, self.mksem())
        self.engs = [self.pe, self.dve, self.act, self.pool, self.sp]

    def mksem(self):
        self.nsem += 1
        return self.es.enter_context(self.nc.semaphore("sm%d" % self.nsem))

    def chan(self):
        return Chan(self.mksem())

    def _sync(self, e, reads, writes):
        deps = []
        for b in reads:
            if b.w is not None:
                deps.append(b.w)
        for b in writes:
            if b.w is not None:
                deps.append(b.w)
            deps.extend(b.r.values())
        for (sem, val) in deps:
            if e.is_pe and sem is e.sem:
                continue
            k = id(sem)
            if e.waited.get(k, 0) >= val:
                continue
            e.h.wait_ge(sem, val)
            e.waited[k] = val

    def _mark(self, tok, reads, writes):
        k = id(tok[0])
        for b in reads:
            if b.r.get(k, (None, 0))[1] < tok[1]:
                b.r[k] = tok
        for b in writes:
            b.w = tok
            b.r = {}

    def op(self, e, fn, reads=(), writes=()):
        self._sync(e, reads, writes)
        inst = fn(e.h)
        e.cnt += 1
        inst.then_inc(e.sem, 1)
        tok = (e.sem, e.cnt)
        self._mark(tok, reads, writes)
        return tok

    def dma(self, e, fn, chan, reads=(), writes=()):
        self._sync(e, reads, writes)
        inst = fn(e.h)
        chan.cnt += 16
        inst.then_inc(chan.sem, 16)
        tok = (chan.sem, chan.cnt)
        self._mark(tok, reads, writes)
        return tok

    def barrier(self, chans=()):
        for e in self.engs:
            for o in self.engs:
                if o is e or o.cnt == 0:
                    continue
                if e.waited.get(id(o.sem), 0) < o.cnt:
                    e.h.wait_ge(o.sem, o.cnt)
                    e.waited[id(o.sem)] = o.cnt
            for c in chans:
                if c.cnt and e.waited.get(id(c.sem), 0) < c.cnt:
                    e.h.wait_ge(c.sem, c.cnt)
                    e.waited[id(c.sem)] = c.cnt


def build_nc():
    nc = bass.Bass("TRN2", target_bir_lowering=False)

    def din(name, shape, dt=F32):
        return nc.dram_tensor(name, list(shape), dt, kind="ExternalInput").ap()

    x_own = din("x_own", [NTOK, D])
    x_prev = din("x_prev", [NTOK, D])
    smallp = din("smallp", [128, 512])
    iota_d = din("iota", [128, 512])
    cst_d = din("cst", [128, 4, 128])
    w_ada = din("w_ada", [D, 6 * D])
    b_ada = din("b_ada", [1, 6 * D])
    w_in = din("w_in", [D, INC])
    wb_d = din("wb", [2, 128, 16, 128])
    wc_d = din("wc", [2, 128, 16, 128])
    w_sg = din("w_s5_gate", [512, 512])
    w_su = din("w_s5_up", [512, D])
    w_co = din("w_conv_out", [1024, D])
    w_out = din("w_out", [D, D])
    lnbc_d = din("lnbc", [4, 128, D])
    w_rt = din("w_route", [D, 72])
    brt_d = din("b_route", [128, 72])
    w_eg = din("w_exp_gate", [NE, D, 512])
    w_eu = din("w_exp_up", [NE, D, 512])
    w_ed = din("w_exp_down", [NE, 512, D])
    out = nc.dram_tensor("out", [NTOK, D], F32, kind="ExternalOutput").ap()
    x1_d = nc.dram_tensor("x1s", [NTOK, D], F32, kind="ExternalOutput" if DEBUG else "Internal").ap()
    xdisp = nc.dram_tensor("xdisp", [NE * CAP, D], BF16, kind="Internal").ap()
    ydh = [nc.dram_tensor("ydisp%d" % i, [NE * CAP, 1024], F32, kind="Internal").ap() for i in range(2)]

    es = contextlib.ExitStack()
    S = Sched(nc, es)
    pe, dve, act, pool, sp = S.pe, S.dve, S.act, S.pool, S.sp

    dbg_ch = S.chan()

    def dump(name, ap, shape, dt, reads):
        if not DEBUG:
            return
        dd = nc.dram_tensor("dbg_" + name, list(shape), dt, kind="ExternalOutput").ap()
        S.dma(sp, lambda h: h.dma_start(out=dd, in_=ap), dbg_ch, reads=reads)

    def sb(stack, name, shape, dt):
        return stack.enter_context(nc.sbuf_tensor("s_" + name, list(shape), dt))

    PS = [es.enter_context(nc.psum_tensor("ps%d" % i, [128, 512], F32)) for i in range(6)]
    PSB = [Buf("ps%d" % i) for i in range(6)]
    PT = [es.enter_context(nc.psum_tensor("pt%d" % i, [128, 1024], BF16)) for i in range(2)]
    PTB = [Buf("pt%d" % i) for i in range(2)]
    pctr = [0]

    def pbank():
        i = pctr[0] % 6
        pctr[0] += 1
        return PS[i], PSB[i]

    tctr = [0]

    def tbank():
        i = tctr[0] % 2
        tctr[0] += 1
        return PT[i], PTB[i]

    sp_t = sb(es, "smallp", [128, 512], F32)
    iota = sb(es, "iota", [128, 512], F32)
    cst = sb(es, "cst", [128, 4, 128], F32)
    identb = sb(es, "identb", [128, 128], BF16)
    onesb = sb(es, "onesb", [128, 128], BF16)
    trib = sb(es, "trib", [128, 128], BF16)
    modpp = sb(es, "modpp", [128, 4, 16], F32)
    modbc = sb(es, "modbc", [128, 4, D], F32)
    rinfo = sb(es, "rinfo", [128, 16, 4], F32)
    rdest = sb(es, "rdest", [128, 16, 2], I32)
    basebc = sb(es, "basebc", [128, 64], F32)
    B_const = Buf("const")
    B_modpp = Buf("modpp")
    B_modbc = Buf("modbc")
    B_rinfo = Buf("rinfo")
    B_base = Buf("base")
    identf = cst[:, 0, :]
    onesf = cst[:, 1, :]

    C_C = 0
    C_BIN = 16
    C_ARE = 68
    C_AIM = 84
    C_LDT = 100
    C_SD = 116
    C_DWB = 120
    C_LNG = 128
    C_LNB = 136
    C_FLAG = 144
    C_DW = 160

    ch_par = S.chan()
    S.dma(sp, lambda h: h.dma_start(out=sp_t[:], in_=smallp), ch_par, writes=[B_const])
    S.dma(sp, lambda h: h.dma_start(out=iota[:], in_=iota_d), ch_par, writes=[B_const])
    S.dma(sp, lambda h: h.dma_start(out=cst[:], in_=cst_d), ch_par, writes=[B_const])
    B_const.w = (ch_par.sem, ch_par.cnt)
    S.op(dve, lambda h: h.tensor_copy(out=identb[:], in_=cst[:, 0, :]), reads=[B_const], writes=[B_const])
    S.op(dve, lambda h: h.tensor_copy(out=onesb[:], in_=cst[:, 1, :]), reads=[B_const], writes=[B_const])
    S.op(dve, lambda h: h.tensor_copy(out=trib[:], in_=cst[:, 2, :]), reads=[B_const], writes=[B_const])
    S.op(dve, lambda h: h.memset(basebc[:], 0.0), writes=[B_base])

    B_xd = Buf("xdisp")
    ch_xd = S.chan()
    bc_reg = nc.gpsimd.to_reg(NE * CAP - 1)

    NSLOT = 6
    wbf = [sb(es, "wbf%d" % i, [128, 4096], BF16) for i in range(NSLOT)]
    wbfB = [Buf("wbf%d" % i) for i in range(NSLOT)]
    wbfC = [S.chan() for _ in range(NSLOT)]
    wctr = [0]
    fst = {"t": [], "b": [], "c": [S.chan(), S.chan(), S.chan()], "n": 0, "k": 0}

    def alloc_fst(stack, n, width):
        fst["k"] += 1
        fst["t"] = [sb(stack, "fst%d_%d" % (fst["k"], i), [128, width], F32) for i in range(n)]
        fst["b"] = [Buf("fst%d" % i) for i in range(n)]
        fst["n"] = 0

    def cast(out_ap, in_ap, reads, writes, eng=None):
        if eng is act:
            S.op(act, lambda h: h.activation(out=out_ap, in_=in_ap, func=AF.Copy), reads=reads, writes=writes)
        else:
            S.op(eng, lambda h: h.tensor_copy(out=out_ap, in_=in_ap), reads=reads, writes=writes)

    def fload(pieces):
        i = fst["n"] % len(fst["t"])
        fst["n"] += 1
        off = 0
        for (ap, a, b) in pieces:
            v = fst["t"][i][:, off:off + a * b].rearrange("p (a b) -> p a b", a=a)
            S.dma(sp, lambda h, v=v, ap=ap: h.dma_start(out=v, in_=ap), fst["c"][i], writes=[fst["b"][i]])
            off += a * b
        return fst["t"][i], fst["b"][i]

    def wload(pieces, do_cast=True, dst=None, dstB=None):
        if not do_cast:
            return fload(pieces)
        i = wctr[0] % NSLOT
        wctr[0] += 1
        off = 0
        for (ap, a, b) in pieces:
            v = wbf[i][:, off:off + a * b].rearrange("p (a b) -> p a b", a=a)
            S.dma(pool, lambda h, v=v, ap=ap: h.dma_start(out=v, in_=ap), wbfC[i], writes=[wbfB[i]])
            off += a * b
        return wbf[i], wbfB[i]

    NCV = NE * 6
    wscr_l = [nc.dram_tensor("wscr%d" % j, [NCV // 2, 128, 4096], BF16, kind="Internal").ap() for j in range(2)]
    wscr = lambda u: wscr_l[u // (NCV // 2)][u % (NCV // 2)]
    cvB = [Buf("cv%d" % u) for u in range(NCV)]
    CVG = 32
    CV_MAX = 160
    cvC = [S.chan() for _ in range(NCV // CVG)]
    cv_next = [0]

    def cv_src(u):
        e, r = divmod(u, 6)
        if r < 4:
            wsrc = w_eg if r < 2 else w_eu
            hf = r % 2
            return kview(wsrc[e], 16)[:, :, hf * 256:(hf + 1) * 256], 16
        hf = r - 4
        return kview(w_ed[e], 4)[:, :, hf * 1024:(hf + 1) * 1024], 4

    def cv_issue(n):
        for _ in range(n):
            u = cv_next[0]
            if u >= NCV:
                return
            cv_next[0] += 1
            src, a = cv_src(u)
            dstv = wscr(u).rearrange("p (a b) -> p a b", a=a)
            S.dma(pool, lambda h, dstv=dstv, src=src: h.dma_start(out=dstv, in_=src), cvC[u // CVG], writes=[cvB[u]])

    def cv_finalize():
        for u in range(cv_next[0]):
            c = cvC[u // CVG]
            cvB[u].w = (c.sem, c.cnt)

    def eload(u):
        if u >= cv_next[0]:
            src, a = cv_src(u)
            return wload([(src, a, 4096 // a)])
        i = wctr[0] % NSLOT
        wctr[0] += 1
        S.dma(sp, lambda h: h.dma_start(out=wbf[i][:], in_=wscr(u)), wbfC[i], reads=[cvB[u]], writes=[wbfB[i]])
        return wbf[i], wbfB[i]

    def pipeline(units, depth=4):
        loaded = []
        n = len(units)
        for i in range(n + depth):
            if i < n:
                loaded.append(units[i][0]())
            j = i - depth
            if j >= 0:
                units[j][1](*loaded[j])
                loaded[j] = None

    def kview(ap2d, kt):
        return ap2d.rearrange("(kt p) n -> p kt n", p=128)

    w_in_v = kview(w_in, 16)
    w_ada_v = kview(w_ada, 16)

    mx_pieces = []
    for kp_ in range(8):
        mx_pieces.append([(w_in_v[:, :, 512 + kp_ * 128:512 + (kp_ + 1) * 128], 16, 128),
                          (w_in_v[:, :, 1536 + kp_ * 128:1536 + (kp_ + 1) * 128], 16, 128)])
    for k_ in range(16):
        mx_pieces.append([(w_in_v[:, :, 2560 + k_ * 128:2560 + (k_ + 1) * 128], 16, 128),
                          (w_in_v[:, :, 4608 + k_ * 128:4608 + (k_ + 1) * 128], 16, 128)])
    for k_ in range(16):
        mx_pieces.append([(kview(w_su, 4)[:, :, k_ * 128:(k_ + 1) * 128], 4, 128),
                          (kview(w_co, 8)[:, :, k_ * 128:(k_ + 1) * 128], 8, 128)])
    for fb_ in range(8):
        mx_pieces.append([(kview(w_out, 16)[:, :, fb_ * 256:(fb_ + 1) * 256], 16, 256)])
    MX_CONV, MX_GATE, MX_PROJ, MX_WO = 0, 8, 24, 40
    mscr = nc.dram_tensor("mscr", [48, 128, 4096], BF16, kind="Internal").ap()
    mxB = [Buf("mx%d" % j) for j in range(48)]
    mxC = S.chan()

    mxq = []
    for j_ in range(48):
        off_ = 0
        for (ap_, a_, b_) in mx_pieces[j_]:
            mxq.append((j_, off_, ap_, a_, b_))
            off_ += a_ * b_

    def mx_issue(n):
        k = 0
        while k < n and mxq:
            j, off, ap, a, b = mxq.pop(0)
            dstv = mscr[j][:, off:off + a * b].rearrange("p (a b) -> p a b", a=a)
            S.dma(pool, lambda h, dstv=dstv, ap=ap: h.dma_start(out=dstv, in_=ap), mxC, writes=[mxB[j]])
            k += 1
        return k

    def mx_convert_all():
        mx_issue(10 ** 6)

    def mx_finalize():
        for j in range(48):
            mxB[j].w = (mxC.sem, mxC.cnt)

    def mload(j):
        i = wctr[0] % NSLOT
        wctr[0] += 1
        S.dma(sp, lambda h: h.dma_start(out=wbf[i][:], in_=mscr[j]), wbfC[i], reads=[mxB[j]], writes=[wbfB[i]])
        return wbf[i], wbfB[i]


    def mm(ps, lhsT, rhs, start, stop, reads, pbuf):
        S.op(pe, lambda h: h.matmul(ps, lhsT=lhsT, rhs=rhs, start=start, stop=stop), reads=reads, writes=[pbuf])

    with contextlib.ExitStack() as pa:
        zt = sb(pa, "zt", [128, D], BF16)
        B_zt = Buf("zt")
        S.op(pool, lambda h: h.memset(zt[:], 0.0), writes=[B_zt])
        for e in range(NE):
            S.dma(pool, lambda h, e=e: h.dma_start(out=xdisp[e * CAP:(e + 1) * CAP, :], in_=zt[:]), ch_xd,
                  reads=[B_zt], writes=[B_xd])
        alloc_fst(pa, 3, D)
        cact = sb(pa, "cact", [128, 16], F32)
        cb = sb(pa, "cb", [128, 16, 128], F32)
        R = sb(pa, "R", [128, D], F32)
        bada = sb(pa, "bada", [1, D], F32)
        tmp3 = sb(pa, "tmp3", [128, 16, 128], F32)
        B_c = Buf("cact")
        B_R = Buf("R")
        B_ba = Buf("bada")
        B_t3 = Buf("tmp3")
        ch_a = S.chan()
        S.op(act, lambda h: h.activation(out=cact[:], in_=sp_t[:, C_C:C_C + 16], func=AF.Silu),
             reads=[B_const], writes=[B_c])
        for kt in range(16):
            S.op(dve, lambda h, kt=kt: h.tensor_copy(out=cb[:, kt, :], in_=cact[:, kt:kt + 1].to_broadcast([128, 128])),
                 reads=[B_c], writes=[B_c])
        pp_dst = {0: (0, False), 1: (1, True), 3: (2, False), 4: (3, True)}
        bc_dst = {2: [(0, 1.0)], 5: [(1, 1.0)], 4: [(2, 1.0)], 3: [(3, 0.0)]}
        for grp in range(6):
            banks = [pbank() for _ in range(4)]
            S.dma(sp, lambda h, grp=grp: h.dma_start(out=bada[:], in_=b_ada[:, grp * D:(grp + 1) * D]), ch_a, writes=[B_ba])

            def ld(grp, kt):
                return wload([(w_ada_v[:, kt:kt + 1, grp * D:(grp + 1) * D], 1, D)], do_cast=False)

            def use(t, tb, kt, banks):
                for nb in range(4):
                    mm(banks[nb][0][:], cb[:, kt, :], t[:, nb * 512:(nb + 1) * 512], kt == 0, False,
                       [tb, B_c], banks[nb][1])

            units = [((lambda kt=kt, grp=grp: ld(grp, kt)), (lambda t, tb, kt=kt, banks=banks: use(t, tb, kt, banks)))
                     for kt in range(16)]
            pipeline(units, depth=2)
            for nb in range(4):
                c0 = nb * 512
                mm(banks[nb][0][:], cst[0:1, 1, :], bada[0:1, c0:c0 + 512], False, True, [B_ba, B_const], banks[nb][1])
                S.op(act, lambda h, nb=nb, c0=c0, banks=banks: h.activation(out=R[:, c0:c0 + 512], in_=banks[nb][0][:], func=AF.Copy),
                     reads=[banks[nb][1]], writes=[B_R])
            if grp in pp_dst:
                vi, plus1 = pp_dst[grp]
                S.op(dve, lambda h: h.tensor_tensor(
                    out=tmp3[:], in0=R[:].rearrange("p (a b) -> p a b", a=16),
                    in1=identf.unsqueeze(1).to_broadcast([128, 16, 128]), op=ALU.mult),
                    reads=[B_R, B_const], writes=[B_t3])
                S.op(dve, lambda h, vi=vi: h.reduce_sum(out=modpp[:, vi, :], in_=tmp3[:], axis=AX.X),
                     reads=[B_t3], writes=[B_modpp])
                if plus1:
                    S.op(dve, lambda h, vi=vi: h.tensor_scalar_add(out=modpp[:, vi, :], in0=modpp[:, vi, :], scalar1=1.0),
                         reads=[B_modpp], writes=[B_modpp])
            for (di, add) in bc_dst.get(grp, []):
                S.op(dve, lambda h, di=di, add=add: h.tensor_scalar_add(out=modbc[:, di, :], in0=R[:], scalar1=add),
                     reads=[B_R], writes=[B_modbc])
        dump("modpp", modpp[:], [128, 4, 16], F32, [B_modpp])
        dump("modbc", modbc[:], [128, 4, D], F32, [B_modbc])
        S.barrier([ch_a, ch_xd, dbg_ch])

    s5p = sb(es, "s5p", [128, 12, 16], F32)
    B_s5p = Buf("s5p")
    hT_halo = sb(es, "hT_halo", [128, 16, 32], BF16)
    B_halo = Buf("halo")
    s5o = sb(es, "s5o", [128, 4, NTOK], BF16)
    B_s5o = Buf("s5o")
    lnst = sb(es, "lnst", [128, 4, 6], F32)
    lnmv = sb(es, "lnmv", [128, 4], F32)
    B_ln = Buf("lnst")
    pus = contextlib.ExitStack()
    wbb = sb(pus, "wbb", [128, 2, 16, 128], BF16)
    wcb = sb(pus, "wcb", [128, 2, 16, 128], BF16)
    B_wb = Buf("wbb")
    uT = sb(pus, "uT", [128, 4, 2 * NTOK], BF16)
    B_uT = [Buf("uT%d" % g) for g in range(2 * NBLK)]

    def sincos(eng, ang_ap, cos_out, sin_out, rb, wbuf, t, a, Bt):
        for (shift, outp) in ((0.0, sin_out), (np.pi / 2, cos_out)):
            S.op(eng, lambda h, shift=shift: h.tensor_scalar(out=a[:], in0=ang_ap, scalar1=float(shift), scalar2=None, op0=ALU.add),
                 reads=rb, writes=[Bt])
            S.op(eng, lambda h: h.tensor_scalar(out=t[:], in0=a[:], scalar1=1.0 / TWO_PI, scalar2=MAGIC, op0=ALU.mult, op1=ALU.add),
                 reads=[Bt], writes=[Bt])
            S.op(eng, lambda h: h.tensor_scalar(out=t[:], in0=t[:], scalar1=-MAGIC, scalar2=None, op0=ALU.add),
                 reads=[Bt], writes=[Bt])
            S.op(eng, lambda h: h.scalar_tensor_tensor(out=a[:], in0=t[:], scalar=-TWO_PI, in1=a[:], op0=ALU.mult, op1=ALU.add),
                 reads=[Bt], writes=[Bt])
            S.op(eng, lambda h: h.tensor_scalar(out=a[:], in0=a[:], scalar1=3.1415925, scalar2=-3.1415925, op0=ALU.min, op1=ALU.max),
                 reads=[Bt], writes=[Bt])
            S.op(act, lambda h, outp=outp: h.activation(out=outp, in_=a[:], func=AF.Sin), reads=[Bt], writes=wbuf)

    with contextlib.ExitStack() as pp:
        alloc_fst(pp, 2, 512)
        are = sp_t[:, C_ARE:C_ARE + 16]
        aim = sp_t[:, C_AIM:C_AIM + 16]
        sc = lambda i: s5p[:, i, :]
        S.op(act, lambda h: h.activation(out=sc(6), in_=sp_t[:, C_LDT:C_LDT + 16], func=AF.Exp), reads=[B_const], writes=[B_s5p])
        S.op(dve, lambda h: h.tensor_mul(out=sc(7), in0=are, in1=sc(6)), reads=[B_s5p, B_const], writes=[B_s5p])
        S.op(dve, lambda h: h.tensor_mul(out=sc(5), in0=aim, in1=sc(6)), reads=[B_s5p, B_const], writes=[B_s5p])
        S.op(act, lambda h: h.activation(out=sc(0), in_=sc(7), func=AF.Exp), reads=[B_s5p], writes=[B_s5p])
        sct = sb(pp, "sct", [128, 16], F32)
        sca = sb(pp, "sca", [128, 16], F32)
        sincos(dve, sc(5), sc(1), sc(2), [B_s5p], [B_s5p], sct, sca, Buf("scp"))
        S.op(dve, lambda h: h.tensor_mul(out=sc(8), in0=sc(0), in1=sc(1)), reads=[B_s5p], writes=[B_s5p])
        S.op(dve, lambda h: h.tensor_scalar_add(out=sc(8), in0=sc(8), scalar1=-1.0), reads=[B_s5p], writes=[B_s5p])
        S.op(dve, lambda h: h.tensor_mul(out=sc(9), in0=sc(0), in1=sc(2)), reads=[B_s5p], writes=[B_s5p])
        S.op(dve, lambda h: h.tensor_mul(out=sc(10), in0=are, in1=are), reads=[B_s5p, B_const], writes=[B_s5p])
        S.op(dve, lambda h: h.tensor_mul(out=sc(11), in0=aim, in1=aim), reads=[B_s5p, B_const], writes=[B_s5p])
        S.op(dve, lambda h: h.tensor_add(out=sc(10), in0=sc(10), in1=sc(11)), reads=[B_s5p], writes=[B_s5p])
        S.op(dve, lambda h: h.reciprocal(out=sc(10), in_=sc(10)), reads=[B_s5p], writes=[B_s5p])
        S.op(dve, lambda h: h.tensor_mul(out=sc(3), in0=sc(8), in1=are), reads=[B_s5p, B_const], writes=[B_s5p])
        S.op(dve, lambda h: h.tensor_mul(out=sc(11), in0=sc(9), in1=aim), reads=[B_s5p, B_const], writes=[B_s5p])
        S.op(dve, lambda h: h.tensor_add(out=sc(3), in0=sc(3), in1=sc(11)), reads=[B_s5p], writes=[B_s5p])
        S.op(dve, lambda h: h.tensor_mul(out=sc(3), in0=sc(3), in1=sc(10)), reads=[B_s5p], writes=[B_s5p])
        S.op(dve, lambda h: h.tensor_mul(out=sc(4), in0=sc(9), in1=are), reads=[B_s5p, B_const], writes=[B_s5p])
        S.op(dve, lambda h: h.tensor_mul(out=sc(11), in0=sc(8), in1=aim), reads=[B_s5p, B_const], writes=[B_s5p])
        S.op(dve, lambda h: h.tensor_sub(out=sc(4), in0=sc(4), in1=sc(11)), reads=[B_s5p], writes=[B_s5p])
        S.op(dve, lambda h: h.tensor_mul(out=sc(4), in0=sc(4), in1=sc(10)), reads=[B_s5p], writes=[B_s5p])
        dump("s5p", s5p[:], [128, 12, 16], F32, [B_s5p])
        for pl in range(2):
            for hf in range(4):
                t, tb = wload([(wb_d[pl, :, hf * 4:(hf + 1) * 4, :], 4, 128)], do_cast=False)
                S.op(dve, lambda h, t=t, pl=pl, hf=hf: h.tensor_copy(
                    out=wbb[:, pl, hf * 4:(hf + 1) * 4, :], in_=t[:, 0:512].rearrange("p (a b) -> p a b", a=4)),
                    reads=[tb], writes=[B_wb])
                t, tb = wload([(wc_d[pl, :, hf * 4:(hf + 1) * 4, :], 4, 128)], do_cast=False)
                S.op(dve, lambda h, t=t, pl=pl, hf=hf: h.tensor_scalar(
                    out=wcb[:, pl, hf * 4:(hf + 1) * 4, :], in0=t[:, 0:512].rearrange("p (a b) -> p a b", a=4),
                    scalar1=(1.0 if pl == 0 else -1.0), scalar2=None, op0=ALU.mult),
                    reads=[tb], writes=[B_wb])
        S.barrier()


    def ln_stats(x_ap, xb):
        for c in range(4):
            S.op(dve, lambda h, c=c: h.bn_stats(out=lnst[:, c, :], in_=x_ap[:, c * 512:(c + 1) * 512]),
                 reads=[xb], writes=[B_ln])
        S.op(dve, lambda h: h.bn_aggr(out=lnmv[:, 0:2], in_=lnst[:].rearrange("p a b -> p (a b)")), reads=[B_ln], writes=[B_ln])
        S.op(dve, lambda h: h.tensor_scalar_add(out=lnmv[:, 3:4], in0=lnmv[:, 1:2], scalar1=EPS), reads=[B_ln], writes=[B_ln])
        S.op(act, lambda h: h.activation(out=lnmv[:, 3:4], in_=lnmv[:, 3:4], func=AF.Sqrt), reads=[B_ln], writes=[B_ln])
        S.op(dve, lambda h: h.reciprocal(out=lnmv[:, 2:3], in_=lnmv[:, 3:4]), reads=[B_ln], writes=[B_ln])

    xts = {"t": [], "b": [], "c": [S.chan(), S.chan()], "n": 0, "k": 0}

    def alloc_xt(stack, n):
        xts["k"] += 1
        xts["t"] = [sb(stack, "xt%d_%d" % (xts["k"], i), [128, D], F32) for i in range(n)]
        xts["b"] = [Buf("xt%d" % i) for i in range(n)]
        xts["n"] = 0

    def load_x(src_ap):
        i = xts["n"] % len(xts["t"])
        xts["n"] += 1
        tt_, bb_ = xts["t"][i], xts["b"][i]
        S.dma(sp, lambda h: h.dma_start(out=tt_[:], in_=src_ap), xts["c"][i], writes=[bb_])
        return tt_, bb_

    with contextlib.ExitStack() as pu:
        alloc_xt(pu, 2)
        xn = sb(pu, "xn", [128, 4, D], BF16)
        B_xn = Buf("xn")
        hTp = [sb(pu, "hTp%d" % i, [128, 16, TB], BF16) for i in range(1)]
        B_hTp = [Buf("hTp%d" % i) for i in range(1)]
        def make_hT(src, blk, xn_t, xn_b, hdst, hbuf):
            for tt in range(4):
                t, tb = load_x(src[blk * TB + tt * 128: blk * TB + (tt + 1) * 128, :])
                ln_stats(t, tb)
                S.op(dve, lambda h, t=t, tt=tt: h.tensor_scalar(out=xn_t[:, tt, :], in0=t[:], scalar1=lnmv[:, 0:1], scalar2=lnmv[:, 2:3],
                                                              op0=ALU.subtract, op1=ALU.mult),
                     reads=[tb, B_ln], writes=[xn_b])
            for kt in range(16):
                ptile, ptb = tbank()
                for tt in range(4):
                    S.op(pe, lambda h, kt=kt, tt=tt, ptile=ptile: h.transpose(
                        out=ptile[:, tt * 128:(tt + 1) * 128], in_=xn_t[:, tt, kt * 128:(kt + 1) * 128], identity=identb[:]),
                        reads=[xn_b, B_const], writes=[ptb])
                S.op(act, lambda h, kt=kt, ptile=ptile: h.activation(
                    out=hdst[:, kt, :], in_=ptile[:, 0:512], func=AF.Identity,
                    scale=modpp[:, 1, kt:kt + 1], bias=modpp[:, 0, kt:kt + 1]),
                    reads=[ptb, B_modpp], writes=[hbuf])

        for g in range(2 * NBLK):
            own = g >= NBLK
            blk = g - NBLK if own else g
            src = x_own if own else x_prev
            make_hT(src, blk, xn, B_xn, hTp[0], B_hTp[0])
            def u_use(t, tb, hf, g=g):
                w = t[:, 0:4096].rearrange("p (a b) -> p a b", a=16)
                for m2 in range(2):
                    m = hf * 2 + m2
                    ps, pb = pbank()
                    for kt in range(16):
                        mm(ps[:], w[:, kt, m2 * 128:(m2 + 1) * 128], hTp[0][:, kt, :], kt == 0, kt == 15, [tb, B_hTp[0]], pb)
                    S.op(act, lambda h, m=m, ps=ps, g=g: h.activation(
                        out=uT[:, m, g * TB:(g + 1) * TB], in_=ps[:], func=AF.Identity, bias=sp_t[:, C_BIN + m:C_BIN + m + 1]),
                        reads=[pb, B_const], writes=[B_uT[g]])

            pipeline([((lambda hf=hf: wload([(w_in_v[:, :, hf * 256:(hf + 1) * 256], 16, 256)])),
                       (lambda t, tb, hf=hf: u_use(t, tb, hf))) for hf in range(2)])
            if g == NBLK - 1:
                S.op(dve, lambda h: h.tensor_copy(out=hT_halo[:], in_=hTp[0][:, :, TB - 32:TB]),
                     reads=[B_hTp[0]], writes=[B_halo])
        S.barrier()

    with contextlib.ExitStack() as ps5:
        cs = sb(ps5, "cs", [128, TB], F32)
        sn = sb(ps5, "sn", [128, TB], F32)
        mqr = sb(ps5, "mqr", [128, TB], F32)
        mqi = sb(ps5, "mqi", [128, TB], F32)
        dec = sb(ps5, "dec", [128, TB], F32)
        B_tab = Buf("tab")
        bre = sb(ps5, "bre", [128, TB], F32)
        bim = sb(ps5, "bim", [128, TB], F32)
        B_b = Buf("b")
        t1 = sb(ps5, "t1", [128, TB], F32)
        t2 = sb(ps5, "t2", [128, TB], F32)
        t3 = sb(ps5, "t3", [128, TB], F32)
        t4 = sb(ps5, "t4", [128, TB], F32)
        B_t12 = Buf("t12")
        B_t34 = Buf("t34")
        ang = t1
        sct2, sca2, B_sc2 = t3, t4, B_t34
        mre = sb(ps5, "mre", [128, TB], F32)
        mim = sb(ps5, "mim", [128, TB], F32)
        B_mre = Buf("mre")
        B_mim = Buf("mim")
        sre = sb(ps5, "sre", [128, TB], F32)
        sim = sb(ps5, "sim", [128, TB], F32)
        B_sre = Buf("sre")
        B_sim = Buf("sim")
        srb = sb(ps5, "srb", [128, TB], BF16)
        sib = sb(ps5, "sib", [128, TB], BF16)
        B_srb = Buf("srb")
        B_sib = Buf("sib")
        st = sb(ps5, "st", [128, 8], F32)
        B_st = Buf("st")
        gT = sb(ps5, "gT", [128, 4, NTOK], BF16)
        B_gT = Buf("gT")
        yp, y2, B_yp = bre, bim, B_b
        sreX = sb(ps5, "sreX", [128, TB], F32)
        simX = sb(ps5, "simX", [128, TB], F32)
        bre2, bim2, B_b2 = [bre, bre], [bim, bim], [B_b, B_b]
        sre2, sim2 = [sre, sreX], [sim, simX]
        B_sre2, B_sim2 = [B_sre, Buf("sreX")], [B_sim, Buf("simX")]
        ybanks = None
        for i in range(16):
            q = i % 4
            ut = i // 4
            if q == 0:
                ybanks = [(PS[b_], PSB[b_]) for b_ in range(NBLK)]
            th = s5p[:, 5, i:i + 1]
            S.op(dve, lambda h, th=th: h.tensor_scalar(out=ang[:], in0=iota[:], scalar1=th, scalar2=None, op0=ALU.mult),
                 reads=[B_const, B_s5p], writes=[B_t12])
            sincos(dve, ang[:], cs[:], sn[:], [B_t12], [B_tab], sct2, sca2, B_sc2)
            qre = s5p[:, 3, i:i + 1]
            qim = s5p[:, 4, i:i + 1]
            S.op(dve, lambda h, qre=qre: h.tensor_scalar(out=mqr[:], in0=cs[:], scalar1=qre, scalar2=None, op0=ALU.mult),
                 reads=[B_tab, B_s5p], writes=[B_tab])
            S.op(dve, lambda h, qim=qim: h.scalar_tensor_tensor(out=mqr[:], in0=sn[:], scalar=qim, in1=mqr[:], op0=ALU.mult, op1=ALU.add),
                 reads=[B_tab, B_s5p], writes=[B_tab])
            S.op(dve, lambda h, qim=qim: h.tensor_scalar(out=mqi[:], in0=cs[:], scalar1=qim, scalar2=None, op0=ALU.mult),
                 reads=[B_tab, B_s5p], writes=[B_tab])
            S.op(dve, lambda h, qre=qre: h.tensor_scalar(out=t1[:], in0=sn[:], scalar1=qre, scalar2=None, op0=ALU.mult),
                 reads=[B_tab, B_s5p], writes=[B_t12])
            S.op(dve, lambda h: h.tensor_sub(out=mqi[:], in0=mqi[:], in1=t1[:]), reads=[B_tab, B_t12], writes=[B_tab])
            S.op(dve, lambda h, i=i: h.tensor_copy(out=dec[:], in_=s5p[:, 0, i:i + 1].to_broadcast([128, TB])),
                 reads=[B_s5p], writes=[B_tab])
            S.op(dve, lambda h: h.memset(st[:], 0.0), writes=[B_st])
            cth = s5p[:, 1, i:i + 1]
            sth = s5p[:, 2, i:i + 1]
            LL = TB - 1
            S.op(dve, lambda h, sth=sth: h.tensor_scalar(out=st[:, 4:5], in0=sn[:, LL:LL + 1], scalar1=sth, scalar2=None, op0=ALU.mult),
                 reads=[B_tab, B_s5p, B_st], writes=[B_st])
            S.op(dve, lambda h, cth=cth: h.scalar_tensor_tensor(out=st[:, 6:7], in0=cs[:, LL:LL + 1], scalar=cth, in1=st[:, 4:5],
                                                                op0=ALU.mult, op1=ALU.subtract), reads=[B_tab, B_s5p, B_st], writes=[B_st])
            S.op(dve, lambda h, cth=cth: h.tensor_scalar(out=st[:, 5:6], in0=sn[:, LL:LL + 1], scalar1=cth, scalar2=None, op0=ALU.mult),
                 reads=[B_tab, B_s5p, B_st], writes=[B_st])
            S.op(dve, lambda h, sth=sth: h.scalar_tensor_tensor(out=st[:, 7:8], in0=cs[:, LL:LL + 1], scalar=sth, in1=st[:, 5:6],
                                                                op0=ALU.mult, op1=ALU.add), reads=[B_tab, B_s5p, B_st], writes=[B_st])
            pend_c = []

            def flush_c(i=i, q=q, pend_c=pend_c, ybanks=ybanks):
                while pend_c:
                    b_ = pend_c.pop(0)
                    yb, ybb = ybanks[b_]
                    mm(yb[:], wcb[:, 0, i, :], srb[:], q == 0, False, [B_wb, B_srb], ybb)
                    mm(yb[:], wcb[:, 1, i, :], sib[:], False, q == 3, [B_wb, B_sib], ybb)

            for g in range(2 * NBLK):
                own = g >= NBLK
                blk = g - NBLK
                n_mx = mx_issue(2)
                if n_mx < 2 and cv_next[0] < CV_MAX:
                    cv_issue(2 - n_mx)
                pr, prb = PS[4], PSB[4]
                pi, pib = PS[5], PSB[5]
                par = g % 2
                bre_, bim_, B_b_ = bre2[par], bim2[par], B_b2[par]
                sre_, sim_, B_sre_, B_sim_ = sre2[par], sim2[par], B_sre2[par], B_sim2[par]
                mm(pr[:], wbb[:, 0, i, :], uT[:, ut, g * TB:(g + 1) * TB], True, True, [B_wb, B_uT[g]], prb)
                mm(pi[:], wbb[:, 1, i, :], uT[:, ut, g * TB:(g + 1) * TB], True, True, [B_wb, B_uT[g]], pib)
                flush_c()
                S.op(dve, lambda h, pr=pr: h.tensor_mul(out=t1[:], in0=pr[:], in1=mqr[:]), reads=[prb, B_tab], writes=[B_t12])
                S.op(dve, lambda h, pi=pi: h.tensor_mul(out=t2[:], in0=pi[:], in1=mqi[:]), reads=[pib, B_tab], writes=[B_t12])
                S.op(dve, lambda h: h.tensor_sub(out=mre[:], in0=t1[:], in1=t2[:]), reads=[B_t12], writes=[B_mre])
                S.op(dve, lambda h, pr=pr: h.tensor_mul(out=t1[:], in0=pr[:], in1=mqi[:]), reads=[prb, B_tab], writes=[B_t12])
                S.op(dve, lambda h, pi=pi: h.tensor_mul(out=t2[:], in0=pi[:], in1=mqr[:]), reads=[pib, B_tab], writes=[B_t12])
                S.op(dve, lambda h: h.tensor_add(out=mim[:], in0=t1[:], in1=t2[:]), reads=[B_t12], writes=[B_mim])
                S.op(dve, lambda h, sre_=sre_: h.tensor_tensor_scan(out=sre_[:], data0=dec[:], data1=mre[:], initial=st[:, 2:3],
                                                                    op0=ALU.mult, op1=ALU.add), reads=[B_tab, B_mre, B_st], writes=[B_sre_])
                S.op(dve, lambda h, sim_=sim_: h.tensor_tensor_scan(out=sim_[:], data0=dec[:], data1=mim[:], initial=st[:, 3:4],
                                                                    op0=ALU.mult, op1=ALU.add), reads=[B_tab, B_mim, B_st], writes=[B_sim_])
                L = TB - 1
                S.op(dve, lambda h, sim_=sim_: h.tensor_scalar(out=st[:, 4:5], in0=sim_[:, L:L + 1], scalar1=st[:, 7:8], scalar2=None, op0=ALU.mult),
                     reads=[B_sim_, B_st], writes=[B_st])
                S.op(dve, lambda h, sre_=sre_: h.scalar_tensor_tensor(out=st[:, 2:3], in0=sre_[:, L:L + 1], scalar=st[:, 6:7], in1=st[:, 4:5],
                                                                      op0=ALU.mult, op1=ALU.subtract), reads=[B_sre_, B_st], writes=[B_st])
                S.op(dve, lambda h, sim_=sim_: h.tensor_scalar(out=st[:, 5:6], in0=sim_[:, L:L + 1], scalar1=st[:, 6:7], scalar2=None, op0=ALU.mult),
                     reads=[B_sim_, B_st], writes=[B_st])
                S.op(dve, lambda h, sre_=sre_: h.scalar_tensor_tensor(out=st[:, 3:4], in0=sre_[:, L:L + 1], scalar=st[:, 7:8], in1=st[:, 5:6],
                                                                      op0=ALU.mult, op1=ALU.add), reads=[B_sre_, B_st], writes=[B_st])
                if g == NBLK - 1:
                    S.op(dve, lambda h: h.tensor_scalar(out=st[:, 2:4], in0=st[:, 2:4], scalar1=sp_t[:, C_FLAG:C_FLAG + 1],
                                                        scalar2=None, op0=ALU.mult), reads=[B_st, B_const], writes=[B_st])
                if not own:
                    continue
                S.op(pool, lambda h, sre_=sre_: h.tensor_mul(out=t3[:], in0=sre_[:], in1=cs[:]), reads=[B_sre_, B_tab], writes=[B_t34])
                S.op(pool, lambda h, sim_=sim_: h.tensor_mul(out=t4[:], in0=sim_[:], in1=sn[:]), reads=[B_sim_, B_tab], writes=[B_t34])
                S.op(pool, lambda h: h.tensor_sub(out=srb[:], in0=t3[:], in1=t4[:]), reads=[B_t34], writes=[B_srb])
                S.op(pool, lambda h, sre_=sre_: h.tensor_mul(out=t3[:], in0=sre_[:], in1=sn[:]), reads=[B_sre_, B_tab], writes=[B_t34])
                S.op(pool, lambda h, sim_=sim_: h.tensor_mul(out=t4[:], in0=sim_[:], in1=cs[:]), reads=[B_sim_, B_tab], writes=[B_t34])
                S.op(pool, lambda h: h.tensor_add(out=sib[:], in0=t3[:], in1=t4[:]), reads=[B_t34], writes=[B_sib])
                pend_c.append(blk)
            flush_c()
            if q == 3:
                for blk in range(NBLK):
                    yb, ybb = ybanks[blk]
                    g = NBLK + blk
                    S.op(dve, lambda h, yb=yb, g=g: h.scalar_tensor_tensor(
                        out=yp[:], in0=uT[:, ut, g * TB:(g + 1) * TB], scalar=sp_t[:, C_SD + ut:C_SD + ut + 1], in1=yb[:],
                        op0=ALU.mult, op1=ALU.add), reads=[B_uT[g], ybb, B_const], writes=[B_yp])
                    S.op(dve, lambda h: h.tensor_mul(out=y2[:], in0=yp[:], in1=yp[:]), reads=[B_yp], writes=[B_yp])
                    S.op(dve, lambda h: h.tensor_scalar(out=y2[:], in0=y2[:], scalar1=0.044715, scalar2=1.0, op0=ALU.mult, op1=ALU.add),
                         reads=[B_yp], writes=[B_yp])
                    S.op(dve, lambda h: h.tensor_mul(out=y2[:], in0=y2[:], in1=yp[:]), reads=[B_yp], writes=[B_yp])
                    S.op(act, lambda h: h.activation(out=y2[:], in_=y2[:], func=AF.Sigmoid, scale=1.5957691216057308),
                         reads=[B_yp], writes=[B_yp])
                    S.op(dve, lambda h, blk=blk: h.tensor_mul(out=gT[:, ut, blk * TB:(blk + 1) * TB], in0=y2[:], in1=yp[:]),
                         reads=[B_yp], writes=[B_gT])
        wsg_t, B_wsg = wload([(kview(w_sg, 4), 4, 512)])
        wsg = wsg_t[:, 0:2048].rearrange("p (a b) -> p a b", a=4)
        for m in range(4):
            for blk in range(NBLK):
                ps_, pb = pbank()
                for kt in range(4):
                    mm(ps_[:], wsg[:, kt, m * 128:(m + 1) * 128], gT[:, kt, blk * TB:(blk + 1) * TB], kt == 0, kt == 3,
                       [B_wsg, B_gT], pb)
                S.op(act, lambda h, ps_=ps_: h.activation(out=yp[:], in_=ps_[:], func=AF.Sigmoid), reads=[pb], writes=[B_yp])
                S.op(dve, lambda h, m=m, blk=blk: h.tensor_mul(out=s5o[:, m, blk * TB:(blk + 1) * TB],
                                                              in0=gT[:, m, blk * TB:(blk + 1) * TB], in1=yp[:]),
                     reads=[B_yp, B_gT], writes=[B_s5o])
        dump("uT", uT[:], [128, 4, 2 * NTOK], BF16, B_uT)
        dump("gT", gT[:], [128, 4, NTOK], BF16, [B_gT])
        dump("s5o", s5o[:], [128, 4, NTOK], BF16, [B_s5o])
        S.barrier([dbg_ch])
    pus.close()

    mx_convert_all()
    mx_finalize()
    ch_x1 = S.chan()
    ch_sc = S.chan()
    B_x1d = Buf("x1d")
    with contextlib.ExitStack() as pm:
        alloc_xt(pm, 1)
        big = sb(pm, "big", [128, 4 * D], F32)
        cv = big[:, 0:8 * TB].rearrange("p (a b) -> p a b", a=8)
        sq = big[:, 8 * TB:16 * TB].rearrange("p (a b) -> p a b", a=8)
        res = big[:].rearrange("p (a b) -> p a b", a=4)
        B_cv = Buf("cv")
        B_sq = Buf("sq")
        B_res = [Buf("res%d" % i) for i in range(4)]
        b16a = sb(pm, "b16a", [128, 4 * D], BF16)
        xn2 = b16a[:].rearrange("p (a b) -> p a b", a=4)
        mgT = b16a[:].rearrange("p (a b) -> p a b", a=16)
        B_xn2 = Buf("xn2")
        B_mg = Buf("mgT")
        vtail = sb(pm, "vtail", [128, 8, 32], BF16)
        B_vt = Buf("vtail")
        flag = sp_t[:, C_FLAG:C_FLAG + 1]

        for blk in range(NBLK):
          with contextlib.ExitStack() as sa:
            hTb = sb(sa, "hTb%d" % blk, [128, 16, TB], BF16)
            hb = Buf("hTb")
            coT = sb(sa, "coT%d" % blk, [128, 8, TB], BF16)
            B_co = Buf("coT")
            hsl = lambda kt, hTb=hTb: hTb[:, kt, :]
            make_hT(x_own, blk, xn2, B_xn2, hTb, hb)
            S.barrier()
            sa12 = contextlib.ExitStack()
            vT = sb(sa12, "vT%d" % blk, [128, 8, 32 + TB], BF16)
            B_vT = Buf("vT")
            asb = sb(sa12, "asb%d" % blk, [128, TB], F32)
            gsb = sb(sa12, "gsb%d" % blk, [128, TB], F32)
            B_ag = Buf("ag")
            diag2 = [b16a[:, j_ * 3968:(j_ + 1) * 3968].rearrange("p (a b) -> p a b", a=31) for j_ in range(2)]
            B_dg2 = [Buf("diag0"), Buf("diag1")]
            mean, rstd, B_mr = asb, gsb, B_ag
            ctmp = xts["t"][0][:, 0:TB]
            B_ct = xts["b"][0]
            if blk > 0:
                S.op(dve, lambda h: h.tensor_copy(out=vT[:, :, 0:32], in_=vtail[:]), reads=[B_vt], writes=[B_vT])
            def conv_ld(kp):
                return mload(MX_CONV + kp)

            def conv_use(t, tb, kp, blk=blk, hb=hb, hsl=hsl):
                wa = t[:, 0:2048].rearrange("p (a b) -> p a b", a=16)
                wg = t[:, 2048:4096].rearrange("p (a b) -> p a b", a=16)
                pa_, pab = pbank()
                pg_, pgb = pbank()
                for kt in range(16):
                    mm(pa_[:], wa[:, kt, :], hsl(kt), kt == 0, kt == 15, [tb, hb], pab)
                for kt in range(16):
                    mm(pg_[:], wg[:, kt, :], hsl(kt), kt == 0, kt == 15, [tb, hb], pgb)
                ba = sp_t[:, C_BIN + 4 + kp:C_BIN + 5 + kp]
                bg = sp_t[:, C_BIN + 12 + kp:C_BIN + 13 + kp]
                S.op(act, lambda h: h.activation(out=asb[:], in_=pa_[:], func=AF.Identity, bias=ba), reads=[pab, B_const], writes=[B_ag])
                S.op(act, lambda h: h.activation(out=gsb[:], in_=pg_[:], func=AF.Sigmoid, bias=bg), reads=[pgb, B_const], writes=[B_ag])
                S.op(dve, lambda h: h.tensor_mul(out=vT[:, kp, 32:32 + TB], in0=asb[:], in1=gsb[:]), reads=[B_ag], writes=[B_vT])
                if blk == 0:
                    ph_, phb = pbank()
                    for kt in range(16):
                        mm(ph_[:, 0:32], wa[:, kt, :], hT_halo[:, kt, :], kt == 0, kt == 15, [tb, B_halo], phb)
                    for kt in range(16):
                        mm(ph_[:, 32:64], wg[:, kt, :], hT_halo[:, kt, :], kt == 0, kt == 15, [tb, B_halo], phb)
                    S.op(act, lambda h: h.activation(out=asb[:, 0:32], in_=ph_[:, 0:32], func=AF.Identity, bias=ba),
                         reads=[phb, B_const], writes=[B_ag])
                    S.op(act, lambda h: h.activation(out=gsb[:, 0:32], in_=ph_[:, 32:64], func=AF.Sigmoid, bias=bg),
                         reads=[phb, B_const], writes=[B_ag])
                    S.op(dve, lambda h: h.scalar_tensor_tensor(out=vT[:, kp, 0:32], in0=asb[:, 0:32], scalar=flag, in1=gsb[:, 0:32],
                                                               op0=ALU.mult, op1=ALU.mult), reads=[B_ag, B_const], writes=[B_vT])

            pipeline([((lambda kp=kp: conv_ld(kp)), (lambda t, tb, kp=kp: conv_use(t, tb, kp))) for kp in range(8)])

            for kp in range(8):
                diag, B_dg = diag2[kp % 2], B_dg2[kp % 2]
                S.op(dve, lambda h, kp=kp, diag=diag: h.tensor_tensor(
                    out=diag, in0=identb[:].unsqueeze(1).to_broadcast([128, 31, 128]),
                    in1=sp_t[:, C_DW + kp * 31:C_DW + (kp + 1) * 31].unsqueeze(2).to_broadcast([128, 31, 128]), op=ALU.mult),
                    reads=[B_const], writes=[B_dg])
                pc_, pcb = pbank()
                for tap in range(31):
                    mm(pc_[:], diag[:, tap, :], vT[:, kp, 2 + tap:2 + tap + TB], tap == 0, tap == 30, [B_dg, B_vT], pcb)
                bb = sp_t[:, C_DWB + kp:C_DWB + kp + 1]
                S.op(act, lambda h, kp=kp, pc_=pc_, bb=bb: h.activation(out=cv[:, kp, :], in_=pc_[:], func=AF.Identity, bias=bb),
                     reads=[pcb, B_const], writes=[B_cv])
                S.op(act, lambda h, kp=kp, pc_=pc_, bb=bb: h.activation(out=sq[:, kp, :], in_=pc_[:], func=AF.Square, bias=bb),
                     reads=[pcb, B_const], writes=[B_sq])
            S.op(dve, lambda h: h.tensor_copy(out=vtail[:], in_=vT[:, :, TB:TB + 32]), reads=[B_vT], writes=[B_vt])
            pm_, pmb = pbank()
            pq_, pqb = pbank()
            for kp in range(8):
                mm(pm_[:], onesf, cv[:, kp, :], kp == 0, kp == 7, [B_const, B_cv], pmb)
            for kp in range(8):
                mm(pq_[:], onesf, sq[:, kp, :], kp == 0, kp == 7, [B_const, B_sq], pqb)
            S.op(dve, lambda h: h.tensor_scalar(out=mean[:], in0=pm_[:], scalar1=1.0 / 1024, scalar2=None, op0=ALU.mult),
                 reads=[pmb], writes=[B_mr])
            S.op(dve, lambda h: h.tensor_mul(out=ctmp[:], in0=mean[:], in1=mean[:]), reads=[B_mr], writes=[B_ct])
            S.op(dve, lambda h: h.scalar_tensor_tensor(out=rstd[:], in0=pq_[:], scalar=1.0 / 1024, in1=ctmp[:], op0=ALU.mult, op1=ALU.subtract),
                 reads=[pqb, B_ct], writes=[B_mr])
            S.op(dve, lambda h: h.tensor_scalar_add(out=rstd[:], in0=rstd[:], scalar1=EPS), reads=[B_mr], writes=[B_mr])
            S.op(act, lambda h: h.activation(out=rstd[:], in_=rstd[:], func=AF.Sqrt), reads=[B_mr], writes=[B_mr])
            S.op(dve, lambda h: h.reciprocal(out=rstd[:], in_=rstd[:]), reads=[B_mr], writes=[B_mr])
            for kp in range(8):
                S.op(dve, lambda h, kp=kp: h.tensor_sub(out=ctmp[:], in0=cv[:, kp, :], in1=mean[:]), reads=[B_cv, B_mr], writes=[B_ct])
                S.op(dve, lambda h: h.tensor_mul(out=ctmp[:], in0=ctmp[:], in1=rstd[:]), reads=[B_ct, B_mr], writes=[B_ct])
                S.op(act, lambda h, kp=kp: h.activation(out=coT[:, kp, :], in_=ctmp[:], func=AF.Silu,
                                                        scale=sp_t[:, C_LNG + kp:C_LNG + kp + 1], bias=sp_t[:, C_LNB + kp:C_LNB + kp + 1]),
                     reads=[B_ct, B_const], writes=[B_co])

            S.barrier()
            sa12.close()
            sa3 = contextlib.ExitStack()
            sg1 = sb(sa3, "sg1%d" % blk, [128, TB], F32)
            sg2 = sb(sa3, "sg2%d" % blk, [128, TB], F32)
            B_sg = Buf("sg")
            def gate_ld(k):
                return mload(MX_GATE + k)

            def proj_ld(k):
                return mload(MX_PROJ + k)

            def gate_use(t, tb, k, blk=blk, hb=hb, hsl=hsl):
                w1 = t[:, 0:2048].rearrange("p (a b) -> p a b", a=16)
                w2 = t[:, 2048:4096].rearrange("p (a b) -> p a b", a=16)
                p1, p1b = pbank()
                p2, p2b = pbank()
                for kt in range(16):
                    mm(p1[:], w1[:, kt, :], hsl(kt), kt == 0, kt == 15, [tb, hb], p1b)
                for kt in range(16):
                    mm(p2[:], w2[:, kt, :], hsl(kt), kt == 0, kt == 15, [tb, hb], p2b)
                b1 = sp_t[:, C_BIN + 20 + k:C_BIN + 21 + k]
                b2 = sp_t[:, C_BIN + 36 + k:C_BIN + 37 + k]
                S.op(act, lambda h: h.activation(out=sg1[:], in_=p1[:], func=AF.Sigmoid, bias=b1), reads=[p1b, B_const], writes=[B_sg])
                S.op(act, lambda h: h.activation(out=sg2[:], in_=p2[:], func=AF.Sigmoid, bias=b2), reads=[p2b, B_const], writes=[B_sg])

            def proj_use(t, tb, k, blk=blk):
                wu_ = t[:, 0:512].rearrange("p (a b) -> p a b", a=4)
                wc_ = t[:, 512:1536].rearrange("p (a b) -> p a b", a=8)
                p3, p3b = pbank()
                p4, p4b = pbank()
                for kt in range(4):
                    mm(p3[:], wu_[:, kt, :], s5o[:, kt, blk * TB:(blk + 1) * TB], kt == 0, kt == 3, [tb, B_s5o], p3b)
                for kt in range(8):
                    mm(p4[:], wc_[:, kt, :], coT[:, kt, :], kt == 0, kt == 7, [tb, B_co], p4b)
                S.op(dve, lambda h: h.tensor_mul(out=sg1[:], in0=sg1[:], in1=p3[:]), reads=[B_sg, p3b], writes=[B_sg])
                S.op(dve, lambda h: h.tensor_mul(out=sg2[:], in0=sg2[:], in1=p4[:]), reads=[B_sg, p4b], writes=[B_sg])
                S.op(dve, lambda h: h.tensor_add(out=mgT[:, k, :], in0=sg1[:], in1=sg2[:]), reads=[B_sg], writes=[B_mg])

            units = []
            for k in range(16):
                units.append(((lambda k=k: gate_ld(k)), (lambda t, tb, k=k: gate_use(t, tb, k))))
                units.append(((lambda k=k: proj_ld(k)), (lambda t, tb, k=k: proj_use(t, tb, k))))
            pipeline(units)

            if blk == 0:
                dump("coT", coT[:], [128, 8, TB], BF16, [B_co])
                dump("mgT", mgT, [128, 16, TB], BF16, [B_mg])
            S.barrier([dbg_ch])
            sa3.close()
          with contextlib.ExitStack() as sb_:
            lnb1 = sb(sb_, "lnb1%d" % blk, [128, 2, D], F32)
            B_lnb = Buf("lnb1")
            ch_l = S.chan()
            S.dma(sp, lambda h: h.dma_start(out=lnb1[:, 0, :], in_=lnbc_d[0]), ch_l, writes=[B_lnb])
            S.dma(sp, lambda h: h.dma_start(out=lnb1[:, 1, :], in_=lnbc_d[1]), ch_l, writes=[B_lnb])
            B_lnb.w = (ch_l.sem, ch_l.cnt)
            x1t = sb(sb_, "x1t%d" % blk, [128, D], F32)
            B_x1t = Buf("x1t")
            h2, B_h2 = x1t, B_x1t
            h2b = sb(sb_, "h2b%d" % blk, [128, D], BF16)
            B_h2b = Buf("h2b")
            h2T = sb(sb_, "h2T%d" % blk, [128, 16, 128], F32)
            B_h2T = Buf("h2T")
            rt = sb(sb_, "rt%d" % blk, [128, 16, 64], F32)
            B_rt = Buf("rt")
            ohb = sb(sb_, "ohb%d" % blk, [128, 64], BF16)
            B_oh = Buf("ohb")
            wr = sb(sb_, "wr%d" % blk, [128, 16, 72], F32)
            brt = sb(sb_, "brt%d" % blk, [128, 72], F32)
            B_wr = Buf("wr")
            S.dma(sp, lambda h: h.dma_start(out=wr[:], in_=kview(w_rt, 16)), ch_l, writes=[B_wr])
            S.dma(sp, lambda h: h.dma_start(out=brt[:], in_=brt_d), ch_l, writes=[B_wr])
            B_wr.w = (ch_l.sem, ch_l.cnt)
            B_lnb.w = (ch_l.sem, ch_l.cnt)
            def wo_ld(fb):
                return mload(MX_WO + fb)

            def wo_use(t, tb, fb):
                wo = t[:, 0:4096].rearrange("p (a b) -> p a b", a=16)
                for tt in range(4):
                    po, pob = pbank()
                    for kt in range(16):
                        mm(po[:, 0:256], mgT[:, kt, tt * 128:(tt + 1) * 128], wo[:, kt, :], kt == 0, kt == 15, [tb, B_mg], pob)
                    S.op(dve, lambda h, tt=tt, po=po: h.tensor_mul(out=res[:, tt, fb * 256:(fb + 1) * 256], in0=po[:, 0:256],
                                                                  in1=modbc[:, 0, fb * 256:(fb + 1) * 256]),
                         reads=[pob, B_modbc], writes=[B_res[tt]])

            pipeline([((lambda fb=fb: wo_ld(fb)), (lambda t, tb, fb=fb: wo_use(t, tb, fb))) for fb in range(8)])

            for tt in range(4):
                T = blk * 4 + tt
                rows = slice(blk * TB + tt * 128, blk * TB + (tt + 1) * 128)
                t, tb = load_x(x_own[rows, :])
                r_ = res[:, tt, :]
                rb_ = B_res[tt]
                S.op(dve, lambda h, t=t, r_=r_: h.scalar_tensor_tensor(out=r_, in0=t[:], scalar=ALPHA, in1=r_, op0=ALU.mult, op1=ALU.add),
                     reads=[tb, rb_], writes=[rb_])
                ln_stats(r_, rb_)
                S.op(dve, lambda h, r_=r_: h.tensor_scalar(out=r_, in0=r_, scalar1=lnmv[:, 0:1], scalar2=lnmv[:, 2:3],
                                                         op0=ALU.subtract, op1=ALU.mult), reads=[rb_, B_ln], writes=[rb_])
                S.op(dve, lambda h, r_=r_: h.tensor_mul(out=r_, in0=r_, in1=lnb1[:, 0, :]), reads=[rb_, B_lnb], writes=[rb_])
                S.op(dve, lambda h, r_=r_: h.tensor_add(out=x1t[:], in0=r_, in1=lnb1[:, 1, :]), reads=[rb_, B_lnb], writes=[B_x1t])
                S.dma(sp, lambda h, rows=rows: h.dma_start(out=x1_d[rows, :], in_=x1t[:]), ch_x1, reads=[B_x1t], writes=[B_x1d])
                ln_stats(x1t[:], B_x1t)
                S.op(dve, lambda h: h.tensor_scalar(out=h2[:], in0=x1t[:], scalar1=lnmv[:, 0:1], scalar2=lnmv[:, 2:3],
                                                    op0=ALU.subtract, op1=ALU.mult), reads=[B_x1t, B_ln], writes=[B_h2])
                S.op(dve, lambda h: h.tensor_mul(out=h2[:], in0=h2[:], in1=modbc[:, 2, :]), reads=[B_h2, B_modbc], writes=[B_h2])
                S.op(dve, lambda h: h.tensor_add(out=h2[:], in0=h2[:], in1=modbc[:, 3, :]), reads=[B_h2, B_modbc], writes=[B_h2])
                S.op(act, lambda h: h.activation(out=h2b[:], in_=h2[:], func=AF.Copy), reads=[B_h2], writes=[B_h2b])
                for k4 in range(4):
                    pt_, ptb_ = pbank()
                    for j in range(4):
                        kt = k4 * 4 + j
                        S.op(pe, lambda h, kt=kt, j=j, pt_=pt_: h.transpose(out=pt_[:, j * 128:(j + 1) * 128],
                                                                        in_=h2[:, kt * 128:(kt + 1) * 128], identity=identf),
                             reads=[B_h2, B_const], writes=[ptb_])
                    S.op(act, lambda h, k4=k4, pt_=pt_: h.activation(
                        out=h2T[:, k4 * 4:(k4 + 1) * 4, :].rearrange("p a b -> p (a b)"), in_=pt_[:], func=AF.Copy),
                        reads=[ptb_], writes=[B_h2T])
                pl_, plb = pbank()
                for kt in range(16):
                    mm(pl_[:, 0:72], h2T[:, kt, :], wr[:, kt, :], kt == 0, kt == 15, [B_h2T, B_wr], plb)
                lg = rt[:, 0:2, :].rearrange("p a b -> p (a b)")[:, 0:72]
                V = lambda r, n=8: rt[:, r, 0:n]
                S.op(dve, lambda h: h.tensor_add(out=lg, in0=pl_[:, 0:72], in1=brt[:]), reads=[plb, B_wr], writes=[B_rt])
                o = lambda fn, **kw: S.op(dve, fn, reads=[B_rt] + kw.get("r", []), writes=[B_rt] + kw.get("w", []))
                gl = lg[:, 0:8]
                el3 = lg[:, 8:72].rearrange("p (g e) -> p g e", g=8)
                o(lambda h: h.reduce_max(out=V(2, 1), in_=gl, axis=AX.X))
                o(lambda h: h.tensor_scalar(out=V(3), in0=gl, scalar1=V(2, 1), scalar2=None, op0=ALU.is_equal))
                o(lambda h: h.tensor_scalar(out=V(4), in0=gl, scalar1=V(2, 1), scalar2=None, op0=ALU.subtract))
                S.op(act, lambda h: h.activation(out=V(4), in_=V(4), func=AF.Exp), reads=[B_rt], writes=[B_rt])
                o(lambda h: h.reduce_sum(out=V(5, 1), in_=V(4), axis=AX.X))
                o(lambda h: h.reciprocal(out=V(5, 1), in_=V(5, 1)))
                prod = rt[:, 6, :].rearrange("p (g e) -> p g e", g=8)
                o(lambda h: h.tensor_tensor(out=prod, in0=el3, in1=V(3).unsqueeze(2).to_broadcast([128, 8, 8]), op=ALU.mult))
                o(lambda h: h.reduce_sum(out=V(7), in_=rt[:, 6, :].rearrange("p (g e) -> p e g", g=8), axis=AX.X))
                o(lambda h: h.reduce_max(out=V(8, 1), in_=V(7), axis=AX.X))
                o(lambda h: h.tensor_scalar(out=V(9), in0=V(7), scalar1=V(8, 1), scalar2=None, op0=ALU.is_equal))
                o(lambda h: h.scalar_tensor_tensor(out=V(10), in0=V(9), scalar=-1e30, in1=V(7), op0=ALU.mult, op1=ALU.add))
                o(lambda h: h.reduce_max(out=V(11, 1), in_=V(10), axis=AX.X))
                o(lambda h: h.tensor_scalar(out=V(12), in0=V(10), scalar1=V(11, 1), scalar2=None, op0=ALU.is_equal))
                o(lambda h: h.tensor_sub(out=V(13, 1), in0=V(11, 1), in1=V(8, 1)))
                S.op(act, lambda h: h.activation(out=V(13, 1), in_=V(13, 1), func=AF.Exp), reads=[B_rt], writes=[B_rt])
                o(lambda h: h.tensor_scalar_add(out=V(14, 1), in0=V(13, 1), scalar1=1.0))
                o(lambda h: h.reciprocal(out=V(14, 1), in_=V(14, 1)))
                o(lambda h: h.tensor_mul(out=V(15, 1), in0=V(13, 1), in1=V(14, 1)))
                o(lambda h: h.tensor_mul(out=rinfo[:, T, 2:3], in0=V(14, 1), in1=V(5, 1)), w=[B_rinfo])
                o(lambda h: h.tensor_mul(out=rinfo[:, T, 3:4], in0=V(15, 1), in1=V(5, 1)), w=[B_rinfo])
                oh1 = rt[:, 0, :].rearrange("p (g e) -> p g e", g=8)
                oh2 = rt[:, 1, :].rearrange("p (g e) -> p g e", g=8)
                o(lambda h: h.tensor_tensor(out=oh1, in0=V(3).unsqueeze(2).to_broadcast([128, 8, 8]),
                                            in1=V(9).unsqueeze(1).to_broadcast([128, 8, 8]), op=ALU.mult))
                o(lambda h: h.tensor_tensor(out=oh2, in0=V(3).unsqueeze(2).to_broadcast([128, 8, 8]),
                                            in1=V(12).unsqueeze(1).to_broadcast([128, 8, 8]), op=ALU.mult))
                S.op(dve, lambda h: h.tensor_add(out=ohb[:], in0=rt[:, 0, :], in1=rt[:, 1, :]), reads=[B_rt], writes=[B_oh])
                pc2, pc2b = pbank()
                mm(pc2[:, 0:64], trib[:], ohb[:], True, True, [B_const, B_oh], pc2b)
                pt2, pt2b = pbank()
                mm(pt2[:, 0:64], onesb[:], ohb[:], True, True, [B_const, B_oh], pt2b)
                S.op(dve, lambda h: h.tensor_add(out=rt[:, 6, :], in0=pc2[:, 0:64], in1=basebc[:]), reads=[pc2b, B_base, B_rt], writes=[B_rt])
                S.op(dve, lambda h: h.tensor_add(out=basebc[:], in0=basebc[:], in1=pt2[:, 0:64]), reads=[pt2b, B_rt], writes=[B_base])
                for s_, ohr in ((0, 0), (1, 1)):
                    o(lambda h, ohr=ohr: h.tensor_mul(out=rt[:, 7, :], in0=rt[:, 6, :], in1=rt[:, ohr, :]))
                    o(lambda h: h.reduce_sum(out=V(8, 1), in_=rt[:, 7, :], axis=AX.X))
                    o(lambda h, ohr=ohr: h.tensor_mul(out=rt[:, 7, :], in0=iota[:, 0:64], in1=rt[:, ohr, :]), r=[B_const])
                    o(lambda h: h.reduce_sum(out=V(9, 1), in_=rt[:, 7, :], axis=AX.X))
                    o(lambda h: h.tensor_scalar(out=V(10, 1), in0=V(8, 1), scalar1=float(CAP), scalar2=1.0e6, op0=ALU.is_ge, op1=ALU.mult))
                    o(lambda h: h.scalar_tensor_tensor(out=V(9, 1), in0=V(9, 1), scalar=float(CAP), in1=V(8, 1), op0=ALU.mult, op1=ALU.add))
                    o(lambda h, s_=s_: h.tensor_add(out=rinfo[:, T, s_:s_ + 1], in0=V(9, 1), in1=V(10, 1)), w=[B_rinfo])
                S.op(dve, lambda h, T=T: h.tensor_copy(out=rdest[:, T, :], in_=rinfo[:, T, 0:2]), reads=[B_rinfo], writes=[B_rinfo])
                for s_ in range(2):
                    S.dma(pool, lambda h, T=T, s_=s_: h.indirect_dma_start(
                        out=xdisp[:, :], out_offset=bass.IndirectOffsetOnAxis(ap=rdest[:, T, s_:s_ + 1], axis=0),
                        in_=h2b[:, :], in_offset=None, bounds_check=bc_reg, oob_is_err=False),
                        ch_xd, reads=[B_h2b, B_rinfo], writes=[B_xd])
            S.barrier([ch_l])
        S.barrier([ch_x1, ch_xd])

    cv_finalize()
    ch_y = S.chan()
    B_yd = Buf("ydisp")
    with contextlib.ExitStack() as pe_:
        xg = [sb(pe_, "xg%d" % i, [128, D], BF16) for i in range(2)]
        B_xg = [Buf("xg%d" % i) for i in range(2)]
        C_xg = [S.chan() for _ in range(2)]
        xgT = sb(pe_, "xgT", [128, 16, 128], BF16)
        B_xgT = Buf("xgT")
        sil = sb(pe_, "sil", [128, 512], F32)
        B_sil = Buf("sil")
        actb = sb(pe_, "actb", [128, 512], BF16)
        B_actb = Buf("actb")
        actT = sb(pe_, "actT", [128, 4, 128], BF16)
        B_actT = Buf("actT")
        yo = [sb(pe_, "yo%d" % i, [128, D], F32) for i in range(2)]
        B_yo = [Buf("yo%d" % i) for i in range(2)]

        def ex_load(e):
            i = e % 2
            S.dma(sp, lambda h: h.dma_start(out=xg[i][:], in_=xdisp[e * CAP:(e + 1) * CAP, :]), C_xg[i], reads=[B_xd], writes=[B_xg[i]])

        ex_load(0)
        state = {}
        units = []
        for e in range(NE):
            def tr_in(e):
                i = e % 2
                if e + 1 < NE:
                    ex_load(e + 1)
                for k4 in range(4):
                    ptile, ptb = tbank()
                    for j in range(4):
                        kt = k4 * 4 + j
                        S.op(pe, lambda h, kt=kt, j=j, ptile=ptile: h.transpose(out=ptile[:, j * 128:(j + 1) * 128],
                                                                          in_=xg[i][:, kt * 128:(kt + 1) * 128], identity=identb[:]),
                             reads=[B_xg[i], B_const], writes=[ptb])
                    cast(xgT[:, k4 * 4:(k4 + 1) * 4, :].rearrange("p a b -> p (a b)"), ptile[:, 0:512], [ptb], [B_xgT], eng=[act, dve][k4 % 2])
                if e == 0:
                    dump("xg0", xg[i][:], [128, D], BF16, [B_xg[i]])
                    dump("xgT0", xgT[:], [128, 16, 128], BF16, [B_xgT])
                state["pg"] = pbank()
                state["pu"] = pbank()

            def gu_use(t, tb, which, hf, e=e):
                if which == 0 and hf == 0:
                    tr_in(e)
                w = t[:, 0:4096].rearrange("p (a b) -> p a b", a=16)
                ps_, pb = state["pg"] if which == 0 else state["pu"]
                for kt in range(16):
                    mm(ps_[:, hf * 256:(hf + 1) * 256], xgT[:, kt, :], w[:, kt, :], kt == 0, kt == 15, [tb, B_xgT], pb)
                if which == 1 and hf == 1:
                    pg_, pgb = state["pg"]
                    S.op(act, lambda h: h.activation(out=sil[:], in_=pg_[:], func=AF.Silu), reads=[pgb], writes=[B_sil])
                    S.op(dve, lambda h: h.tensor_mul(out=actb[:], in0=sil[:], in1=ps_[:]), reads=[B_sil, pb], writes=[B_actb])
                    ptile, ptb = tbank()
                    for j in range(4):
                        S.op(pe, lambda h, j=j, ptile=ptile: h.transpose(out=ptile[:, j * 128:(j + 1) * 128],
                                                                     in_=actb[:, j * 128:(j + 1) * 128], identity=identb[:]),
                             reads=[B_actb, B_const], writes=[ptb])
                    cast(actT[:].rearrange("p a b -> p (a b)"), ptile[:, 0:512], [ptb], [B_actT], eng=act)

            def dn_use(t, tb, hf, e=e):
                w = t[:, 0:4096].rearrange("p (a b) -> p a b", a=4)
                i = e % 2
                for nb in range(2):
                    ps_, pb = pbank()
                    for kt in range(4):
                        mm(ps_[:], actT[:, kt, :], w[:, kt, nb * 512:(nb + 1) * 512], kt == 0, kt == 3, [tb, B_actT], pb)
                    c0 = hf * 1024 + nb * 512
                    cast(yo[i][:, c0:c0 + 512], ps_[:], [pb], [B_yo[i]], eng=[act, dve][nb])
                if hf == 1:
                    if e == 0:
                        dump("yo0", yo[i][:], [128, D], F32, [B_yo[i]])
                        dump("actT0", actT[:], [128, 4, 128], BF16, [B_actT])
                    for hc in range(2):
                        S.dma(sp, lambda h, hc=hc: h.dma_start(out=ydh[hc][e * CAP:(e + 1) * CAP, :], in_=yo[i][:, hc * 1024:(hc + 1) * 1024]),
                              ch_y, reads=[B_yo[i]], writes=[B_yd])

            for which in (0, 1):
                for hf in range(2):
                    units.append(((lambda e=e, which=which, hf=hf: eload(e * 6 + which * 2 + hf)),
                                  (lambda t, tb, which=which, hf=hf, f=gu_use: f(t, tb, which, hf))))
            for hf in range(2):
                units.append(((lambda e=e, hf=hf: eload(e * 6 + 4 + hf)),
                              (lambda t, tb, hf=hf, f=dn_use: f(t, tb, hf))))
        pipeline(units)
        S.barrier([ch_y])

    ch_o = [S.chan() for _ in range(2)]
    with contextlib.ExitStack() as pf:
        alloc_xt(pf, 2)
        ya_ = [sb(pf, "ya%d" % i, [128, D], F32) for i in range(2)]
        yb_ = [sb(pf, "yb%d" % i, [128, D], F32) for i in range(2)]
        B_ya_ = [Buf("ya%d" % i) for i in range(2)]
        B_yb_ = [Buf("yb%d" % i) for i in range(2)]
        ch_g = [S.chan() for _ in range(4)]
        ot = [sb(pf, "ot%d" % i, [128, D], F32) for i in range(2)]
        B_ot = [Buf("ot%d" % i) for i in range(2)]
        lnb2 = sb(pf, "lnb2", [128, 2, D], F32)
        B_lnb2 = Buf("lnb2")
        ch_f = S.chan()
        S.dma(sp, lambda h: h.dma_start(out=lnb2[:, 0, :], in_=lnbc_d[2]), ch_f, writes=[B_lnb2])
        S.dma(sp, lambda h: h.dma_start(out=lnb2[:, 1, :], in_=lnbc_d[3]), ch_f, writes=[B_lnb2])
        B_lnb2.w = (ch_f.sem, ch_f.cnt)

        def fetch(T):
            k = T % 2
            rows = slice(T * 128, (T + 1) * 128)
            S.op(pool, lambda h: h.memset(ya_[k][:], 0.0), writes=[B_ya_[k]])
            S.op(pool, lambda h: h.memset(yb_[k][:], 0.0), writes=[B_yb_[k]])
            for s_, (dst, db, chn) in enumerate(((ya_[k], B_ya_[k], ch_g[2 * k]), (yb_[k], B_yb_[k], ch_g[2 * k + 1]))):
                for hc in range(2):
                    S.dma(pool, lambda h, dst=dst, s_=s_, hc=hc: h.indirect_dma_start(
                        out=dst[:, hc * 1024:(hc + 1) * 1024], out_offset=None, in_=ydh[hc][:, :],
                        in_offset=bass.IndirectOffsetOnAxis(ap=rdest[:, T, s_:s_ + 1], axis=0),
                        bounds_check=bc_reg, oob_is_err=False), chn, reads=[B_yd, B_rinfo], writes=[db])
            return load_x(x1_d[rows, :])

        pend = fetch(0)
        for T in range(16):
            rows = slice(T * 128, (T + 1) * 128)
            nxt = fetch(T + 1) if T + 1 < 16 else None
            t, tb = pend
            pend = nxt
            ya, yb2, B_ya, B_yb = ya_[T % 2], yb_[T % 2], B_ya_[T % 2], B_yb_[T % 2]
            o_ = ot[T % 2]
            ob = B_ot[T % 2]
            S.op(dve, lambda h, ya=ya: h.tensor_scalar(out=ya[:], in0=ya[:], scalar1=rinfo[:, T, 2:3], scalar2=None, op0=ALU.mult),
                 reads=[B_ya, B_rinfo], writes=[B_ya])
            S.op(dve, lambda h, ya=ya, yb2=yb2: h.scalar_tensor_tensor(out=ya[:], in0=yb2[:], scalar=rinfo[:, T, 3:4], in1=ya[:], op0=ALU.mult, op1=ALU.add),
                 reads=[B_ya, B_yb, B_rinfo], writes=[B_ya])
            S.op(dve, lambda h, ya=ya: h.tensor_mul(out=ya[:], in0=ya[:], in1=modbc[:, 1, :]), reads=[B_ya, B_modbc], writes=[B_ya])
            S.op(dve, lambda h, t=t, ya=ya: h.scalar_tensor_tensor(out=ya[:], in0=t[:], scalar=ALPHA, in1=ya[:], op0=ALU.mult, op1=ALU.add),
                 reads=[tb, B_ya], writes=[B_ya])
            ln_stats(ya[:], B_ya)
            S.op(dve, lambda h, ya=ya: h.tensor_scalar(out=ya[:], in0=ya[:], scalar1=lnmv[:, 0:1], scalar2=lnmv[:, 2:3],
                                                       op0=ALU.subtract, op1=ALU.mult), reads=[B_ya, B_ln], writes=[B_ya])
            S.op(dve, lambda h, ya=ya: h.tensor_mul(out=ya[:], in0=ya[:], in1=lnb2[:, 0, :]), reads=[B_ya, B_lnb2], writes=[B_ya])
            S.op(dve, lambda h, o_=o_, ya=ya: h.tensor_add(out=o_[:], in0=ya[:], in1=lnb2[:, 1, :]), reads=[B_ya, B_lnb2], writes=[ob])
            S.dma(sp, lambda h, o_=o_, rows=rows: h.dma_start(out=out[rows, :], in_=o_[:]), ch_o[T % 2], reads=[ob])
        dump("rinfo", rinfo[:], [128, 16, 4], F32, [B_rinfo])
        S.barrier(ch_o + [dbg_ch])
    es.close()
    return nc


def _prep_inputs(inp):
    f = lambda k: np.ascontiguousarray(np.asarray(inp[k], dtype=np.float32))
    x = f("x")
    c = f("c")
    iota = np.tile(np.arange(512, dtype=np.float32)[None, :], (128, 1))
    cst = np.zeros((128, 4, 128), np.float32)
    cst[:, 0, :] = np.eye(128, dtype=np.float32)
    cst[:, 1, :] = 1.0
    cst[:, 2, :] = np.triu(np.ones((128, 128), np.float32), 1)
    b_in = f("b_in")[0]
    a_re = f("s5_a_re")[0]
    a_im = f("s5_a_im")[0]
    ldt = f("s5_log_dt")[0]
    b_re = f("s5_b_re")[0]
    b_im = f("s5_b_im")[0]
    c_re = f("s5_c_re")[0]
    c_im = f("s5_c_im")[0]
    sd = f("s5_d")[0].reshape(512)
    dw = f("conv_dw")[0][:, 0, :]
    sm = np.zeros((128, 512), np.float32)
    sm[:, 16:68] = b_in.reshape(52, 128).T
    sm[:, 68:84] = a_re.reshape(16, 128).T
    sm[:, 84:100] = a_im.reshape(16, 128).T
    sm[:, 100:116] = np.repeat(ldt, 64).reshape(16, 128).T
    sm[:, 116:120] = sd.reshape(4, 128).T
    sm[:, 120:128] = f("conv_dw_b")[0].reshape(8, 128).T
    sm[:, 128:136] = f("conv_ln_g")[0].reshape(8, 128).T
    sm[:, 136:144] = f("conv_ln_b")[0].reshape(8, 128).T
    sm[:, 160:408] = dw.T.reshape(8, 128, 31).transpose(1, 0, 2).reshape(128, 248)
    wb = np.zeros((2, 128, 16, 128), np.float32)
    wc = np.zeros((2, 128, 16, 128), np.float32)
    for g in range(32):
        i = g // 2
        gl = g % 2
        ch0 = (g % 8) * 16
        st0 = gl * 64
        wb[0, ch0:ch0 + 16, i, st0:st0 + 64] = b_re[g].T
        wb[1, ch0:ch0 + 16, i, st0:st0 + 64] = b_im[g].T
        wc[0, st0:st0 + 64, i, ch0:ch0 + 16] = c_re[g].T
        wc[1, st0:st0 + 64, i, ch0:ch0 + 16] = c_im[g].T
    lnbc = np.stack([np.tile(f(k)[0][None, :], (128, 1)) for k in ("ln1_g", "ln1_b", "ln2_g", "ln2_b")])
    w_route = np.ascontiguousarray(np.concatenate([f("w_route_group")[0], f("w_route_expert")[0]], axis=1))
    b_route = np.tile(np.concatenate([f("b_route_group")[0], f("b_route_expert")[0]])[None, :], (128, 1)).astype(np.float32)
    shared = {
        "iota": iota, "cst": cst, "w_ada": f("w_ada")[0], "b_ada": f("b_ada"), "w_in": f("w_in")[0],
        "wb": wb, "wc": wc, "w_s5_gate": f("w_s5_gate")[0], "w_s5_up": f("w_s5_up")[0],
        "w_conv_out": f("w_conv_out")[0], "w_out": f("w_out")[0], "lnbc": lnbc, "w_route": w_route,
        "b_route": b_route, "w_exp_gate": f("w_exp_gate")[0], "w_exp_up": f("w_exp_up")[0], "w_exp_down": f("w_exp_down")[0],
    }
    maps = []
    for core in range(8):
        b, half = core // 2, core % 2
        smc = sm.copy()
        smc[:, 0:16] = c[b].reshape(16, 128).T
        smc[:, 144] = float(half)
        m = dict(shared)
        m["smallp"] = smc
        m["x_own"] = np.ascontiguousarray(x[b, half * NTOK:(half + 1) * NTOK])
        m["x_prev"] = np.ascontiguousarray(x[b, 0:NTOK]) if half == 1 else np.zeros((NTOK, D), np.float32)
        maps.append(m)
    return maps


_NC = [None]


def kernel(**inputs):
    maps = _prep_inputs(inputs)
    if _NC[0] is None:
        _NC[0] = build_nc()
    res = run_bass_kernel_spmd(_NC[0], maps, core_ids=list(range(8)))
    outp = np.zeros((4, 2 * NTOK, D), np.float32)
    for core in range(8):
        b, half = core // 2, core % 2
        outp[b, half * NTOK:(half + 1) * NTOK] = res.results[core]["out"]
    if DEBUG:
        kernel.dbg = [res.results[c] for c in range(8)]
    return outp
```

```python
import contextlib
import numpy as np
import concourse.bass as bass
import concourse.mybir as mybir
from concourse.bass_utils import run_bass_kernel_spmd

F32 = mybir.dt.float32
BF16 = mybir.dt.bfloat16
I32 = mybir.dt.int32
ALU = mybir.AluOpType
AF = mybir.ActivationFunctionType
AX = mybir.AxisListType

D = 2048
NTOK = 2048
TB = 512
NBLK = NTOK // TB
INC = 6656
NE = 64
CAP = 128
ALPHA = 2.0 ** 0.25
EPS = 1e-5
MAGIC = 12582912.0
TWO_PI = 6.283185307179586
DEBUG = False


class Buf:
    def __init__(self, name):
        self.name = name
        self.w = None
        self.r = {}


class Chan:
    def __init__(self, sem):
        self.sem = sem
        self.cnt = 0


class Eng:
    def __init__(self, h, sem, is_pe=False):
        self.h = h
        self.sem = sem
        self.cnt = 0
        self.waited = {}
        self.is_pe = is_pe


class Sched:
    def __init__(self, nc, es):
        self.nc = nc
        self.es = es
        self.nsem = 0
        self.pe = Eng(nc.tensor, self.mksem(), True)
        self.dve = Eng(nc.vector, self.mksem())
        self.act = Eng(nc.scalar, self.mksem())
        self.pool = Eng(nc.gpsimd, self.mksem())
        self.sp = Eng(nc.sync, self.mksem())
        self.engs = [self.pe, self.dve, self.act, self.pool, self.sp]

    def mksem(self):
        self.nsem += 1
        return self.es.enter_context(self.nc.semaphore("sm%d" % self.nsem))

    def chan(self):
        return Chan(self.mksem())

    def _sync(self, e, reads, writes):
        deps = []
        for b in reads:
            if b.w is not None:
                deps.append(b.w)
        for b in writes:
            if b.w is not None:
                deps.append(b.w)
            deps.extend(b.r.values())
        for (sem, val) in deps:
            if e.is_pe and sem is e.sem:
                continue
            k = id(sem)
            if e.waited.get(k, 0) >= val:
                continue
            e.h.wait_ge(sem, val)
            e.waited[k] = val

    def _mark(self, tok, reads, writes):
        k = id(tok[0])
        for b in reads:
            if b.r.get(k, (None, 0))[1] < tok[1]:
                b.r[k] = tok
        for b in writes:
            b.w = tok
            b.r = {}

    def op(self, e, fn, reads=(), writes=()):
        self._sync(e, reads, writes)
        inst = fn(e.h)
        e.cnt += 1
        inst.then_inc(e.sem, 1)
        tok = (e.sem, e.cnt)
        self._mark(tok, reads, writes)
        return tok

    def dma(self, e, fn, chan, reads=(), writes=()):
        self._sync(e, reads, writes)
        inst = fn(e.h)
        chan.cnt += 16
        inst.then_inc(chan.sem, 16)
        tok = (chan.sem, chan.cnt)
        self._mark(tok, reads, writes)
        return tok

    def barrier(self, chans=()):
        for e in self.engs:
            for o in self.engs:
                if o is e or o.cnt == 0:
                    continue
                if e.waited.get(id(o.sem), 0) < o.cnt:
                    e.h.wait_ge(o.sem, o.cnt)
                    e.waited[id(o.sem)] = o.cnt
            for c in chans:
                if c.cnt and e.waited.get(id(c.sem), 0) < c.cnt:
                    e.h.wait_ge(c.sem, c.cnt)
                    e.waited[id(c.sem)] = c.cnt


def build_nc():
    nc = bass.Bass("TRN2", target_bir_lowering=False)

    def din(name, shape, dt=F32):
        return nc.dram_tensor(name, list(shape), dt, kind="ExternalInput").ap()

    x_own = din("x_own", [NTOK, D])
    x_prev = din("x_prev", [NTOK, D])
    smallp = din("smallp", [128, 512])
    iota_d = din("iota", [128, 512])
    cst_d = din("cst", [128, 4, 128])
    w_ada = din("w_ada", [D, 6 * D])
    b_ada = din("b_ada", [1, 6 * D])
    w_in = din("w_in", [D, INC])
    wb_d = din("wb", [2, 128, 16, 128])
    wc_d = din("wc", [2, 128, 16, 128])
    w_sg = din("w_s5_gate", [512, 512])
    w_su = din("w_s5_up", [512, D])
    w_co = din("w_conv_out", [1024, D])
    w_out = din("w_out", [D, D])
    lnbc_d = din("lnbc", [4, 128, D])
    w_rt = din("w_route", [D, 72])
    brt_d = din("b_route", [128, 72])
    w_eg = din("w_exp_gate", [NE, D, 512])
    w_eu = din("w_exp_up", [NE, D, 512])
    w_ed = din("w_exp_down", [NE, 512, D])
    out = nc.dram_tensor("out", [NTOK, D], F32, kind="ExternalOutput").ap()
    x1_d = nc.dram_tensor("x1s", [NTOK, D], F32, kind="ExternalOutput" if DEBUG else "Internal").ap()
    xdisp = nc.dram_tensor("xdisp", [NE * CAP, D], BF16, kind="Internal").ap()
    ydh = [nc.dram_tensor("ydisp%d" % i, [NE * CAP, 1024], F32, kind="Internal").ap() for i in range(2)]

    es = contextlib.ExitStack()
    S = Sched(nc, es)
    pe, dve, act, pool, sp = S.pe, S.dve, S.act, S.pool, S.sp

    dbg_ch = S.chan()

    def dump(name, ap, shape, dt, reads):
        if not DEBUG:
            return
        dd = nc.dram_tensor("dbg_" + name, list(shape), dt, kind="ExternalOutput").ap()
        S.dma(sp, lambda h: h.dma_start(out=dd, in_=ap), dbg_ch, reads=reads)

    def sb(stack, name, shape, dt):
        return stack.enter_context(nc.sbuf_tensor("s_" + name, list(shape), dt))

    PS = [es.enter_context(nc.psum_tensor("ps%d" % i, [128, 512], F32)) for i in range(6)]
    PSB = [Buf("ps%d" % i) for i in range(6)]
    PT = [es.enter_context(nc.psum_tensor("pt%d" % i, [128, 1024], BF16)) for i in range(2)]
    PTB = [Buf("pt%d" % i) for i in range(2)]
    pctr = [0]

    def pbank():
        i = pctr[0] % 6
        pctr[0] += 1
        return PS[i], PSB[i]

    tctr = [0]

    def tbank():
        i = tctr[0] % 2
        tctr[0] += 1
        return PT[i], PTB[i]

    sp_t = sb(es, "smallp", [128, 512], F32)
    iota = sb(es, "iota", [128, 512], F32)
    cst = sb(es, "cst", [128, 4, 128], F32)
    identb = sb(es, "identb", [128, 128], BF16)
    onesb = sb(es, "onesb", [128, 128], BF16)
    trib = sb(es, "trib", [128, 128], BF16)
    modpp = sb(es, "modpp", [128, 4, 16], F32)
    modbc = sb(es, "modbc", [128, 4, D], F32)
    rinfo = sb(es, "rinfo", [128, 16, 4], F32)
    rdest = sb(es, "rdest", [128, 16, 2], I32)
    basebc = sb(es, "basebc", [128, 64], F32)
    B_const = Buf("const")
    B_modpp = Buf("modpp")
    B_modbc = Buf("modbc")
    B_rinfo = Buf("rinfo")
    B_base = Buf("base")
    identf = cst[:, 0, :]
    onesf = cst[:, 1, :]

    C_C = 0
    C_BIN = 16
    C_ARE = 68
    C_AIM = 84
    C_LDT = 100
    C_SD = 116
    C_DWB = 120
    C_LNG = 128
    C_LNB = 136
    C_FLAG = 144
    C_DW = 160

    ch_par = S.chan()
    S.dma(sp, lambda h: h.dma_start(out=sp_t[:], in_=smallp), ch_par, writes=[B_const])
    S.dma(sp, lambda h: h.dma_start(out=iota[:], in_=iota_d), ch_par, writes=[B_const])
    S.dma(sp, lambda h: h.dma_start(out=cst[:], in_=cst_d), ch_par, writes=[B_const])
    B_const.w = (ch_par.sem, ch_par.cnt)
    S.op(dve, lambda h: h.tensor_copy(out=identb[:], in_=cst[:, 0, :]), reads=[B_const], writes=[B_const])
    S.op(dve, lambda h: h.tensor_copy(out=onesb[:], in_=cst[:, 1, :]), reads=[B_const], writes=[B_const])
    S.op(dve, lambda h: h.tensor_copy(out=trib[:], in_=cst[:, 2, :]), reads=[B_const], writes=[B_const])
    S.op(dve, lambda h: h.memset(basebc[:], 0.0), writes=[B_base])

    B_xd = Buf("xdisp")
    ch_xd = S.chan()
    bc_reg = nc.gpsimd.to_reg(NE * CAP - 1)

    NSLOT = 6
    wbf = [sb(es, "wbf%d" % i, [128, 4096], BF16) for i in range(NSLOT)]
    wbfB = [Buf("wbf%d" % i) for i in range(NSLOT)]
    wbfC = [S.chan() for _ in range(NSLOT)]
    wctr = [0]
    fst = {"t": [], "b": [], "c": [S.chan(), S.chan(), S.chan()], "n": 0, "k": 0}

    def alloc_fst(stack, n, width):
        fst["k"] += 1
        fst["t"] = [sb(stack, "fst%d_%d" % (fst["k"], i), [128, width], F32) for i in range(n)]
        fst["b"] = [Buf("fst%d" % i) for i in range(n)]
        fst["n"] = 0

    def cast(out_ap, in_ap, reads, writes, eng=None):
        if eng is act:
            S.op(act, lambda h: h.activation(out=out_ap, in_=in_ap, func=AF.Copy), reads=reads, writes=writes)
        else:
            S.op(eng, lambda h: h.tensor_copy(out=out_ap, in_=in_ap), reads=reads, writes=writes)

    def fload(pieces):
        i = fst["n"] % len(fst["t"])
        fst["n"] += 1
        off = 0
        for (ap, a, b) in pieces:
            v = fst["t"][i][:, off:off + a * b].rearrange("p (a b) -> p a b", a=a)
            S.dma(sp, lambda h, v=v, ap=ap: h.dma_start(out=v, in_=ap), fst["c"][i], writes=[fst["b"][i]])
            off += a * b
        return fst["t"][i], fst["b"][i]

    def wload(pieces, do_cast=True, dst=None, dstB=None):
        if not do_cast:
            return fload(pieces)
        i = wctr[0] % NSLOT
        wctr[0] += 1
        off = 0
        for (ap, a, b) in pieces:
            v = wbf[i][:, off:off + a * b].rearrange("p (a b) -> p a b", a=a)
            S.dma(pool, lambda h, v=v, ap=ap: h.dma_start(out=v, in_=ap), wbfC[i], writes=[wbfB[i]])
            off += a * b
        return wbf[i], wbfB[i]

    NCV = NE * 6
    wscr_l = [nc.dram_tensor("wscr%d" % j, [NCV // 2, 128, 4096], BF16, kind="Internal").ap() for j in range(2)]
    wscr = lambda u: wscr_l[u // (NCV // 2)][u % (NCV // 2)]
    cvB = [Buf("cv%d" % u) for u in range(NCV)]
    CVG = 32
    CV_MAX = 160
    cvC = [S.chan() for _ in range(NCV // CVG)]
    cv_next = [0]

    def cv_src(u):
        e, r = divmod(u, 6)
        if r < 4:
            wsrc = w_eg if r < 2 else w_eu
            hf = r % 2
            return kview(wsrc[e], 16)[:, :, hf * 256:(hf + 1) * 256], 16
        hf = r - 4
        return kview(w_ed[e], 4)[:, :, hf * 1024:(hf + 1) * 1024], 4

    def cv_issue(n):
        for _ in range(n):
            u = cv_next[0]
            if u >= NCV:
                return
            cv_next[0] += 1
            src, a = cv_src(u)
            dstv = wscr(u).rearrange("p (a b) -> p a b", a=a)
            S.dma(pool, lambda h, dstv=dstv, src=src: h.dma_start(out=dstv, in_=src), cvC[u // CVG], writes=[cvB[u]])

    def cv_finalize():
        for u in range(cv_next[0]):
            c = cvC[u // CVG]
            cvB[u].w = (c.sem, c.cnt)

    def eload(u):
        if u >= cv_next[0]:
            src, a = cv_src(u)
            return wload([(src, a, 4096 // a)])
        i = wctr[0] % NSLOT
        wctr[0] += 1
        S.dma(sp, lambda h: h.dma_start(out=wbf[i][:], in_=wscr(u)), wbfC[i], reads=[cvB[u]], writes=[wbfB[i]])
        return wbf[i], wbfB[i]

    def pipeline(units, depth=4):
        loaded = []
        n = len(units)
        for i in range(n + depth):
            if i < n:
                loaded.append(units[i][0]())
            j = i - depth
            if j >= 0:
                units[j][1](*loaded[j])
                loaded[j] = None

    def kview(ap2d, kt):
        return ap2d.rearrange("(kt p) n -> p kt n", p=128)

    w_in_v = kview(w_in, 16)
    w_ada_v = kview(w_ada, 16)

    mx_pieces = []
    for kp_ in range(8):
        mx_pieces.append([(w_in_v[:, :, 512 + kp_ * 128:512 + (kp_ + 1) * 128], 16, 128),
                          (w_in_v[:, :, 1536 + kp_ * 128:1536 + (kp_ + 1) * 128], 16, 128)])
    for k_ in range(16):
        mx_pieces.append([(w_in_v[:, :, 2560 + k_ * 128:2560 + (k_ + 1) * 128], 16, 128),
                          (w_in_v[:, :, 4608 + k_ * 128:4608 + (k_ + 1) * 128], 16, 128)])
    for k_ in range(16):
        mx_pieces.append([(kview(w_su, 4)[:, :, k_ * 128:(k_ + 1) * 128], 4, 128),
                          (kview(w_co, 8)[:, :, k_ * 128:(k_ + 1) * 128], 8, 128)])
    for fb_ in range(8):
        mx_pieces.append([(kview(w_out, 16)[:, :, fb_ * 256:(fb_ + 1) * 256], 16, 256)])
    MX_CONV, MX_GATE, MX_PROJ, MX_WO = 0, 8, 24, 40
    mscr = nc.dram_tensor("mscr", [48, 128, 4096], BF16, kind="Internal").ap()
    mxB = [Buf("mx%d" % j) for j in range(48)]
    mxC = S.chan()

    mxq = []
    for j_ in range(48):
        off_ = 0
        for (ap_, a_, b_) in mx_pieces[j_]:
            mxq.append((j_, off_, ap_, a_, b_))
            off_ += a_ * b_

    def mx_issue(n):
        k = 0
        while k < n and mxq:
            j, off, ap, a, b = mxq.pop(0)
            dstv = mscr[j][:, off:off + a * b].rearrange("p (a b) -> p a b", a=a)
            S.dma(pool, lambda h, dstv=dstv, ap=ap: h.dma_start(out=dstv, in_=ap), mxC, writes=[mxB[j]])
            k += 1
        return k

    def mx_convert_all():
        mx_issue(10 ** 6)

    def mx_finalize():
        for j in range(48):
            mxB[j].w = (mxC.sem, mxC.cnt)

    mload_n = [0]

    def mload(j):
        mload_n[0] += 1
        if mload_n[0] % 2 == 0:
            cv_issue(1)
        i = wctr[0] % NSLOT
        wctr[0] += 1
        S.dma(sp, lambda h: h.dma_start(out=wbf[i][:], in_=mscr[j]), wbfC[i], reads=[mxB[j]], writes=[wbfB[i]])
        return wbf[i], wbfB[i]


    def mm(ps, lhsT, rhs, start, stop, reads, pbuf):
        S.op(pe, lambda h: h.matmul(ps, lhsT=lhsT, rhs=rhs, start=start, stop=stop), reads=reads, writes=[pbuf])

    with contextlib.ExitStack() as pa:
        zt = sb(pa, "zt", [128, D], BF16)
        B_zt = Buf("zt")
        S.op(pool, lambda h: h.memset(zt[:], 0.0), writes=[B_zt])
        for e in range(NE):
            S.dma(pool, lambda h, e=e: h.dma_start(out=xdisp[e * CAP:(e + 1) * CAP, :], in_=zt[:]), ch_xd,
                  reads=[B_zt], writes=[B_xd])
        alloc_fst(pa, 3, D)
        cact = sb(pa, "cact", [128, 16], F32)
        cb = sb(pa, "cb", [128, 16, 128], F32)
        R = sb(pa, "R", [128, D], F32)
        bada = sb(pa, "bada", [1, D], F32)
        tmp3 = sb(pa, "tmp3", [128, 16, 128], F32)
        B_c = Buf("cact")
        B_R = Buf("R")
        B_ba = Buf("bada")
        B_t3 = Buf("tmp3")
        ch_a = S.chan()
        S.op(act, lambda h: h.activation(out=cact[:], in_=sp_t[:, C_C:C_C + 16], func=AF.Silu),
             reads=[B_const], writes=[B_c])
        for kt in range(16):
            S.op(dve, lambda h, kt=kt: h.tensor_copy(out=cb[:, kt, :], in_=cact[:, kt:kt + 1].to_broadcast([128, 128])),
                 reads=[B_c], writes=[B_c])
        pp_dst = {0: (0, False), 1: (1, True), 3: (2, False), 4: (3, True)}
        bc_dst = {2: [(0, 1.0)], 5: [(1, 1.0)], 4: [(2, 1.0)], 3: [(3, 0.0)]}
        for grp in range(6):
            banks = [pbank() for _ in range(4)]
            S.dma(sp, lambda h, grp=grp: h.dma_start(out=bada[:], in_=b_ada[:, grp * D:(grp + 1) * D]), ch_a, writes=[B_ba])

            def ld(grp, kt):
                return wload([(w_ada_v[:, kt:kt + 1, grp * D:(grp + 1) * D], 1, D)], do_cast=False)

            def use(t, tb, kt, banks):
                for nb in range(4):
                    mm(banks[nb][0][:], cb[:, kt, :], t[:, nb * 512:(nb + 1) * 512], kt == 0, False,
                       [tb, B_c], banks[nb][1])

            units = [((lambda kt=kt, grp=grp: ld(grp, kt)), (lambda t, tb, kt=kt, banks=banks: use(t, tb, kt, banks)))
                     for kt in range(16)]
            pipeline(units, depth=2)
            for nb in range(4):
                c0 = nb * 512
                mm(banks[nb][0][:], cst[0:1, 1, :], bada[0:1, c0:c0 + 512], False, True, [B_ba, B_const], banks[nb][1])
                S.op(act, lambda h, nb=nb, c0=c0, banks=banks: h.activation(out=R[:, c0:c0 + 512], in_=banks[nb][0][:], func=AF.Copy),
                     reads=[banks[nb][1]], writes=[B_R])
            if grp in pp_dst:
                vi, plus1 = pp_dst[grp]
                S.op(dve, lambda h: h.tensor_tensor(
                    out=tmp3[:], in0=R[:].rearrange("p (a b) -> p a b", a=16),
                    in1=identf.unsqueeze(1).to_broadcast([128, 16, 128]), op=ALU.mult),
                    reads=[B_R, B_const], writes=[B_t3])
                S.op(dve, lambda h, vi=vi: h.reduce_sum(out=modpp[:, vi, :], in_=tmp3[:], axis=AX.X),
                     reads=[B_t3], writes=[B_modpp])
                if plus1:
                    S.op(dve, lambda h, vi=vi: h.tensor_scalar_add(out=modpp[:, vi, :], in0=modpp[:, vi, :], scalar1=1.0),
                         reads=[B_modpp], writes=[B_modpp])
            for (di, add) in bc_dst.get(grp, []):
                S.op(dve, lambda h, di=di, add=add: h.tensor_scalar_add(out=modbc[:, di, :], in0=R[:], scalar1=add),
                     reads=[B_R], writes=[B_modbc])
        dump("modpp", modpp[:], [128, 4, 16], F32, [B_modpp])
        dump("modbc", modbc[:], [128, 4, D], F32, [B_modbc])
        S.barrier([ch_a, ch_xd, dbg_ch])

    s5p = sb(es, "s5p", [128, 12, 16], F32)
    B_s5p = Buf("s5p")
    hT_halo = sb(es, "hT_halo", [128, 16, 32], BF16)
    B_halo = Buf("halo")
    s5o = sb(es, "s5o", [128, 4, NTOK], BF16)
    B_s5o = Buf("s5o")
    lnst = sb(es, "lnst", [128, 4, 6], F32)
    lnmv = sb(es, "lnmv", [128, 4], F32)
    B_ln = Buf("lnst")
    pus = contextlib.ExitStack()
    wbb = sb(pus, "wbb", [128, 2, 16, 128], BF16)
    wcb = sb(pus, "wcb", [128, 2, 16, 128], BF16)
    B_wb = Buf("wbb")
    uT = sb(pus, "uT", [128, 4, 2 * NTOK], BF16)
    B_uT = [Buf("uT%d" % g) for g in range(2 * NBLK)]

    def sincos(eng, ang_ap, cos_out, sin_out, rb, wbuf, t, a, Bt):
        for (shift, outp) in ((0.0, sin_out), (np.pi / 2, cos_out)):
            S.op(eng, lambda h, shift=shift: h.tensor_scalar(out=a[:], in0=ang_ap, scalar1=float(shift), scalar2=None, op0=ALU.add),
                 reads=rb, writes=[Bt])
            S.op(eng, lambda h: h.tensor_scalar(out=t[:], in0=a[:], scalar1=1.0 / TWO_PI, scalar2=MAGIC, op0=ALU.mult, op1=ALU.add),
                 reads=[Bt], writes=[Bt])
            S.op(eng, lambda h: h.tensor_scalar(out=t[:], in0=t[:], scalar1=-MAGIC, scalar2=None, op0=ALU.add),
                 reads=[Bt], writes=[Bt])
            S.op(eng, lambda h: h.scalar_tensor_tensor(out=a[:], in0=t[:], scalar=-TWO_PI, in1=a[:], op0=ALU.mult, op1=ALU.add),
                 reads=[Bt], writes=[Bt])
            S.op(eng, lambda h: h.tensor_scalar(out=a[:], in0=a[:], scalar1=3.1415925, scalar2=-3.1415925, op0=ALU.min, op1=ALU.max),
                 reads=[Bt], writes=[Bt])
            S.op(act, lambda h, outp=outp: h.activation(out=outp, in_=a[:], func=AF.Sin), reads=[Bt], writes=wbuf)

    with contextlib.ExitStack() as pp:
        alloc_fst(pp, 2, 512)
        are = sp_t[:, C_ARE:C_ARE + 16]
        aim = sp_t[:, C_AIM:C_AIM + 16]
        sc = lambda i: s5p[:, i, :]
        S.op(act, lambda h: h.activation(out=sc(6), in_=sp_t[:, C_LDT:C_LDT + 16], func=AF.Exp), reads=[B_const], writes=[B_s5p])
        S.op(dve, lambda h: h.tensor_mul(out=sc(7), in0=are, in1=sc(6)), reads=[B_s5p, B_const], writes=[B_s5p])
        S.op(dve, lambda h: h.tensor_mul(out=sc(5), in0=aim, in1=sc(6)), reads=[B_s5p, B_const], writes=[B_s5p])
        S.op(act, lambda h: h.activation(out=sc(0), in_=sc(7), func=AF.Exp), reads=[B_s5p], writes=[B_s5p])
        sct = sb(pp, "sct", [128, 16], F32)
        sca = sb(pp, "sca", [128, 16], F32)
        sincos(dve, sc(5), sc(1), sc(2), [B_s5p], [B_s5p], sct, sca, Buf("scp"))
        S.op(dve, lambda h: h.tensor_mul(out=sc(8), in0=sc(0), in1=sc(1)), reads=[B_s5p], writes=[B_s5p])
        S.op(dve, lambda h: h.tensor_scalar_add(out=sc(8), in0=sc(8), scalar1=-1.0), reads=[B_s5p], writes=[B_s5p])
        S.op(dve, lambda h: h.tensor_mul(out=sc(9), in0=sc(0), in1=sc(2)), reads=[B_s5p], writes=[B_s5p])
        S.op(dve, lambda h: h.tensor_mul(out=sc(10), in0=are, in1=are), reads=[B_s5p, B_const], writes=[B_s5p])
        S.op(dve, lambda h: h.tensor_mul(out=sc(11), in0=aim, in1=aim), reads=[B_s5p, B_const], writes=[B_s5p])
        S.op(dve, lambda h: h.tensor_add(out=sc(10), in0=sc(10), in1=sc(11)), reads=[B_s5p], writes=[B_s5p])
        S.op(dve, lambda h: h.reciprocal(out=sc(10), in_=sc(10)), reads=[B_s5p], writes=[B_s5p])
        S.op(dve, lambda h: h.tensor_mul(out=sc(3), in0=sc(8), in1=are), reads=[B_s5p, B_const], writes=[B_s5p])
        S.op(dve, lambda h: h.tensor_mul(out=sc(11), in0=sc(9), in1=aim), reads=[B_s5p, B_const], writes=[B_s5p])
        S.op(dve, lambda h: h.tensor_add(out=sc(3), in0=sc(3), in1=sc(11)), reads=[B_s5p], writes=[B_s5p])
        S.op(dve, lambda h: h.tensor_mul(out=sc(3), in0=sc(3), in1=sc(10)), reads=[B_s5p], writes=[B_s5p])
        S.op(dve, lambda h: h.tensor_mul(out=sc(4), in0=sc(9), in1=are), reads=[B_s5p, B_const], writes=[B_s5p])
        S.op(dve, lambda h: h.tensor_mul(out=sc(11), in0=sc(8), in1=aim), reads=[B_s5p, B_const], writes=[B_s5p])
        S.op(dve, lambda h: h.tensor_sub(out=sc(4), in0=sc(4), in1=sc(11)), reads=[B_s5p], writes=[B_s5p])
        S.op(dve, lambda h: h.tensor_mul(out=sc(4), in0=sc(4), in1=sc(10)), reads=[B_s5p], writes=[B_s5p])
        dump("s5p", s5p[:], [128, 12, 16], F32, [B_s5p])
        for pl in range(2):
            for hf in range(4):
                t, tb = wload([(wb_d[pl, :, hf * 4:(hf + 1) * 4, :], 4, 128)], do_cast=False)
                S.op(dve, lambda h, t=t, pl=pl, hf=hf: h.tensor_copy(
                    out=wbb[:, pl, hf * 4:(hf + 1) * 4, :], in_=t[:, 0:512].rearrange("p (a b) -> p a b", a=4)),
                    reads=[tb], writes=[B_wb])
                t, tb = wload([(wc_d[pl, :, hf * 4:(hf + 1) * 4, :], 4, 128)], do_cast=False)
                S.op(dve, lambda h, t=t, pl=pl, hf=hf: h.tensor_scalar(
                    out=wcb[:, pl, hf * 4:(hf + 1) * 4, :], in0=t[:, 0:512].rearrange("p (a b) -> p a b", a=4),
                    scalar1=(1.0 if pl == 0 else -1.0), scalar2=None, op0=ALU.mult),
                    reads=[tb], writes=[B_wb])
        S.barrier()


    def ln_stats(x_ap, xb):
        for c in range(4):
            S.op(dve, lambda h, c=c: h.bn_stats(out=lnst[:, c, :], in_=x_ap[:, c * 512:(c + 1) * 512]),
                 reads=[xb], writes=[B_ln])
        S.op(dve, lambda h: h.bn_aggr(out=lnmv[:, 0:2], in_=lnst[:].rearrange("p a b -> p (a b)")), reads=[B_ln], writes=[B_ln])
        S.op(dve, lambda h: h.tensor_scalar_add(out=lnmv[:, 3:4], in0=lnmv[:, 1:2], scalar1=EPS), reads=[B_ln], writes=[B_ln])
        S.op(act, lambda h: h.activation(out=lnmv[:, 3:4], in_=lnmv[:, 3:4], func=AF.Sqrt), reads=[B_ln], writes=[B_ln])
        S.op(dve, lambda h: h.reciprocal(out=lnmv[:, 2:3], in_=lnmv[:, 3:4]), reads=[B_ln], writes=[B_ln])

    xts = {"t": [], "b": [], "c": [S.chan(), S.chan()], "n": 0, "k": 0}

    def alloc_xt(stack, n):
        xts["k"] += 1
        xts["t"] = [sb(stack, "xt%d_%d" % (xts["k"], i), [128, D], F32) for i in range(n)]
        xts["b"] = [Buf("xt%d" % i) for i in range(n)]
        xts["n"] = 0

    def load_x(src_ap):
        i = xts["n"] % len(xts["t"])
        xts["n"] += 1
        tt_, bb_ = xts["t"][i], xts["b"][i]
        S.dma(sp, lambda h: h.dma_start(out=tt_[:], in_=src_ap), xts["c"][i], writes=[bb_])
        return tt_, bb_

    with contextlib.ExitStack() as pu:
        alloc_xt(pu, 2)
        xn = sb(pu, "xn", [128, 4, D], BF16)
        B_xn = Buf("xn")
        hTp = [sb(pu, "hTp%d" % i, [128, 16, TB], BF16) for i in range(1)]
        B_hTp = [Buf("hTp%d" % i) for i in range(1)]
        def make_hT(src, blk, xn_t, xn_b, hdst, hbuf):
            for tt in range(4):
                t, tb = load_x(src[blk * TB + tt * 128: blk * TB + (tt + 1) * 128, :])
                ln_stats(t, tb)
                S.op(dve, lambda h, t=t, tt=tt: h.tensor_scalar(out=xn_t[:, tt, :], in0=t[:], scalar1=lnmv[:, 0:1], scalar2=lnmv[:, 2:3],
                                                              op0=ALU.subtract, op1=ALU.mult),
                     reads=[tb, B_ln], writes=[xn_b])
            for kt in range(16):
                ptile, ptb = tbank()
                for tt in range(4):
                    S.op(pe, lambda h, kt=kt, tt=tt, ptile=ptile: h.transpose(
                        out=ptile[:, tt * 128:(tt + 1) * 128], in_=xn_t[:, tt, kt * 128:(kt + 1) * 128], identity=identb[:]),
                        reads=[xn_b, B_const], writes=[ptb])
                S.op(act, lambda h, kt=kt, ptile=ptile: h.activation(
                    out=hdst[:, kt, :], in_=ptile[:, 0:512], func=AF.Identity,
                    scale=modpp[:, 1, kt:kt + 1], bias=modpp[:, 0, kt:kt + 1]),
                    reads=[ptb, B_modpp], writes=[hbuf])

        for g in range(2 * NBLK):
            own = g >= NBLK
            blk = g - NBLK if own else g
            src = x_own if own else x_prev
            make_hT(src, blk, xn, B_xn, hTp[0], B_hTp[0])
            def u_use(t, tb, hf, g=g):
                w = t[:, 0:4096].rearrange("p (a b) -> p a b", a=16)
                for m2 in range(2):
                    m = hf * 2 + m2
                    ps, pb = pbank()
                    for kt in range(16):
                        mm(ps[:], w[:, kt, m2 * 128:(m2 + 1) * 128], hTp[0][:, kt, :], kt == 0, kt == 15, [tb, B_hTp[0]], pb)
                    S.op(act, lambda h, m=m, ps=ps, g=g: h.activation(
                        out=uT[:, m, g * TB:(g + 1) * TB], in_=ps[:], func=AF.Identity, bias=sp_t[:, C_BIN + m:C_BIN + m + 1]),
                        reads=[pb, B_const], writes=[B_uT[g]])

            pipeline([((lambda hf=hf: wload([(w_in_v[:, :, hf * 256:(hf + 1) * 256], 16, 256)])),
                       (lambda t, tb, hf=hf: u_use(t, tb, hf))) for hf in range(2)])
            if g == NBLK - 1:
                S.op(dve, lambda h: h.tensor_copy(out=hT_halo[:], in_=hTp[0][:, :, TB - 32:TB]),
                     reads=[B_hTp[0]], writes=[B_halo])
        S.barrier()

    with contextlib.ExitStack() as ps5:
        cs = sb(ps5, "cs", [128, TB], F32)
        sn = sb(ps5, "sn", [128, TB], F32)
        mqr = sb(ps5, "mqr", [128, TB], F32)
        mqi = sb(ps5, "mqi", [128, TB], F32)
        dec = sb(ps5, "dec", [128, TB], F32)
        B_tab = Buf("tab")
        bre = sb(ps5, "bre", [128, TB], F32)
        bim = sb(ps5, "bim", [128, TB], F32)
        B_b = Buf("b")
        t1 = sb(ps5, "t1", [128, TB], F32)
        t2 = sb(ps5, "t2", [128, TB], F32)
        t3 = sb(ps5, "t3", [128, TB], F32)
        t4 = sb(ps5, "t4", [128, TB], F32)
        B_t12 = Buf("t12")
        B_t34 = Buf("t34")
        ang = t1
        sct2, sca2, B_sc2 = t3, t4, B_t34
        mre = sb(ps5, "mre", [128, TB], F32)
        mim = sb(ps5, "mim", [128, TB], F32)
        B_mre = Buf("mre")
        B_mim = Buf("mim")
        sre = sb(ps5, "sre", [128, TB], F32)
        sim = sb(ps5, "sim", [128, TB], F32)
        B_sre = Buf("sre")
        B_sim = Buf("sim")
        srb = sb(ps5, "srb", [128, TB], BF16)
        sib = sb(ps5, "sib", [128, TB], BF16)
        B_srb = Buf("srb")
        B_sib = Buf("sib")
        st = sb(ps5, "st", [128, 8], F32)
        B_st = Buf("st")
        gT = sb(ps5, "gT", [128, 4, NTOK], BF16)
        B_gT = Buf("gT")
        yp, y2, B_yp = bre, bim, B_b
        sreX = sb(ps5, "sreX", [128, TB], F32)
        simX = sb(ps5, "simX", [128, TB], F32)
        bre2, bim2, B_b2 = [bre, bre], [bim, bim], [B_b, B_b]
        sre2, sim2 = [sre, sreX], [sim, simX]
        B_sre2, B_sim2 = [B_sre, Buf("sreX")], [B_sim, Buf("simX")]
        ybanks = None
        for i in range(16):
            q = i % 4
            ut = i // 4
            if q == 0:
                ybanks = [(PS[b_], PSB[b_]) for b_ in range(NBLK)]
            th = s5p[:, 5, i:i + 1]
            S.op(dve, lambda h, th=th: h.tensor_scalar(out=ang[:], in0=iota[:], scalar1=th, scalar2=None, op0=ALU.mult),
                 reads=[B_const, B_s5p], writes=[B_t12])
            sincos(dve, ang[:], cs[:], sn[:], [B_t12], [B_tab], sct2, sca2, B_sc2)
            qre = s5p[:, 3, i:i + 1]
            qim = s5p[:, 4, i:i + 1]
            S.op(dve, lambda h, qre=qre: h.tensor_scalar(out=mqr[:], in0=cs[:], scalar1=qre, scalar2=None, op0=ALU.mult),
                 reads=[B_tab, B_s5p], writes=[B_tab])
            S.op(dve, lambda h, qim=qim: h.scalar_tensor_tensor(out=mqr[:], in0=sn[:], scalar=qim, in1=mqr[:], op0=ALU.mult, op1=ALU.add),
                 reads=[B_tab, B_s5p], writes=[B_tab])
            S.op(dve, lambda h, qim=qim: h.tensor_scalar(out=mqi[:], in0=cs[:], scalar1=qim, scalar2=None, op0=ALU.mult),
                 reads=[B_tab, B_s5p], writes=[B_tab])
            S.op(dve, lambda h, qre=qre: h.tensor_scalar(out=t1[:], in0=sn[:], scalar1=qre, scalar2=None, op0=ALU.mult),
                 reads=[B_tab, B_s5p], writes=[B_t12])
            S.op(dve, lambda h: h.tensor_sub(out=mqi[:], in0=mqi[:], in1=t1[:]), reads=[B_tab, B_t12], writes=[B_tab])
            S.op(dve, lambda h, i=i: h.tensor_copy(out=dec[:], in_=s5p[:, 0, i:i + 1].to_broadcast([128, TB])),
                 reads=[B_s5p], writes=[B_tab])
            S.op(dve, lambda h: h.memset(st[:], 0.0), writes=[B_st])
            cth = s5p[:, 1, i:i + 1]
            sth = s5p[:, 2, i:i + 1]
            LL = TB - 1
            S.op(dve, lambda h, sth=sth: h.tensor_scalar(out=st[:, 4:5], in0=sn[:, LL:LL + 1], scalar1=sth, scalar2=None, op0=ALU.mult),
                 reads=[B_tab, B_s5p, B_st], writes=[B_st])
            S.op(dve, lambda h, cth=cth: h.scalar_tensor_tensor(out=st[:, 6:7], in0=cs[:, LL:LL + 1], scalar=cth, in1=st[:, 4:5],
                                                                op0=ALU.mult, op1=ALU.subtract), reads=[B_tab, B_s5p, B_st], writes=[B_st])
            S.op(dve, lambda h, cth=cth: h.tensor_scalar(out=st[:, 5:6], in0=sn[:, LL:LL + 1], scalar1=cth, scalar2=None, op0=ALU.mult),
                 reads=[B_tab, B_s5p, B_st], writes=[B_st])
            S.op(dve, lambda h, sth=sth: h.scalar_tensor_tensor(out=st[:, 7:8], in0=cs[:, LL:LL + 1], scalar=sth, in1=st[:, 5:6],
                                                                op0=ALU.mult, op1=ALU.add), reads=[B_tab, B_s5p, B_st], writes=[B_st])
            pend_c = []

            def flush_c(i=i, q=q, pend_c=pend_c, ybanks=ybanks):
                while pend_c:
                    b_ = pend_c.pop(0)
                    yb, ybb = ybanks[b_]
                    mm(yb[:], wcb[:, 0, i, :], srb[:], q == 0, False, [B_wb, B_srb], ybb)
                    mm(yb[:], wcb[:, 1, i, :], sib[:], False, q == 3, [B_wb, B_sib], ybb)

            for g in range(2 * NBLK):
                own = g >= NBLK
                blk = g - NBLK
                n_mx = mx_issue(2)
                if n_mx < 2 and cv_next[0] < CV_MAX:
                    cv_issue(2 - n_mx)
                pr, prb = PS[4], PSB[4]
                pi, pib = PS[5], PSB[5]
                par = g % 2
                bre_, bim_, B_b_ = bre2[par], bim2[par], B_b2[par]
                sre_, sim_, B_sre_, B_sim_ = sre2[par], sim2[par], B_sre2[par], B_sim2[par]
                mm(pr[:], wbb[:, 0, i, :], uT[:, ut, g * TB:(g + 1) * TB], True, True, [B_wb, B_uT[g]], prb)
                mm(pi[:], wbb[:, 1, i, :], uT[:, ut, g * TB:(g + 1) * TB], True, True, [B_wb, B_uT[g]], pib)
                flush_c()
                S.op(dve, lambda h, pr=pr: h.tensor_mul(out=t1[:], in0=pr[:], in1=mqr[:]), reads=[prb, B_tab], writes=[B_t12])
                S.op(dve, lambda h, pi=pi: h.tensor_mul(out=t2[:], in0=pi[:], in1=mqi[:]), reads=[pib, B_tab], writes=[B_t12])
                S.op(dve, lambda h: h.tensor_sub(out=mre[:], in0=t1[:], in1=t2[:]), reads=[B_t12], writes=[B_mre])
                S.op(dve, lambda h, pr=pr: h.tensor_mul(out=t1[:], in0=pr[:], in1=mqi[:]), reads=[prb, B_tab], writes=[B_t12])
                S.op(dve, lambda h, pi=pi: h.tensor_mul(out=t2[:], in0=pi[:], in1=mqr[:]), reads=[pib, B_tab], writes=[B_t12])
                S.op(dve, lambda h: h.tensor_add(out=mim[:], in0=t1[:], in1=t2[:]), reads=[B_t12], writes=[B_mim])
                S.op(dve, lambda h, sre_=sre_: h.tensor_tensor_scan(out=sre_[:], data0=dec[:], data1=mre[:], initial=st[:, 2:3],
                                                                    op0=ALU.mult, op1=ALU.add), reads=[B_tab, B_mre, B_st], writes=[B_sre_])
                S.op(dve, lambda h, sim_=sim_: h.tensor_tensor_scan(out=sim_[:], data0=dec[:], data1=mim[:], initial=st[:, 3:4],
                                                                    op0=ALU.mult, op1=ALU.add), reads=[B_tab, B_mim, B_st], writes=[B_sim_])
                L = TB - 1
                S.op(dve, lambda h, sim_=sim_: h.tensor_scalar(out=st[:, 4:5], in0=sim_[:, L:L + 1], scalar1=st[:, 7:8], scalar2=None, op0=ALU.mult),
                     reads=[B_sim_, B_st], writes=[B_st])
                S.op(dve, lambda h, sre_=sre_: h.scalar_tensor_tensor(out=st[:, 2:3], in0=sre_[:, L:L + 1], scalar=st[:, 6:7], in1=st[:, 4:5],
                                                                      op0=ALU.mult, op1=ALU.subtract), reads=[B_sre_, B_st], writes=[B_st])
                S.op(dve, lambda h, sim_=sim_: h.tensor_scalar(out=st[:, 5:6], in0=sim_[:, L:L + 1], scalar1=st[:, 6:7], scalar2=None, op0=ALU.mult),
                     reads=[B_sim_, B_st], writes=[B_st])
                S.op(dve, lambda h, sre_=sre_: h.scalar_tensor_tensor(out=st[:, 3:4], in0=sre_[:, L:L + 1], scalar=st[:, 7:8], in1=st[:, 5:6],
                                                                      op0=ALU.mult, op1=ALU.add), reads=[B_sre_, B_st], writes=[B_st])
                if g == NBLK - 1:
                    S.op(dve, lambda h: h.tensor_scalar(out=st[:, 2:4], in0=st[:, 2:4], scalar1=sp_t[:, C_FLAG:C_FLAG + 1],
                                                        scalar2=None, op0=ALU.mult), reads=[B_st, B_const], writes=[B_st])
                if not own:
                    continue
                S.op(pool, lambda h, sre_=sre_: h.tensor_mul(out=t3[:], in0=sre_[:], in1=cs[:]), reads=[B_sre_, B_tab], writes=[B_t34])
                S.op(pool, lambda h, sim_=sim_: h.tensor_mul(out=t4[:], in0=sim_[:], in1=sn[:]), reads=[B_sim_, B_tab], writes=[B_t34])
                S.op(pool, lambda h: h.tensor_sub(out=srb[:], in0=t3[:], in1=t4[:]), reads=[B_t34], writes=[B_srb])
                S.op(pool, lambda h, sre_=sre_: h.tensor_mul(out=t3[:], in0=sre_[:], in1=sn[:]), reads=[B_sre_, B_tab], writes=[B_t34])
                S.op(pool, lambda h, sim_=sim_: h.tensor_mul(out=t4[:], in0=sim_[:], in1=cs[:]), reads=[B_sim_, B_tab], writes=[B_t34])
                S.op(pool, lambda h: h.tensor_add(out=sib[:], in0=t3[:], in1=t4[:]), reads=[B_t34], writes=[B_sib])
                pend_c.append(blk)
            flush_c()
            if q == 3:
                for blk in range(NBLK):
                    yb, ybb = ybanks[blk]
                    g = NBLK + blk
                    S.op(dve, lambda h, yb=yb, g=g: h.scalar_tensor_tensor(
                        out=yp[:], in0=uT[:, ut, g * TB:(g + 1) * TB], scalar=sp_t[:, C_SD + ut:C_SD + ut + 1], in1=yb[:],
                        op0=ALU.mult, op1=ALU.add), reads=[B_uT[g], ybb, B_const], writes=[B_yp])
                    S.op(dve, lambda h: h.tensor_mul(out=y2[:], in0=yp[:], in1=yp[:]), reads=[B_yp], writes=[B_yp])
                    S.op(dve, lambda h: h.tensor_scalar(out=y2[:], in0=y2[:], scalar1=0.044715, scalar2=1.0, op0=ALU.mult, op1=ALU.add),
                         reads=[B_yp], writes=[B_yp])
                    S.op(dve, lambda h: h.tensor_mul(out=y2[:], in0=y2[:], in1=yp[:]), reads=[B_yp], writes=[B_yp])
                    S.op(act, lambda h: h.activation(out=y2[:], in_=y2[:], func=AF.Sigmoid, scale=1.5957691216057308),
                         reads=[B_yp], writes=[B_yp])
                    S.op(dve, lambda h, blk=blk: h.tensor_mul(out=gT[:, ut, blk * TB:(blk + 1) * TB], in0=y2[:], in1=yp[:]),
                         reads=[B_yp], writes=[B_gT])
        wsg_t, B_wsg = wload([(kview(w_sg, 4), 4, 512)])
        wsg = wsg_t[:, 0:2048].rearrange("p (a b) -> p a b", a=4)
        for m in range(4):
            for blk in range(NBLK):
                ps_, pb = pbank()
                for kt in range(4):
                    mm(ps_[:], wsg[:, kt, m * 128:(m + 1) * 128], gT[:, kt, blk * TB:(blk + 1) * TB], kt == 0, kt == 3,
                       [B_wsg, B_gT], pb)
                S.op(act, lambda h, ps_=ps_: h.activation(out=yp[:], in_=ps_[:], func=AF.Sigmoid), reads=[pb], writes=[B_yp])
                S.op(dve, lambda h, m=m, blk=blk: h.tensor_mul(out=s5o[:, m, blk * TB:(blk + 1) * TB],
                                                              in0=gT[:, m, blk * TB:(blk + 1) * TB], in1=yp[:]),
                     reads=[B_yp, B_gT], writes=[B_s5o])
        dump("uT", uT[:], [128, 4, 2 * NTOK], BF16, B_uT)
        dump("gT", gT[:], [128, 4, NTOK], BF16, [B_gT])
        dump("s5o", s5o[:], [128, 4, NTOK], BF16, [B_s5o])
        S.barrier([dbg_ch])
    pus.close()

    mx_convert_all()
    mx_finalize()
    ch_x1 = S.chan()
    ch_sc = S.chan()
    B_x1d = Buf("x1d")
    with contextlib.ExitStack() as pm:
        alloc_xt(pm, 1)
        big = sb(pm, "big", [128, 4 * D], F32)
        cv = big[:, 0:8 * TB].rearrange("p (a b) -> p a b", a=8)
        sq = big[:, 8 * TB:16 * TB].rearrange("p (a b) -> p a b", a=8)
        res = big[:].rearrange("p (a b) -> p a b", a=4)
        B_cv = Buf("cv")
        B_sq = Buf("sq")
        B_res = [Buf("res%d" % i) for i in range(4)]
        b16a = sb(pm, "b16a", [128, 4 * D], BF16)
        xn2 = b16a[:].rearrange("p (a b) -> p a b", a=4)
        mgT = b16a[:].rearrange("p (a b) -> p a b", a=16)
        B_xn2 = Buf("xn2")
        B_mg = Buf("mgT")
        vtail = sb(pm, "vtail", [128, 8, 32], BF16)
        B_vt = Buf("vtail")
        flag = sp_t[:, C_FLAG:C_FLAG + 1]

        for blk in range(NBLK):
          with contextlib.ExitStack() as sa:
            hTb = sb(sa, "hTb%d" % blk, [128, 16, TB], BF16)
            hb = Buf("hTb")
            coT = sb(sa, "coT%d" % blk, [128, 8, TB], BF16)
            B_co = Buf("coT")
            hsl = lambda kt, hTb=hTb: hTb[:, kt, :]
            make_hT(x_own, blk, xn2, B_xn2, hTb, hb)
            S.barrier()
            sa12 = contextlib.ExitStack()
            vT = sb(sa12, "vT%d" % blk, [128, 8, 32 + TB], BF16)
            B_vT = Buf("vT")
            asb = sb(sa12, "asb%d" % blk, [128, TB], F32)
            gsb = sb(sa12, "gsb%d" % blk, [128, TB], F32)
            B_ag = Buf("ag")
            diag2 = [b16a[:, j_ * 3968:(j_ + 1) * 3968].rearrange("p (a b) -> p a b", a=31) for j_ in range(2)]
            B_dg2 = [Buf("diag0"), Buf("diag1")]
            mean, rstd, B_mr = asb, gsb, B_ag
            ctmp = xts["t"][0][:, 0:TB]
            B_ct = xts["b"][0]
            if blk > 0:
                S.op(dve, lambda h: h.tensor_copy(out=vT[:, :, 0:32], in_=vtail[:]), reads=[B_vt], writes=[B_vT])
            def conv_ld(kp):
                return mload(MX_CONV + kp)

            def conv_use(t, tb, kp, blk=blk, hb=hb, hsl=hsl):
                wa = t[:, 0:2048].rearrange("p (a b) -> p a b", a=16)
                wg = t[:, 2048:4096].rearrange("p (a b) -> p a b", a=16)
                pa_, pab = pbank()
                pg_, pgb = pbank()
                for kt in range(16):
                    mm(pa_[:], wa[:, kt, :], hsl(kt), kt == 0, kt == 15, [tb, hb], pab)
                for kt in range(16):
                    mm(pg_[:], wg[:, kt, :], hsl(kt), kt == 0, kt == 15, [tb, hb], pgb)
                ba = sp_t[:, C_BIN + 4 + kp:C_BIN + 5 + kp]
                bg = sp_t[:, C_BIN + 12 + kp:C_BIN + 13 + kp]
                S.op(act, lambda h: h.activation(out=asb[:], in_=pa_[:], func=AF.Identity, bias=ba), reads=[pab, B_const], writes=[B_ag])
                S.op(act, lambda h: h.activation(out=gsb[:], in_=pg_[:], func=AF.Sigmoid, bias=bg), reads=[pgb, B_const], writes=[B_ag])
                S.op(dve, lambda h: h.tensor_mul(out=vT[:, kp, 32:32 + TB], in0=asb[:], in1=gsb[:]), reads=[B_ag], writes=[B_vT])
                if blk == 0:
                    ph_, phb = pbank()
                    for kt in range(16):
                        mm(ph_[:, 0:32], wa[:, kt, :], hT_halo[:, kt, :], kt == 0, kt == 15, [tb, B_halo], phb)
                    for kt in range(16):
                        mm(ph_[:, 32:64], wg[:, kt, :], hT_halo[:, kt, :], kt == 0, kt == 15, [tb, B_halo], phb)
                    S.op(act, lambda h: h.activation(out=asb[:, 0:32], in_=ph_[:, 0:32], func=AF.Identity, bias=ba),
                         reads=[phb, B_const], writes=[B_ag])
                    S.op(act, lambda h: h.activation(out=gsb[:, 0:32], in_=ph_[:, 32:64], func=AF.Sigmoid, bias=bg),
                         reads=[phb, B_const], writes=[B_ag])
                    S.op(dve, lambda h: h.scalar_tensor_tensor(out=vT[:, kp, 0:32], in0=asb[:, 0:32], scalar=flag, in1=gsb[:, 0:32],
                                                               op0=ALU.mult, op1=ALU.mult), reads=[B_ag, B_const], writes=[B_vT])

            pipeline([((lambda kp=kp: conv_ld(kp)), (lambda t, tb, kp=kp: conv_use(t, tb, kp))) for kp in range(8)])

            for kp in range(8):
                diag, B_dg = diag2[kp % 2], B_dg2[kp % 2]
                S.op(dve, lambda h, kp=kp, diag=diag: h.tensor_tensor(
                    out=diag, in0=identb[:].unsqueeze(1).to_broadcast([128, 31, 128]),
                    in1=sp_t[:, C_DW + kp * 31:C_DW + (kp + 1) * 31].unsqueeze(2).to_broadcast([128, 31, 128]), op=ALU.mult),
                    reads=[B_const], writes=[B_dg])
                pc_, pcb = pbank()
                for tap in range(31):
                    mm(pc_[:], diag[:, tap, :], vT[:, kp, 2 + tap:2 + tap + TB], tap == 0, tap == 30, [B_dg, B_vT], pcb)
                bb = sp_t[:, C_DWB + kp:C_DWB + kp + 1]
                S.op(act, lambda h, kp=kp, pc_=pc_, bb=bb: h.activation(out=cv[:, kp, :], in_=pc_[:], func=AF.Identity, bias=bb),
                     reads=[pcb, B_const], writes=[B_cv])
                S.op(act, lambda h, kp=kp, pc_=pc_, bb=bb: h.activation(out=sq[:, kp, :], in_=pc_[:], func=AF.Square, bias=bb),
                     reads=[pcb, B_const], writes=[B_sq])
            S.op(dve, lambda h: h.tensor_copy(out=vtail[:], in_=vT[:, :, TB:TB + 32]), reads=[B_vT], writes=[B_vt])
            pm_, pmb = pbank()
            pq_, pqb = pbank()
            for kp in range(8):
                mm(pm_[:], onesf, cv[:, kp, :], kp == 0, kp == 7, [B_const, B_cv], pmb)
            for kp in range(8):
                mm(pq_[:], onesf, sq[:, kp, :], kp == 0, kp == 7, [B_const, B_sq], pqb)
            S.op(dve, lambda h: h.tensor_scalar(out=mean[:], in0=pm_[:], scalar1=1.0 / 1024, scalar2=None, op0=ALU.mult),
                 reads=[pmb], writes=[B_mr])
            S.op(dve, lambda h: h.tensor_mul(out=ctmp[:], in0=mean[:], in1=mean[:]), reads=[B_mr], writes=[B_ct])
            S.op(dve, lambda h: h.scalar_tensor_tensor(out=rstd[:], in0=pq_[:], scalar=1.0 / 1024, in1=ctmp[:], op0=ALU.mult, op1=ALU.subtract),
                 reads=[pqb, B_ct], writes=[B_mr])
            S.op(dve, lambda h: h.tensor_scalar_add(out=rstd[:], in0=rstd[:], scalar1=EPS), reads=[B_mr], writes=[B_mr])
            S.op(act, lambda h: h.activation(out=rstd[:], in_=rstd[:], func=AF.Sqrt), reads=[B_mr], writes=[B_mr])
            S.op(dve, lambda h: h.reciprocal(out=rstd[:], in_=rstd[:]), reads=[B_mr], writes=[B_mr])
            for kp in range(8):
                S.op(dve, lambda h, kp=kp: h.tensor_sub(out=ctmp[:], in0=cv[:, kp, :], in1=mean[:]), reads=[B_cv, B_mr], writes=[B_ct])
                S.op(dve, lambda h: h.tensor_mul(out=ctmp[:], in0=ctmp[:], in1=rstd[:]), reads=[B_ct, B_mr], writes=[B_ct])
                S.op(act, lambda h, kp=kp: h.activation(out=coT[:, kp, :], in_=ctmp[:], func=AF.Silu,
                                                        scale=sp_t[:, C_LNG + kp:C_LNG + kp + 1], bias=sp_t[:, C_LNB + kp:C_LNB + kp + 1]),
                     reads=[B_ct, B_const], writes=[B_co])

            S.barrier()
            sa12.close()
            sa3 = contextlib.ExitStack()
            sg1 = sb(sa3, "sg1%d" % blk, [128, TB], F32)
            sg2 = sb(sa3, "sg2%d" % blk, [128, TB], F32)
            B_sg = Buf("sg")
            def gate_ld(k):
                return mload(MX_GATE + k)

            def proj_ld(k):
                return mload(MX_PROJ + k)

            def gate_use(t, tb, k, blk=blk, hb=hb, hsl=hsl):
                w1 = t[:, 0:2048].rearrange("p (a b) -> p a b", a=16)
                w2 = t[:, 2048:4096].rearrange("p (a b) -> p a b", a=16)
                p1, p1b = pbank()
                p2, p2b = pbank()
                for kt in range(16):
                    mm(p1[:], w1[:, kt, :], hsl(kt), kt == 0, kt == 15, [tb, hb], p1b)
                for kt in range(16):
                    mm(p2[:], w2[:, kt, :], hsl(kt), kt == 0, kt == 15, [tb, hb], p2b)
                b1 = sp_t[:, C_BIN + 20 + k:C_BIN + 21 + k]
                b2 = sp_t[:, C_BIN + 36 + k:C_BIN + 37 + k]
                S.op(act, lambda h: h.activation(out=sg1[:], in_=p1[:], func=AF.Sigmoid, bias=b1), reads=[p1b, B_const], writes=[B_sg])
                S.op(act, lambda h: h.activation(out=sg2[:], in_=p2[:], func=AF.Sigmoid, bias=b2), reads=[p2b, B_const], writes=[B_sg])

            def proj_use(t, tb, k, blk=blk):
                wu_ = t[:, 0:512].rearrange("p (a b) -> p a b", a=4)
                wc_ = t[:, 512:1536].rearrange("p (a b) -> p a b", a=8)
                p3, p3b = pbank()
                p4, p4b = pbank()
                for kt in range(4):
                    mm(p3[:], wu_[:, kt, :], s5o[:, kt, blk * TB:(blk + 1) * TB], kt == 0, kt == 3, [tb, B_s5o], p3b)
                for kt in range(8):
                    mm(p4[:], wc_[:, kt, :], coT[:, kt, :], kt == 0, kt == 7, [tb, B_co], p4b)
                S.op(dve, lambda h: h.tensor_mul(out=sg1[:], in0=sg1[:], in1=p3[:]), reads=[B_sg, p3b], writes=[B_sg])
                S.op(dve, lambda h: h.tensor_mul(out=sg2[:], in0=sg2[:], in1=p4[:]), reads=[B_sg, p4b], writes=[B_sg])
                S.op(dve, lambda h: h.tensor_add(out=mgT[:, k, :], in0=sg1[:], in1=sg2[:]), reads=[B_sg], writes=[B_mg])

            units = []
            for k in range(16):
                units.append(((lambda k=k: gate_ld(k)), (lambda t, tb, k=k: gate_use(t, tb, k))))
                units.append(((lambda k=k: proj_ld(k)), (lambda t, tb, k=k: proj_use(t, tb, k))))
            pipeline(units)

            if blk == 0:
                dump("coT", coT[:], [128, 8, TB], BF16, [B_co])
                dump("mgT", mgT, [128, 16, TB], BF16, [B_mg])
            S.barrier([dbg_ch])
            sa3.close()
          with contextlib.ExitStack() as sb_:
            lnb1 = sb(sb_, "lnb1%d" % blk, [128, 2, D], F32)
            B_lnb = Buf("lnb1")
            ch_l = S.chan()
            S.dma(sp, lambda h: h.dma_start(out=lnb1[:, 0, :], in_=lnbc_d[0]), ch_l, writes=[B_lnb])
            S.dma(sp, lambda h: h.dma_start(out=lnb1[:, 1, :], in_=lnbc_d[1]), ch_l, writes=[B_lnb])
            B_lnb.w = (ch_l.sem, ch_l.cnt)
            x1t = sb(sb_, "x1t%d" % blk, [128, D], F32)
            B_x1t = Buf("x1t")
            h2, B_h2 = x1t, B_x1t
            h2b = sb(sb_, "h2b%d" % blk, [128, D], BF16)
            B_h2b = Buf("h2b")
            h2T = sb(sb_, "h2T%d" % blk, [128, 16, 128], F32)
            B_h2T = Buf("h2T")
            rt = sb(sb_, "rt%d" % blk, [128, 16, 64], F32)
            B_rt = Buf("rt")
            ohb = sb(sb_, "ohb%d" % blk, [128, 64], BF16)
            B_oh = Buf("ohb")
            wr = sb(sb_, "wr%d" % blk, [128, 16, 72], F32)
            brt = sb(sb_, "brt%d" % blk, [128, 72], F32)
            B_wr = Buf("wr")
            S.dma(sp, lambda h: h.dma_start(out=wr[:], in_=kview(w_rt, 16)), ch_l, writes=[B_wr])
            S.dma(sp, lambda h: h.dma_start(out=brt[:], in_=brt_d), ch_l, writes=[B_wr])
            B_wr.w = (ch_l.sem, ch_l.cnt)
            B_lnb.w = (ch_l.sem, ch_l.cnt)
            def wo_ld(fb):
                return mload(MX_WO + fb)

            def wo_use(t, tb, fb):
                wo = t[:, 0:4096].rearrange("p (a b) -> p a b", a=16)
                for tt in range(4):
                    po, pob = pbank()
                    for kt in range(16):
                        mm(po[:, 0:256], mgT[:, kt, tt * 128:(tt + 1) * 128], wo[:, kt, :], kt == 0, kt == 15, [tb, B_mg], pob)
                    S.op(dve, lambda h, tt=tt, po=po: h.tensor_mul(out=res[:, tt, fb * 256:(fb + 1) * 256], in0=po[:, 0:256],
                                                                  in1=modbc[:, 0, fb * 256:(fb + 1) * 256]),
                         reads=[pob, B_modbc], writes=[B_res[tt]])

            pipeline([((lambda fb=fb: wo_ld(fb)), (lambda t, tb, fb=fb: wo_use(t, tb, fb))) for fb in range(8)])

            for tt in range(4):
                T = blk * 4 + tt
                rows = slice(blk * TB + tt * 128, blk * TB + (tt + 1) * 128)
                t, tb = load_x(x_own[rows, :])
                r_ = res[:, tt, :]
                rb_ = B_res[tt]
                S.op(dve, lambda h, t=t, r_=r_: h.scalar_tensor_tensor(out=r_, in0=t[:], scalar=ALPHA, in1=r_, op0=ALU.mult, op1=ALU.add),
                     reads=[tb, rb_], writes=[rb_])
                ln_stats(r_, rb_)
                S.op(dve, lambda h, r_=r_: h.tensor_scalar(out=r_, in0=r_, scalar1=lnmv[:, 0:1], scalar2=lnmv[:, 2:3],
                                                         op0=ALU.subtract, op1=ALU.mult), reads=[rb_, B_ln], writes=[rb_])
                S.op(dve, lambda h, r_=r_: h.tensor_mul(out=r_, in0=r_, in1=lnb1[:, 0, :]), reads=[rb_, B_lnb], writes=[rb_])
                S.op(dve, lambda h, r_=r_: h.tensor_add(out=x1t[:], in0=r_, in1=lnb1[:, 1, :]), reads=[rb_, B_lnb], writes=[B_x1t])
                S.dma(sp, lambda h, rows=rows: h.dma_start(out=x1_d[rows, :], in_=x1t[:]), ch_x1, reads=[B_x1t], writes=[B_x1d])
                ln_stats(x1t[:], B_x1t)
                S.op(dve, lambda h: h.tensor_scalar(out=h2[:], in0=x1t[:], scalar1=lnmv[:, 0:1], scalar2=lnmv[:, 2:3],
                                                    op0=ALU.subtract, op1=ALU.mult), reads=[B_x1t, B_ln], writes=[B_h2])
                S.op(dve, lambda h: h.tensor_mul(out=h2[:], in0=h2[:], in1=modbc[:, 2, :]), reads=[B_h2, B_modbc], writes=[B_h2])
                S.op(dve, lambda h: h.tensor_add(out=h2[:], in0=h2[:], in1=modbc[:, 3, :]), reads=[B_h2, B_modbc], writes=[B_h2])
                S.op(act, lambda h: h.activation(out=h2b[:], in_=h2[:], func=AF.Copy), reads=[B_h2], writes=[B_h2b])
                for k4 in range(4):
                    pt_, ptb_ = pbank()
                    for j in range(4):
                        kt = k4 * 4 + j
                        S.op(pe, lambda h, kt=kt, j=j, pt_=pt_: h.transpose(out=pt_[:, j * 128:(j + 1) * 128],
                                                                        in_=h2[:, kt * 128:(kt + 1) * 128], identity=identf),
                             reads=[B_h2, B_const], writes=[ptb_])
                    S.op(act, lambda h, k4=k4, pt_=pt_: h.activation(
                        out=h2T[:, k4 * 4:(k4 + 1) * 4, :].rearrange("p a b -> p (a b)"), in_=pt_[:], func=AF.Copy),
                        reads=[ptb_], writes=[B_h2T])
                pl_, plb = pbank()
                for kt in range(16):
                    mm(pl_[:, 0:72], h2T[:, kt, :], wr[:, kt, :], kt == 0, kt == 15, [B_h2T, B_wr], plb)
                lg = rt[:, 0:2, :].rearrange("p a b -> p (a b)")[:, 0:72]
                V = lambda r, n=8: rt[:, r, 0:n]
                S.op(dve, lambda h: h.tensor_add(out=lg, in0=pl_[:, 0:72], in1=brt[:]), reads=[plb, B_wr], writes=[B_rt])
                o = lambda fn, **kw: S.op(dve, fn, reads=[B_rt] + kw.get("r", []), writes=[B_rt] + kw.get("w", []))
                gl = lg[:, 0:8]
                el3 = lg[:, 8:72].rearrange("p (g e) -> p g e", g=8)
                o(lambda h: h.reduce_max(out=V(2, 1), in_=gl, axis=AX.X))
                o(lambda h: h.tensor_scalar(out=V(3), in0=gl, scalar1=V(2, 1), scalar2=None, op0=ALU.is_equal))
                o(lambda h: h.tensor_scalar(out=V(4), in0=gl, scalar1=V(2, 1), scalar2=None, op0=ALU.subtract))
                S.op(act, lambda h: h.activation(out=V(4), in_=V(4), func=AF.Exp), reads=[B_rt], writes=[B_rt])
                o(lambda h: h.reduce_sum(out=V(5, 1), in_=V(4), axis=AX.X))
                o(lambda h: h.reciprocal(out=V(5, 1), in_=V(5, 1)))
                prod = rt[:, 6, :].rearrange("p (g e) -> p g e", g=8)
                o(lambda h: h.tensor_tensor(out=prod, in0=el3, in1=V(3).unsqueeze(2).to_broadcast([128, 8, 8]), op=ALU.mult))
                o(lambda h: h.reduce_sum(out=V(7), in_=rt[:, 6, :].rearrange("p (g e) -> p e g", g=8), axis=AX.X))
                o(lambda h: h.reduce_max(out=V(8, 1), in_=V(7), axis=AX.X))
                o(lambda h: h.tensor_scalar(out=V(9), in0=V(7), scalar1=V(8, 1), scalar2=None, op0=ALU.is_equal))
                o(lambda h: h.scalar_tensor_tensor(out=V(10), in0=V(9), scalar=-1e30, in1=V(7), op0=ALU.mult, op1=ALU.add))
                o(lambda h: h.reduce_max(out=V(11, 1), in_=V(10), axis=AX.X))
                o(lambda h: h.tensor_scalar(out=V(12), in0=V(10), scalar1=V(11, 1), scalar2=None, op0=ALU.is_equal))
                o(lambda h: h.tensor_sub(out=V(13, 1), in0=V(11, 1), in1=V(8, 1)))
                S.op(act, lambda h: h.activation(out=V(13, 1), in_=V(13, 1), func=AF.Exp), reads=[B_rt], writes=[B_rt])
                o(lambda h: h.tensor_scalar_add(out=V(14, 1), in0=V(13, 1), scalar1=1.0))
                o(lambda h: h.reciprocal(out=V(14, 1), in_=V(14, 1)))
                o(lambda h: h.tensor_mul(out=V(15, 1), in0=V(13, 1), in1=V(14, 1)))
                o(lambda h: h.tensor_mul(out=rinfo[:, T, 2:3], in0=V(14, 1), in1=V(5, 1)), w=[B_rinfo])
                o(lambda h: h.tensor_mul(out=rinfo[:, T, 3:4], in0=V(15, 1), in1=V(5, 1)), w=[B_rinfo])
                oh1 = rt[:, 0, :].rearrange("p (g e) -> p g e", g=8)
                oh2 = rt[:, 1, :].rearrange("p (g e) -> p g e", g=8)
                o(lambda h: h.tensor_tensor(out=oh1, in0=V(3).unsqueeze(2).to_broadcast([128, 8, 8]),
                                            in1=V(9).unsqueeze(1).to_broadcast([128, 8, 8]), op=ALU.mult))
                o(lambda h: h.tensor_tensor(out=oh2, in0=V(3).unsqueeze(2).to_broadcast([128, 8, 8]),
                                            in1=V(12).unsqueeze(1).to_broadcast([128, 8, 8]), op=ALU.mult))
                S.op(dve, lambda h: h.tensor_add(out=ohb[:], in0=rt[:, 0, :], in1=rt[:, 1, :]), reads=[B_rt], writes=[B_oh])
                pc2, pc2b = pbank()
                mm(pc2[:, 0:64], trib[:], ohb[:], True, True, [B_const, B_oh], pc2b)
                pt2, pt2b = pbank()
                mm(pt2[:, 0:64], onesb[:], ohb[:], True, True, [B_const, B_oh], pt2b)
                S.op(dve, lambda h: h.tensor_add(out=rt[:, 6, :], in0=pc2[:, 0:64], in1=basebc[:]), reads=[pc2b, B_base, B_rt], writes=[B_rt])
                S.op(dve, lambda h: h.tensor_add(out=basebc[:], in0=basebc[:], in1=pt2[:, 0:64]), reads=[pt2b, B_rt], writes=[B_base])
                for s_, ohr in ((0, 0), (1, 1)):
                    o(lambda h, ohr=ohr: h.tensor_mul(out=rt[:, 7, :], in0=rt[:, 6, :], in1=rt[:, ohr, :]))
                    o(lambda h: h.reduce_sum(out=V(8, 1), in_=rt[:, 7, :], axis=AX.X))
                    o(lambda h, ohr=ohr: h.tensor_mul(out=rt[:, 7, :], in0=iota[:, 0:64], in1=rt[:, ohr, :]), r=[B_const])
                    o(lambda h: h.reduce_sum(out=V(9, 1), in_=rt[:, 7, :], axis=AX.X))
                    o(lambda h: h.tensor_scalar(out=V(10, 1), in0=V(8, 1), scalar1=float(CAP), scalar2=1.0e6, op0=ALU.is_ge, op1=ALU.mult))
                    o(lambda h: h.scalar_tensor_tensor(out=V(9, 1), in0=V(9, 1), scalar=float(CAP), in1=V(8, 1), op0=ALU.mult, op1=ALU.add))
                    o(lambda h, s_=s_: h.tensor_add(out=rinfo[:, T, s_:s_ + 1], in0=V(9, 1), in1=V(10, 1)), w=[B_rinfo])
                S.op(dve, lambda h, T=T: h.tensor_copy(out=rdest[:, T, :], in_=rinfo[:, T, 0:2]), reads=[B_rinfo], writes=[B_rinfo])
                for s_ in range(2):
                    S.dma(pool, lambda h, T=T, s_=s_: h.indirect_dma_start(
                        out=xdisp[:, :], out_offset=bass.IndirectOffsetOnAxis(ap=rdest[:, T, s_:s_ + 1], axis=0),
                        in_=h2b[:, :], in_offset=None, bounds_check=bc_reg, oob_is_err=False),
                        ch_xd, reads=[B_h2b, B_rinfo], writes=[B_xd])
            S.barrier([ch_l])
        S.barrier([ch_x1, ch_xd])

    cv_finalize()
    ch_y = S.chan()
    B_yd = Buf("ydisp")
    with contextlib.ExitStack() as pe_:
        xg = [sb(pe_, "xg%d" % i, [128, D], BF16) for i in range(2)]
        B_xg = [Buf("xg%d" % i) for i in range(2)]
        C_xg = [S.chan() for _ in range(2)]
        xgT = sb(pe_, "xgT", [128, 16, 128], BF16)
        B_xgT = Buf("xgT")
        sil = sb(pe_, "sil", [128, 512], F32)
        B_sil = Buf("sil")
        actb = sb(pe_, "actb", [128, 512], BF16)
        B_actb = Buf("actb")
        actT = sb(pe_, "actT", [128, 4, 128], BF16)
        B_actT = Buf("actT")
        yo = [sb(pe_, "yo%d" % i, [128, D], F32) for i in range(2)]
        B_yo = [Buf("yo%d" % i) for i in range(2)]

        def ex_load(e):
            i = e % 2
            S.dma(sp, lambda h: h.dma_start(out=xg[i][:], in_=xdisp[e * CAP:(e + 1) * CAP, :]), C_xg[i], reads=[B_xd], writes=[B_xg[i]])

        ex_load(0)
        state = {}
        units = []
        for e in range(NE):
            def tr_in(e):
                i = e % 2
                if e + 1 < NE:
                    ex_load(e + 1)
                for k4 in range(4):
                    ptile, ptb = tbank()
                    for j in range(4):
                        kt = k4 * 4 + j
                        S.op(pe, lambda h, kt=kt, j=j, ptile=ptile: h.transpose(out=ptile[:, j * 128:(j + 1) * 128],
                                                                          in_=xg[i][:, kt * 128:(kt + 1) * 128], identity=identb[:]),
                             reads=[B_xg[i], B_const], writes=[ptb])
                    cast(xgT[:, k4 * 4:(k4 + 1) * 4, :].rearrange("p a b -> p (a b)"), ptile[:, 0:512], [ptb], [B_xgT], eng=[act, dve][k4 % 2])
                if e == 0:
                    dump("xg0", xg[i][:], [128, D], BF16, [B_xg[i]])
                    dump("xgT0", xgT[:], [128, 16, 128], BF16, [B_xgT])
                state["pg"] = pbank()
                state["pu"] = pbank()

            def gu_use(t, tb, which, hf, e=e):
                if which == 0 and hf == 0:
                    tr_in(e)
                w = t[:, 0:4096].rearrange("p (a b) -> p a b", a=16)
                ps_, pb = state["pg"] if which == 0 else state["pu"]
                for kt in range(16):
                    mm(ps_[:, hf * 256:(hf + 1) * 256], xgT[:, kt, :], w[:, kt, :], kt == 0, kt == 15, [tb, B_xgT], pb)
                if which == 1 and hf == 1:
                    pg_, pgb = state["pg"]
                    S.op(act, lambda h: h.activation(out=sil[:], in_=pg_[:], func=AF.Silu), reads=[pgb], writes=[B_sil])
                    S.op(dve, lambda h: h.tensor_mul(out=actb[:], in0=sil[:], in1=ps_[:]), reads=[B_sil, pb], writes=[B_actb])
                    ptile, ptb = tbank()
                    for j in range(4):
                        S.op(pe, lambda h, j=j, ptile=ptile: h.transpose(out=ptile[:, j * 128:(j + 1) * 128],
                                                                     in_=actb[:, j * 128:(j + 1) * 128], identity=identb[:]),
                             reads=[B_actb, B_const], writes=[ptb])
                    cast(actT[:].rearrange("p a b -> p (a b)"), ptile[:, 0:512], [ptb], [B_actT], eng=act)

            def dn_use(t, tb, hf, e=e):
                w = t[:, 0:4096].rearrange("p (a b) -> p a b", a=4)
                i = e % 2
                for nb in range(2):
                    ps_, pb = pbank()
                    for kt in range(4):
                        mm(ps_[:], actT[:, kt, :], w[:, kt, nb * 512:(nb + 1) * 512], kt == 0, kt == 3, [tb, B_actT], pb)
                    c0 = hf * 1024 + nb * 512
                    cast(yo[i][:, c0:c0 + 512], ps_[:], [pb], [B_yo[i]], eng=[act, dve][nb])
                if hf == 1:
                    if e == 0:
                        dump("yo0", yo[i][:], [128, D], F32, [B_yo[i]])
                        dump("actT0", actT[:], [128, 4, 128], BF16, [B_actT])
                    for hc in range(2):
                        S.dma(sp, lambda h, hc=hc: h.dma_start(out=ydh[hc][e * CAP:(e + 1) * CAP, :], in_=yo[i][:, hc * 1024:(hc + 1) * 1024]),
                              ch_y, reads=[B_yo[i]], writes=[B_yd])

            for which in (0, 1):
                for hf in range(2):
                    units.append(((lambda e=e, which=which, hf=hf: eload(e * 6 + which * 2 + hf)),
                                  (lambda t, tb, which=which, hf=hf, f=gu_use: f(t, tb, which, hf))))
            for hf in range(2):
                units.append(((lambda e=e, hf=hf: eload(e * 6 + 4 + hf)),
                              (lambda t, tb, hf=hf, f=dn_use: f(t, tb, hf))))
        pipeline(units)
        S.barrier([ch_y])

    ch_o = [S.chan() for _ in range(2)]
    with contextlib.ExitStack() as pf:
        alloc_xt(pf, 2)
        ya_ = [sb(pf, "ya%d" % i, [128, D], F32) for i in range(2)]
        yb_ = [sb(pf, "yb%d" % i, [128, D], F32) for i in range(2)]
        B_ya_ = [Buf("ya%d" % i) for i in range(2)]
        B_yb_ = [Buf("yb%d" % i) for i in range(2)]
        ch_g = [S.chan() for _ in range(4)]
        ot = [sb(pf, "ot%d" % i, [128, D], F32) for i in range(2)]
        B_ot = [Buf("ot%d" % i) for i in range(2)]
        lnb2 = sb(pf, "lnb2", [128, 2, D], F32)
        B_lnb2 = Buf("lnb2")
        ch_f = S.chan()
        S.dma(sp, lambda h: h.dma_start(out=lnb2[:, 0, :], in_=lnbc_d[2]), ch_f, writes=[B_lnb2])
        S.dma(sp, lambda h: h.dma_start(out=lnb2[:, 1, :], in_=lnbc_d[3]), ch_f, writes=[B_lnb2])
        B_lnb2.w = (ch_f.sem, ch_f.cnt)

        def fetch(T):
            k = T % 2
            rows = slice(T * 128, (T + 1) * 128)
            S.op(pool, lambda h: h.memset(ya_[k][:], 0.0), writes=[B_ya_[k]])
            S.op(pool, lambda h: h.memset(yb_[k][:], 0.0), writes=[B_yb_[k]])
            for s_, (dst, db, chn) in enumerate(((ya_[k], B_ya_[k], ch_g[2 * k]), (yb_[k], B_yb_[k], ch_g[2 * k + 1]))):
                for hc in range(2):
                    S.dma(pool, lambda h, dst=dst, s_=s_, hc=hc: h.indirect_dma_start(
                        out=dst[:, hc * 1024:(hc + 1) * 1024], out_offset=None, in_=ydh[hc][:, :],
                        in_offset=bass.IndirectOffsetOnAxis(ap=rdest[:, T, s_:s_ + 1], axis=0),
                        bounds_check=bc_reg, oob_is_err=False), chn, reads=[B_yd, B_rinfo], writes=[db])
            return load_x(x1_d[rows, :])

        pend = fetch(0)
        for T in range(16):
            rows = slice(T * 128, (T + 1) * 128)
            nxt = fetch(T + 1) if T + 1 < 16 else None
            t, tb = pend
            pend = nxt
            ya, yb2, B_ya, B_yb = ya_[T % 2], yb_[T % 2], B_ya_[T % 2], B_yb_[T % 2]
            o_ = ot[T % 2]
            ob = B_ot[T % 2]
            S.op(dve, lambda h, ya=ya: h.tensor_scalar(out=ya[:], in0=ya[:], scalar1=rinfo[:, T, 2:3], scalar2=None, op0=ALU.mult),
                 reads=[B_ya, B_rinfo], writes=[B_ya])
            S.op(dve, lambda h, ya=ya, yb2=yb2: h.scalar_tensor_tensor(out=ya[:], in0=yb2[:], scalar=rinfo[:, T, 3:4], in1=ya[:], op0=ALU.mult, op1=ALU.add),
                 reads=[B_ya, B_yb, B_rinfo], writes=[B_ya])
            S.op(dve, lambda h, ya=ya: h.tensor_mul(out=ya[:], in0=ya[:], in1=modbc[:, 1, :]), reads=[B_ya, B_modbc], writes=[B_ya])
            S.op(dve, lambda h, t=t, ya=ya: h.scalar_tensor_tensor(out=ya[:], in0=t[:], scalar=ALPHA, in1=ya[:], op0=ALU.mult, op1=ALU.add),
                 reads=[tb, B_ya], writes=[B_ya])
            ln_stats(ya[:], B_ya)
            S.op(dve, lambda h, ya=ya: h.tensor_scalar(out=ya[:], in0=ya[:], scalar1=lnmv[:, 0:1], scalar2=lnmv[:, 2:3],
                                                       op0=ALU.subtract, op1=ALU.mult), reads=[B_ya, B_ln], writes=[B_ya])
            S.op(dve, lambda h, ya=ya: h.tensor_mul(out=ya[:], in0=ya[:], in1=lnb2[:, 0, :]), reads=[B_ya, B_lnb2], writes=[B_ya])
            S.op(dve, lambda h, o_=o_, ya=ya: h.tensor_add(out=o_[:], in0=ya[:], in1=lnb2[:, 1, :]), reads=[B_ya, B_lnb2], writes=[ob])
            S.dma(sp, lambda h, o_=o_, rows=rows: h.dma_start(out=out[rows, :], in_=o_[:]), ch_o[T % 2], reads=[ob])
        dump("rinfo", rinfo[:], [128, 16, 4], F32, [B_rinfo])
        S.barrier(ch_o + [dbg_ch])
    es.close()
    return nc


def _prep_inputs(inp):
    f = lambda k: np.ascontiguousarray(np.asarray(inp[k], dtype=np.float32))
    x = f("x")
    c = f("c")
    iota = np.tile(np.arange(512, dtype=np.float32)[None, :], (128, 1))
    cst = np.zeros((128, 4, 128), np.float32)
    cst[:, 0, :] = np.eye(128, dtype=np.float32)
    cst[:, 1, :] = 1.0
    cst[:, 2, :] = np.triu(np.ones((128, 128), np.float32), 1)
    b_in = f("b_in")[0]
    a_re = f("s5_a_re")[0]
    a_im = f("s5_a_im")[0]
    ldt = f("s5_log_dt")[0]
    b_re = f("s5_b_re")[0]
    b_im = f("s5_b_im")[0]
    c_re = f("s5_c_re")[0]
    c_im = f("s5_c_im")[0]
    sd = f("s5_d")[0].reshape(512)
    dw = f("conv_dw")[0][:, 0, :]
    sm = np.zeros((128, 512), np.float32)
    sm[:, 16:68] = b_in.reshape(52, 128).T
    sm[:, 68:84] = a_re.reshape(16, 128).T
    sm[:, 84:100] = a_im.reshape(16, 128).T
    sm[:, 100:116] = np.repeat(ldt, 64).reshape(16, 128).T
    sm[:, 116:120] = sd.reshape(4, 128).T
    sm[:, 120:128] = f("conv_dw_b")[0].reshape(8, 128).T
    sm[:, 128:136] = f("conv_ln_g")[0].reshape(8, 128).T
    sm[:, 136:144] = f("conv_ln_b")[0].reshape(8, 128).T
    sm[:, 160:408] = dw.T.reshape(8, 128, 31).transpose(1, 0, 2).reshape(128, 248)
    wb = np.zeros((2, 128, 16, 128), np.float32)
    wc = np.zeros((2, 128, 16, 128), np.float32)
    for g in range(32):
        i = g // 2
        gl = g % 2
        ch0 = (g % 8) * 16
        st0 = gl * 64
        wb[0, ch0:ch0 + 16, i, st0:st0 + 64] = b_re[g].T
        wb[1, ch0:ch0 + 16, i, st0:st0 + 64] = b_im[g].T
        wc[0, st0:st0 + 64, i, ch0:ch0 + 16] = c_re[g].T
        wc[1, st0:st0 + 64, i, ch0:ch0 + 16] = c_im[g].T
    lnbc = np.stack([np.tile(f(k)[0][None, :], (128, 1)) for k in ("ln1_g", "ln1_b", "ln2_g", "ln2_b")])
    w_route = np.ascontiguousarray(np.concatenate([f("w_route_group")[0], f("w_route_expert")[0]], axis=1))
    b_route = np.tile(np.concatenate([f("b_route_group")[0], f("b_route_expert")[0]])[None, :], (128, 1)).astype(np.float32)
    shared = {
        "iota": iota, "cst": cst, "w_ada": f("w_ada")[0], "b_ada": f("b_ada"), "w_in": f("w_in")[0],
        "wb": wb, "wc": wc, "w_s5_gate": f("w_s5_gate")[0], "w_s5_up": f("w_s5_up")[0],
        "w_conv_out": f("w_conv_out")[0], "w_out": f("w_out")[0], "lnbc": lnbc, "w_route": w_route,
        "b_route": b_route, "w_exp_gate": f("w_exp_gate")[0], "w_exp_up": f("w_exp_up")[0], "w_exp_down": f("w_exp_down")[0],
    }
    maps = []
    for core in range(8):
        b, half = core // 2, core % 2
        smc = sm.copy()
        smc[:, 0:16] = c[b].reshape(16, 128).T
        smc[:, 144] = float(half)
        m = dict(shared)
        m["smallp"] = smc
        m["x_own"] = np.ascontiguousarray(x[b, half * NTOK:(half + 1) * NTOK])
        m["x_prev"] = np.ascontiguousarray(x[b, 0:NTOK]) if half == 1 else np.zeros((NTOK, D), np.float32)
        maps.append(m)
    return maps


_NC = [None]


def kernel(**inputs):
    maps = _prep_inputs(inputs)
    if _NC[0] is None:
        _NC[0] = build_nc()
    res = run_bass_kernel_spmd(_NC[0], maps, core_ids=list(range(8)))
    outp = np.zeros((4, 2 * NTOK, D), np.float32)
    for core in range(8):
        b, half = core // 2, core % 2
        outp[b, half * NTOK:(half + 1) * NTOK] = res.results[core]["out"]
    if DEBUG:
        kernel.dbg = [res.results[c] for c in range(8)]
    return outp
```
